# Optimizing a Trainium2 kernel written in Bass

```python
import jax, jax.numpy as jnp
from jax import lax
import numpy as np

D_MODEL = 1024
BATCH = 8
SEQ = 4096
DEPTH = 1

N_META = 16
CHUNK = 128
PAD = CHUNK - N_META
CONF_WIDTH = D_MODEL
CONF_KERNEL = 31
SSM_INNER = 2 * D_MODEL
SSM_HEAD_DIM = 64
SSM_HEADS = SSM_INNER // SSM_HEAD_DIM
SSM_GROUPS = 4
SSM_STATE = 128
SSM_CONV = 7
XBC_WIDTH = SSM_INNER + 2 * SSM_GROUPS * SSM_STATE
N_BRANCHES = 2
N_EXPERTS = 16
CAPACITY_FACTOR = 2
EXPERT_FF = 2 * D_MODEL
OFF_CONF = 0
OFF_Z = OFF_CONF + 2 * CONF_WIDTH
OFF_XBC = OFF_Z + SSM_INNER
OFF_DT = OFF_XBC + XBC_WIDTH
OFF_GATE = OFF_DT + 2 * SSM_HEADS
IN_WIDTH = OFF_GATE + N_BRANCHES * D_MODEL
EPS = 1e-6

kernel_name = "hybrid_conformer_ssd_ecmoe_encoder"


def rms_norm(x, w):
    xf = x.astype(jnp.float32)
    y = xf * lax.rsqrt(jnp.mean(xf * xf, axis=-1, keepdims=True) + EPS)
    return (y * w.astype(jnp.float32)).astype(x.dtype)


def layer_norm(x, g, b):
    xf = x.astype(jnp.float32)
    mu = jnp.mean(xf, axis=-1, keepdims=True)
    var = jnp.mean(jnp.square(xf - mu), axis=-1, keepdims=True)
    y = (xf - mu) * lax.rsqrt(var + EPS)
    return (y * g.astype(jnp.float32) + b.astype(jnp.float32)).astype(x.dtype)


def depthwise_conv(x, w, b):
    y = lax.conv_general_dilated(x, w[:, None, :].astype(x.dtype), window_strides=(1,), padding='SAME',
                                 dimension_numbers=('NWC', 'WIO', 'NWC'), feature_group_count=x.shape[-1])
    return y + b.astype(x.dtype)


def pad_front(a):
    return jnp.pad(a, [(0, 0), (PAD, 0)] + [(0, 0)] * (a.ndim - 2))


def ssd_scan(x, dt, A, B, C):
    b, T, h, p = x.shape
    g, n = B.shape[2], B.shape[3]
    j = h // g
    c = T // CHUNK
    xd = (x * dt[..., None]).reshape(b, c, CHUNK, g, j, p)
    a = jnp.moveaxis((dt * A).reshape(b, c, CHUNK, g, j), 2, -1)
    a_cs = jnp.cumsum(a, axis=-1)
    Bc = B.reshape(b, c, CHUNK, g, n)
    Cc = C.reshape(b, c, CHUNK, g, n)
    seg = a_cs[..., :, None] - a_cs[..., None, :]
    mask = jnp.tril(jnp.ones((CHUNK, CHUNK), dtype=bool))
    decay = jnp.exp(jnp.where(mask, seg, -jnp.inf))
    cb = jnp.einsum('bclgn,bcsgn->bcgls', Cc, Bc)
    y_diag = jnp.einsum('bcgls,bcgjls,bcsgjp->bclgjp', cb, decay, xd)
    decay_to_end = jnp.exp(a_cs[..., -1:] - a_cs)
    states = jnp.einsum('bclgn,bcgjl,bclgjp->bcgjpn', Bc, decay_to_end, xd)
    chunk_decay = jnp.exp(a_cs[..., -1])

    def step(carry, inp):
        st, dec = inp
        return carry * dec[..., None, None] + st, carry

    init = jnp.zeros((b, g, j, p, n), dtype=states.dtype)
    _, prev = lax.scan(step, init, (jnp.moveaxis(states, 1, 0), jnp.moveaxis(chunk_decay, 1, 0)))
    prev = jnp.moveaxis(prev, 0, 1)
    y_off = jnp.einsum('bclgn,bcgjpn,bcgjl->bclgjp', Cc, prev, jnp.exp(a_cs))
    return (y_diag + y_off).reshape(b, T, h, p)


def bidir_ssd(xs, dt_raw, dt_bias, a_log, Bs, Cs, d_skip):
    f32 = jnp.float32
    xf, Bf, Cf = xs.astype(f32), Bs.astype(f32), Cs.astype(f32)
    dtr = dt_raw.astype(f32)
    dt_f = jax.nn.softplus(dtr[..., :SSM_HEADS] + dt_bias[0].astype(f32))
    dt_b = jax.nn.softplus(dtr[..., SSM_HEADS:] + dt_bias[1].astype(f32))
    A = -jnp.exp(a_log.astype(f32))
    xp, Bp, Cp = pad_front(xf), pad_front(Bf), pad_front(Cf)
    y_f = ssd_scan(xp, pad_front(dt_f), A[0], Bp, Cp)
    fl = lambda t: jnp.flip(t, axis=1)
    y_b = fl(ssd_scan(fl(xp), fl(pad_front(dt_b)), A[1], fl(Bp), fl(Cp)))
    y = (y_f + y_b)[:, PAD:] + d_skip.astype(f32)[:, None] * xf
    return y.astype(xs.dtype)


def expert_choice_ffn(xn, w_router, w_gate, w_up, w_down):
    b, T, d = xn.shape
    cap = CAPACITY_FACTOR * T // N_EXPERTS
    aff = jax.nn.softmax(jnp.einsum('btd,de->bte', xn, w_router).astype(jnp.float32), axis=-1)
    top_aff, top_idx = lax.top_k(jnp.swapaxes(aff, 1, 2), cap)
    bi = jnp.arange(b)[:, None, None]
    xg = xn[bi, top_idx]
    hg = jnp.einsum('becd,edf->becf', xg, w_gate)
    hu = jnp.einsum('becd,edf->becf', xg, w_up)
    y = jnp.einsum('becf,efd->becd', jax.nn.silu(hg) * hu, w_down)
    y = y * top_aff[..., None].astype(y.dtype)
    return jnp.zeros_like(xn).at[bi, top_idx].add(y)


def setup_inputs(seed: int = 0) -> dict:
    key = jax.random.key(seed)
    ks = jax.random.split(key, 24)
    f32 = jnp.float32
    nrm = lambda k, shape, scale: jax.random.normal(k, shape, f32) * scale
    gain = lambda k, shape: 1.0 + 0.02 * jax.random.normal(k, shape, f32)
    L = DEPTH
    dt0 = jnp.exp(jax.random.uniform(ks[10], (L, 2, SSM_HEADS), f32, np.log(1e-3), np.log(1e-1)))
    dt_bias = dt0 + jnp.log(-jnp.expm1(-dt0))
    a_log = jnp.log(jax.random.uniform(ks[11], (L, 2, SSM_HEADS), f32, 1.0, 16.0))
    return {
        "x": jax.random.normal(ks[0], (BATCH, SEQ, D_MODEL), f32),
        "meta_tokens": nrm(ks[1], (N_META, D_MODEL), 1.0),
        "w_norm_mix": gain(ks[2], (L, D_MODEL)),
        "w_in": nrm(ks[3], (L, D_MODEL, IN_WIDTH), D_MODEL ** -0.5),
        "w_conf_dw": nrm(ks[4], (L, CONF_KERNEL, CONF_WIDTH), CONF_KERNEL ** -0.5),
        "b_conf_dw": nrm(ks[5], (L, CONF_WIDTH), 0.02),
        "conf_ln_g": gain(ks[6], (L, CONF_WIDTH)),
        "conf_ln_b": nrm(ks[7], (L, CONF_WIDTH), 0.02),
        "w_conf_out": nrm(ks[8], (L, CONF_WIDTH, D_MODEL), CONF_WIDTH ** -0.5),
        "w_ssm_conv": nrm(ks[9], (L, SSM_CONV, XBC_WIDTH), SSM_CONV ** -0.5),
        "b_ssm_conv": nrm(ks[12], (L, XBC_WIDTH), 0.02),
        "ssm_dt_bias": dt_bias,
        "ssm_a_log": a_log,
        "ssm_d": gain(ks[13], (L, SSM_HEADS)),
        "w_ssm_norm": gain(ks[14], (L, SSM_INNER)),
        "w_ssm_out": nrm(ks[15], (L, SSM_INNER, D_MODEL), SSM_INNER ** -0.5),
        "w_out": nrm(ks[16], (L, D_MODEL, D_MODEL), D_MODEL ** -0.5),
        "w_norm_ffn": gain(ks[17], (L, D_MODEL)),
        "w_router": nrm(ks[18], (L, D_MODEL, N_EXPERTS), D_MODEL ** -0.5),
        "w_exp_gate": nrm(ks[19], (L, N_EXPERTS, D_MODEL, EXPERT_FF), D_MODEL ** -0.5),
        "w_exp_up": nrm(ks[20], (L, N_EXPERTS, D_MODEL, EXPERT_FF), D_MODEL ** -0.5),
        "w_exp_down": nrm(ks[21], (L, N_EXPERTS, EXPERT_FF, D_MODEL), EXPERT_FF ** -0.5),
        "w_norm_final": gain(ks[22], (D_MODEL,)),
    }


def reference(x, meta_tokens, w_norm_mix, w_in, w_conf_dw, b_conf_dw, conf_ln_g, conf_ln_b, w_conf_out,
              w_ssm_conv, b_ssm_conv, ssm_dt_bias, ssm_a_log, ssm_d, w_ssm_norm, w_ssm_out, w_out,
              w_norm_ffn, w_router, w_exp_gate, w_exp_up, w_exp_down, w_norm_final):
    b = x.shape[0]
    meta = jnp.broadcast_to(meta_tokens[None].astype(x.dtype), (b, N_META, D_MODEL))
    h = jnp.concatenate([meta, x], axis=1)
    Lt = h.shape[1]
    for l in range(DEPTH):
        u = rms_norm(h, w_norm_mix[l])
        proj = jnp.einsum('btd,dk->btk', u, w_in[l])
        conf_a = proj[..., OFF_CONF:OFF_CONF + CONF_WIDTH]
        conf_g = proj[..., OFF_CONF + CONF_WIDTH:OFF_Z]
        z = proj[..., OFF_Z:OFF_XBC]
        xbc = proj[..., OFF_XBC:OFF_DT]
        dt_raw = proj[..., OFF_DT:OFF_GATE]
        gates = jax.nn.sigmoid(proj[..., OFF_GATE:].astype(jnp.float32)).astype(h.dtype)

        c = conf_a * jax.nn.sigmoid(conf_g)
        c = depthwise_conv(c, w_conf_dw[l], b_conf_dw[l])
        c = jax.nn.silu(layer_norm(c, conf_ln_g[l], conf_ln_b[l]))
        branch_conf = jnp.einsum('btc,cd->btd', c, w_conf_out[l])

        xbc = jax.nn.silu(depthwise_conv(xbc, w_ssm_conv[l], b_ssm_conv[l]))
        xs = xbc[..., :SSM_INNER].reshape(b, Lt, SSM_HEADS, SSM_HEAD_DIM)
        Bs = xbc[..., SSM_INNER:SSM_INNER + SSM_GROUPS * SSM_STATE].reshape(b, Lt, SSM_GROUPS, SSM_STATE)
        Cs = xbc[..., SSM_INNER + SSM_GROUPS * SSM_STATE:].reshape(b, Lt, SSM_GROUPS, SSM_STATE)
        y = bidir_ssd(xs, dt_raw, ssm_dt_bias[l], ssm_a_log[l], Bs, Cs, ssm_d[l]).reshape(b, Lt, SSM_INNER)
        y = rms_norm(y * jax.nn.silu(z), w_ssm_norm[l])
        branch_ssm = jnp.einsum('btc,cd->btd', y, w_ssm_out[l])

        merged = gates[..., :D_MODEL] * branch_conf + gates[..., D_MODEL:] * branch_ssm
        h = h + jnp.einsum('btd,de->bte', merged, w_out[l])

        hn = rms_norm(h, w_norm_ffn[l])
        h = h + expert_choice_ffn(hn, w_router[l], w_exp_gate[l], w_exp_up[l], w_exp_down[l])
    return rms_norm(h[:, N_META:], w_norm_final)
```

```python
import os
import numpy as np
import ml_dtypes
from contextlib import ExitStack
import concourse.bass as bass
import concourse.mybir as mybir
from concourse.bass_utils import run_bass_kernel_spmd

F32 = mybir.dt.float32
F32R = mybir.dt.float32r
BF16 = mybir.dt.bfloat16
I32 = mybir.dt.int32
AF = mybir.ActivationFunctionType
ALU = mybir.AluOpType
AX = mybir.AxisListType

T = 4224
NT = 33
D = 1024
CAP = 514
NE = 16
TBS = [(i * 512, 512) for i in range(8)] + [(4096, 128)]
EPS = 1e-6
S_BCONF, S_LNG, S_LNB, S_WCONF, S_WSSM, S_BSSM, S_N = 0, 8, 16, 24, 272, 440, 464
B_WMIX, B_WSSM, B_WFFN, B_WFIN, B_DTB, B_ALOG, B_DSK, B_N = 0, 1024, 3072, 4096, 5120, 5184, 5248, 5280
C_UF, C_LF, C_UB, C_LB, C_ONES, C_ID, C_IOTA, C_N = 0, 128, 256, 384, 512, 640, 768, 768 + 516
CB_ID, CB_ONES, CB_TV, CB_N = 0, 128, 256, 256 + 66


class Buf:
    __slots__ = ("w", "r")

    def __init__(self):
        self.w = None
        self.r = {}


class KB:
    EPOCH = 2048

    def __init__(self, nc, st):
        self.nc = nc
        self.st = st
        self.eng = dict(pe=nc.tensor, act=nc.scalar, dve=nc.vector, pool=nc.gpsimd, sp=nc.sync)
        self.cnt = {e: 0 for e in self.eng}
        self.sems = {e: [] for e in self.eng}
        self.known = {e: {} for e in self.eng}
        self.semh = []
        self.origin = []
        self.dq = {}
        self.enabled = True

    def newsem(self, origin):
        h = self.st.enter_context(self.nc.semaphore("s%d" % len(self.semh)))
        self.semh.append(h)
        self.origin.append(origin)
        return len(self.semh) - 1

    def wait(self, e, toks, keep_one=False):
        need = {}
        kn = self.known[e]
        for sid, val in toks:
            if e == "pe" and self.origin[sid] == "pe":
                continue
            if kn.get(sid, 0) < val and need.get(sid, 0) < val:
                need[sid] = val
        items = list(need.items())
        attach = items.pop() if (keep_one and items) else None
        for sid, val in items:
            self.eng[e].wait_ge(self.semh[sid], val)
            kn[sid] = val
        if attach is not None:
            kn[attach[0]] = attach[1]
        return attach

    def _deps(self, rd, wr):
        toks = []
        for b in rd:
            if b.w is not None:
                toks.append(b.w)
        for b in wr:
            if b.w is not None:
                toks.append(b.w)
            toks.extend(b.r.items())
        return toks

    def _mark(self, tok, rd, wr):
        sid, val = tok
        for b in rd:
            if b.r.get(sid, 0) < val:
                b.r[sid] = val
        for b in wr:
            b.w = tok
            b.r = {}

    def op(self, e, fn, rd=(), wr=()):
        if not self.enabled:
            return None
        attach = self.wait(e, self._deps(rd, wr), keep_one=True)
        ins = fn(self.eng[e])
        if isinstance(ins, (list, tuple)):
            first, ins = ins[0], ins[-1]
        else:
            first = ins
        if attach is not None:
            first.wait_op(self.semh[attach[0]], attach[1], "sem-ge")
        self.cnt[e] += 1
        k = self.cnt[e]
        ep = (k - 1) // self.EPOCH
        while len(self.sems[e]) <= ep:
            self.sems[e].append(self.newsem(e))
        sid = self.sems[e][ep]
        val = (k - 1) % self.EPOCH + 1
        ins.then_inc(self.semh[sid], 1)
        tok = (sid, val)
        self._mark(tok, rd, wr)
        return tok

    def barrier(self):
        toks = []
        for e in self.eng:
            if self.cnt[e] > 0:
                k = self.cnt[e]
                toks.append((self.sems[e][(k - 1) // self.EPOCH], (k - 1) % self.EPOCH + 1))
        for pool in self.dq.values():
            toks.extend((sid, val) for sid, val in zip(pool["sids"], pool["vals"]) if val > 0)
        for e in self.eng:
            self.wait(e, toks)

    def dma(self, q, fn, rd=(), wr=(), nslots=16):
        if not self.enabled:
            return None
        self.wait(q, self._deps(rd, wr))
        pool = self.dq.setdefault(q, dict(sids=[], vals=[], n=0))
        i = pool["n"] % nslots
        pool["n"] += 1
        if len(pool["sids"]) <= i:
            pool["sids"].append(self.newsem("dma"))
            pool["vals"].append(0)
        sid = pool["sids"][i]
        if pool["vals"][i] > 0:
            self.wait(q, [(sid, pool["vals"][i])])
        ins = fn(self.eng[q])
        val = pool["vals"][i] + 16
        pool["vals"][i] = val
        ins.then_inc(self.semh[sid], 16)
        tok = (sid, val)
        self._mark(tok, rd, wr)
        return tok


def build_program(run="ABbCDEFGH", debug=False):
    nc = bass.Bass("TRN2", target_bir_lowering=False)
    dt_ = nc.dram_tensor
    x_d = dt_("x", [4096, D], F32, kind="ExternalInput").ap()
    meta_d = dt_("meta", [16, D], F32, kind="ExternalInput").ap()
    w_in = dt_("w_in", [D, 9280], F32, kind="ExternalInput").ap()
    w_co = dt_("w_conf_out", [D, D], F32, kind="ExternalInput").ap()
    w_so = dt_("w_ssm_out", [2048, D], F32, kind="ExternalInput").ap()
    w_o = dt_("w_out", [D, D], F32, kind="ExternalInput").ap()
    w_r = dt_("w_router", [D, NE], F32, kind="ExternalInput").ap()
    ned = NE if ("G" in run or os.environ.get("FORCE_NE") == "1") else 1
    w_eg = dt_("w_eg", [ned, D, 2048], F32, kind="ExternalInput").ap()
    w_eu = dt_("w_eu", [ned, D, 2048], F32, kind="ExternalInput").ap()
    w_ed = dt_("w_ed", [ned, 2048, D], F32, kind="ExternalInput").ap()
    small_d = dt_("smallT", [128, S_N], F32, kind="ExternalInput").ap()
    bc_d = dt_("bcast", [128, B_N], F32, kind="ExternalInput").ap()
    cf_d = dt_("cf32", [128, C_N], F32, kind="ExternalInput").ap()
    cb_d = dt_("cbf", [128, CB_N], BF16, kind="ExternalInput").ap()
    out_d = dt_("out", [4096, D], F32, kind="ExternalOutput").ap()
    skw = dict(kind="ExternalOutput") if debug else {}
    conv_d = dt_("conv_s", [128, 8, T], BF16, **skw).ap()
    m1_d = dt_("m1_s", [128, 8, T], BF16, **skw).ap()
    g2_d = dt_("g2_s", [128, 8, T], BF16, **skw).ap()
    xbc_d = dt_("xbc_s", [128, 24, T], BF16, **skw).ap()
    sz_d = dt_("sz_s", [NT, 128, 2048], BF16, **skw).ap()
    yb_d = dt_("yb_s", [NT, 128, 2048], BF16, **skw).ap()
    yn_d = dt_("yn_s", [NT, 128, 2048], BF16, **skw).ap()
    hn_d = dt_("hn_s", [T, D], BF16, **skw).ap()
    hacc_d = dt_("hacc_s", [T, D], F32, **skw).ap()
    uT_dbg = dt_("uT_s", [128, 8, T], BF16, **skw).ap() if debug else None
    dt_dbg = dt_("dt_s", [128, NT, 64], F32, **skw).ap() if debug else None
    aff_dbg = dt_("aff_s", [128, NT, NE], F32, **skw).ap() if debug else None
    posm_dbg = dt_("posm_s", [128, NT, NE], F32, **skw).ap() if debug else None

    win_v = w_in.rearrange("(kc p) n -> p kc n", p=128)

    with ExitStack() as st:
        kb = KB(nc, st)

        def sb(name, shape, dtype, stack=None):
            return (stack or st).enter_context(nc.sbuf_tensor("sb_" + name, shape, dtype))

        ps = [st.enter_context(nc.psum_tensor("ps%d" % i, [128, 512], F32)) for i in range(8)]
        psb = [Buf() for _ in range(8)]
        psn = [0]

        def bank():
            i = psn[0] % 8
            psn[0] += 1
            return ps[i], psb[i]

        cf = sb("cf", [128, C_N], F32)
        cbf = sb("cbf", [128, CB_N], BF16)
        sm = sb("sm", [128, S_N], F32)
        b_cf, b_cbf, b_sm = Buf(), Buf(), Buf()
        kb.dma("sp", lambda q: q.dma_start(out=cf[:], in_=cf_d), wr=[b_cf])
        kb.dma("sp", lambda q: q.dma_start(out=cbf[:], in_=cb_d), wr=[b_cbf])
        kb.dma("sp", lambda q: q.dma_start(out=sm[:], in_=small_d), wr=[b_sm])
        identb = cbf[:, CB_ID:CB_ID + 128]
        onesb = cbf[:, CB_ONES:CB_ONES + 128]
        identf = cf[:, C_ID:C_ID + 128]

        dtall = sb("dtall", [128, NT, 64], F32)
        b_dt = [Buf() for _ in range(NT)]

        def load_x_tile(i, xin, b_xin):
            if i == 0:
                kb.op("dve", lambda e: e.memset(xin[:], 0.0), wr=[b_xin])
                kb.dma("sp", lambda q: q.dma_start(out=xin[112:128, :], in_=meta_d), wr=[b_xin])
            else:
                kb.dma("sp", lambda q: q.dma_start(out=xin[:], in_=x_d[(i - 1) * 128:i * 128, :]), wr=[b_xin])

        def rms_rstd(src, b_src, junk, b_junk, ss, b_ss, rstd, b_rstd, n):
            kb.op("act", lambda e: e.activation(out=junk, in_=src, func=AF.Square, accum_out=ss[:]),
                  rd=[b_src], wr=[b_junk, b_ss])
            kb.op("act", lambda e: e.activation(out=ss[:], in_=ss[:], func=AF.Sqrt, bias=EPS, scale=1.0 / n),
                  rd=[b_ss], wr=[b_ss])
            kb.op("dve", lambda e: e.reciprocal(out=rstd[:], in_=ss[:]), rd=[b_ss], wr=[b_rstd])

        with ExitStack() as s1:
            uT = sb("uT", [128, 8, T], BF16, s1)
            b_uT = [Buf() for _ in range(NT)]

            def uT_bufs(t0, n):
                return b_uT[t0 // 128:(t0 + n) // 128]

            with ExitStack() as sa:
                kb.enabled = "A" in run
                wmix = sb("wmix", [128, D], F32, sa)
                b_wmix = Buf()
                kb.dma("sp", lambda q: q.dma_start(out=wmix[:], in_=bc_d[:, B_WMIX:B_WMIX + D]), wr=[b_wmix])
                xin = [sb("xinA%d" % i, [128, D], F32, sa) for i in range(4)]
                b_xin = [Buf() for _ in range(4)]
                junk = [sb("junkA%d" % i, [128, D], BF16, sa) for i in range(2)]
                b_junk = [Buf(), Buf()]
                ub = [sb("ubA%d" % i, [128, D], BF16, sa) for i in range(4)]
                b_ub = [Buf() for _ in range(4)]
                ss = [sb("ssA%d" % i, [128, 1], F32, sa) for i in range(4)]
                rs = [sb("rsA%d" % i, [128, 1], F32, sa) for i in range(4)]
                b_ss = [Buf() for _ in range(4)]
                b_rs = [Buf() for _ in range(4)]
                for i0 in range(0, NT, 4):
                    tiles = list(range(i0, min(i0 + 4, NT)))
                    for p, i in enumerate(tiles):
                        load_x_tile(i, xin[p], b_xin[p])
                    for p, i in enumerate(tiles):
                        kb.op("act", lambda e: e.activation(out=junk[p % 2][:], in_=xin[p][:], func=AF.Square, accum_out=ss[p][:]),
                              rd=[b_xin[p]], wr=[b_junk[p % 2], b_ss[p]])
                    for p, i in enumerate(tiles):
                        kb.op("act", lambda e: e.activation(out=ss[p][:], in_=ss[p][:], func=AF.Sqrt, bias=EPS, scale=1.0 / D),
                              rd=[b_ss[p]], wr=[b_ss[p]])
                    for p, i in enumerate(tiles):
                        kb.op("dve", lambda e: e.reciprocal(out=rs[p][:], in_=ss[p][:]), rd=[b_ss[p]], wr=[b_rs[p]])
                    for p, i in enumerate(tiles):
                        kb.op("dve", lambda e: e.scalar_tensor_tensor(out=ub[p][:], in0=xin[p][:], scalar=rs[p][:, 0:1],
                                                                       in1=wmix[:], op0=ALU.mult, op1=ALU.mult),
                              rd=[b_xin[p], b_rs[p], b_wmix], wr=[b_ub[p]])
                    for p, i in enumerate(tiles):
                        pt, pb = bank()
                        ptb = pt[:].bitcast(BF16)
                        kb.op("pe", lambda e: [e.transpose(ptb[:, kc * 128:(kc + 1) * 128],
                                                           ub[p][:, kc * 128:(kc + 1) * 128], identb)
                                               for kc in range(8)],
                              rd=[b_ub[p], b_cbf], wr=[pb])
                        kb.op("act", lambda e: e.activation(out=uT[:, :, i * 128:(i + 1) * 128],
                                                            in_=ptb.rearrange("p (k t) -> p k t", k=8), func=AF.Copy),
                              rd=[pb], wr=[b_uT[i]])

            kb.barrier()
            if debug:
                kb.enabled = "A" in run
                kb.dma("sp", lambda q: q.dma_start(out=uT_dbg, in_=uT[:]), rd=b_uT, wr=[Buf()])
            with ExitStack() as sbk:
                kb.enabled = "B" in run
                wA = [sb("wA%d" % i, [128, 8, 256], BF16, sbk) for i in range(2)]
                b_wA = [Buf(), Buf()]
                cT = [sb("cT%d" % i, [128, T + 30], BF16, sbk) for i in range(2)]
                b_cT = [Buf(), Buf()]
                dg = [sb("dg%d" % i, [128, 31, 128], BF16, sbk) for i in range(2)]
                b_dg = [Buf(), Buf()]
                sgt = [sb("sgt%d" % i, [128, 512], BF16, sbk) for i in range(2)]
                b_sgt = [Buf(), Buf()]
                cv = [sb("cv%d" % i, [128, 512], BF16, sbk) for i in range(2)]
                b_cv = [Buf(), Buf()]
                b_conv = [[Buf() for _ in TBS] for _ in range(8)]
                for i in range(2):
                    kb.op("pool", lambda e: e.memset(cT[i][:], 0.0), wr=[b_cT[i]])
                it = 0
                for j in range(8):
                    p = j % 2
                    kb.dma("pool", lambda q: q.dma_start(out=wA[p][:, :, 0:128], in_=win_v[:, :, j * 128:(j + 1) * 128]),
                           wr=[b_wA[p]])
                    kb.dma("pool", lambda q: q.dma_start(out=wA[p][:, :, 128:256],
                                                         in_=win_v[:, :, 1024 + j * 128:1024 + (j + 1) * 128]),
                           wr=[b_wA[p]])
                    for k in range(31):
                        kb.op("dve", lambda e: e.tensor_scalar(out=dg[p][:, k, :], in0=identb,
                                                               scalar1=sm[:, S_WCONF + j * 31 + k:S_WCONF + j * 31 + k + 1],
                                                               scalar2=None, op0=ALU.mult),
                              rd=[b_cbf, b_sm], wr=[b_dg[p]])
                    for (t0, n) in TBS:
                        pa, pab = bank()
                        pg, pgb = bank()
                        kb.op("pe", lambda e: [e.matmul(pa[:, 0:n], wA[p][:, kc, 0:128], uT[:, kc, t0:t0 + n],
                                                        start=(kc == 0), stop=(kc == 7)) for kc in range(8)],
                              rd=[b_wA[p]] + uT_bufs(t0, n), wr=[pab])
                        kb.op("pe", lambda e: [e.matmul(pg[:, 0:n], wA[p][:, kc, 128:256], uT[:, kc, t0:t0 + n],
                                                        start=(kc == 0), stop=(kc == 7)) for kc in range(8)],
                              rd=[b_wA[p]] + uT_bufs(t0, n), wr=[pgb])
                        q = it % 2
                        it += 1
                        kb.op("act", lambda e: e.activation(out=sgt[q][:, 0:n], in_=pg[:, 0:n], func=AF.Sigmoid),
                              rd=[pgb], wr=[b_sgt[q]])
                        kb.op("dve", lambda e: e.tensor_tensor(out=cT[p][:, 15 + t0:15 + t0 + n], in0=pa[:, 0:n],
                                                               in1=sgt[q][:, 0:n], op=ALU.mult),
                              rd=[pab, b_sgt[q]], wr=[b_cT[p]])
                    for ti, (t0, n) in enumerate(TBS):
                        pc, pcb = bank()
                        kb.op("pe", lambda e: [e.matmul(pc[:, 0:n], dg[p][:, k, :], cT[p][:, t0 + k:t0 + k + n],
                                                        start=(k == 0), stop=(k == 30)) for k in range(31)],
                              rd=[b_dg[p], b_cT[p]], wr=[pcb])
                        q = it % 2
                        it += 1
                        kb.op("act", lambda e: e.activation(out=cv[q][:, 0:n], in_=pc[:, 0:n], func=AF.Identity,
                                                            bias=sm[:, S_BCONF + j:S_BCONF + j + 1]),
                              rd=[pcb, b_sm], wr=[b_cv[q]])
                        kb.dma("sp", lambda qq: qq.dma_start(out=conv_d[:, j, t0:t0 + n], in_=cv[q][:, 0:n]),
                               rd=[b_cv[q]], wr=[b_conv[j][ti]])

            kb.barrier()
            with ExitStack() as sb2:
                kb.enabled = "b" in run
                wco = sb("wco", [128, 8, D], BF16, sb2)
                wg1 = sb("wg1", [128, 8, D], BF16, sb2)
                wg2 = sb("wg2", [128, 8, D], BF16, sb2)
                b_wco, b_wg1, b_wg2 = Buf(), Buf(), Buf()
                kb.dma("pool", lambda q: q.dma_start(out=wco[:], in_=w_co.rearrange("(kc p) n -> p kc n", p=128)),
                       wr=[b_wco])
                kb.dma("pool", lambda q: q.dma_start(out=wg1[:], in_=win_v[:, :, 7232:7232 + D]), wr=[b_wg1])
                kb.dma("pool", lambda q: q.dma_start(out=wg2[:], in_=win_v[:, :, 8256:8256 + D]), wr=[b_wg2])
                cvb = [sb("cvb%d" % i, [128, 8, 512], BF16, sb2) for i in range(2)]
                b_cvb = [Buf(), Buf()]
                sq = sb("sq", [128, 8, 512], BF16, sb2)
                b_sq = Buf()
                mean = sb("mean", [128, 512], F32, sb2)
                msq = sb("msq", [128, 512], F32, sb2)
                rstd = sb("rstdb", [128, 512], F32, sb2)
                nmr = sb("nmr", [128, 512], F32, sb2)
                b_mean, b_msq, b_rstd, b_nmr = Buf(), Buf(), Buf(), Buf()
                t2 = [sb("t2_%d" % i, [128, 512], F32, sb2) for i in range(2)]
                b_t2 = [Buf(), Buf()]
                cs2 = [sb("cs%d" % i, [128, 8, 512], BF16, sb2) for i in range(2)]
                b_cs2 = [Buf(), Buf()]
                sg = [sb("sg%d" % i, [128, 512], BF16, sb2) for i in range(2)]
                b_sg = [Buf(), Buf()]
                m1 = [sb("m1_0", [128, 8, 512], BF16, sb2)] * 2
                b_m1 = [Buf()] * 2
                g2 = [sb("g2_0", [128, 8, 512], BF16, sb2)] * 2
                b_g2 = [Buf()] * 2
                b_m1d = [Buf() for _ in TBS]
                b_g2d = [Buf() for _ in TBS]
                itc = [0]

                def b_LN(ti):
                    t0, n = TBS[ti]
                    it = itc[0]
                    p = ti % 2
                    cs, b_cs = cs2[p], b_cs2[p]
                    kb.dma("sp", lambda q: q.dma_start(out=cvb[p][:, :, 0:n], in_=conv_d[:, :, t0:t0 + n]),
                           rd=[b_conv[j][ti] for j in range(8)], wr=[b_cvb[p]])
                    kb.op("pool", lambda e: e.tensor_tensor(out=sq[:, :, 0:n], in0=cvb[p][:, :, 0:n],
                                                            in1=cvb[p][:, :, 0:n], op=ALU.mult),
                          rd=[b_cvb[p]], wr=[b_sq])
                    p1, p1b = bank()
                    p2, p2b = bank()
                    kb.op("pe", lambda e: [e.matmul(p1[:, 0:n], onesb, cvb[p][:, j, 0:n], start=(j == 0), stop=(j == 7))
                                           for j in range(8)], rd=[b_cbf, b_cvb[p]], wr=[p1b])
                    kb.op("pe", lambda e: [e.matmul(p2[:, 0:n], onesb, sq[:, j, 0:n], start=(j == 0), stop=(j == 7))
                                           for j in range(8)], rd=[b_cbf, b_sq], wr=[p2b])
                    kb.op("dve", lambda e: e.tensor_scalar(out=mean[:, 0:n], in0=p1[:, 0:n], scalar1=1.0 / D,
                                                           scalar2=None, op0=ALU.mult), rd=[p1b], wr=[b_mean])
                    kb.op("dve", lambda e: e.tensor_tensor(out=msq[:, 0:n], in0=mean[:, 0:n], in1=mean[:, 0:n],
                                                           op=ALU.mult), rd=[b_mean], wr=[b_msq])
                    kb.op("dve", lambda e: e.scalar_tensor_tensor(out=msq[:, 0:n], in0=p2[:, 0:n], scalar=1.0 / D,
                                                                  in1=msq[:, 0:n], op0=ALU.mult, op1=ALU.subtract),
                          rd=[p2b, b_msq], wr=[b_msq])
                    kb.op("act", lambda e: e.activation(out=msq[:, 0:n], in_=msq[:, 0:n], func=AF.Sqrt, bias=EPS),
                          rd=[b_msq], wr=[b_msq])
                    kb.op("dve", lambda e: e.reciprocal(out=rstd[:, 0:n], in_=msq[:, 0:n]), rd=[b_msq], wr=[b_rstd])
                    kb.op("dve", lambda e: e.scalar_tensor_tensor(out=nmr[:, 0:n], in0=mean[:, 0:n], scalar=-1.0,
                                                                  in1=rstd[:, 0:n], op0=ALU.mult, op1=ALU.mult),
                          rd=[b_mean, b_rstd], wr=[b_nmr])
                    for j in range(8):
                        q = it % 2
                        it += 1
                        kb.op("dve", lambda e: e.tensor_tensor(out=t2[q][:, 0:n], in0=cvb[p][:, j, 0:n],
                                                               in1=rstd[:, 0:n], op=ALU.mult),
                              rd=[b_cvb[p], b_rstd], wr=[b_t2[q]])
                        kb.op("dve", lambda e: e.tensor_tensor(out=t2[q][:, 0:n], in0=t2[q][:, 0:n],
                                                               in1=nmr[:, 0:n], op=ALU.add),
                              rd=[b_nmr], wr=[b_t2[q]])
                        kb.op("act", lambda e: e.activation(out=cs[:, j, 0:n], in_=t2[q][:, 0:n], func=AF.Silu,
                                                            scale=sm[:, S_LNG + j:S_LNG + j + 1],
                                                            bias=sm[:, S_LNB + j:S_LNB + j + 1]),
                              rd=[b_t2[q], b_sm], wr=[b_cs])
                    itc[0] = it

                def b_MM(ti):
                    t0, n = TBS[ti]
                    it = itc[0]
                    p = ti % 2
                    cs, b_cs = cs2[p], b_cs2[p]
                    for dc in range(8):
                        pbk, pbb = bank()
                        pgk, pgb = bank()
                        kb.op("pe", lambda e: [e.matmul(pbk[:, 0:n], wco[:, kc, dc * 128:(dc + 1) * 128], cs[:, kc, 0:n],
                                                        start=(kc == 0), stop=(kc == 7)) for kc in range(8)],
                              rd=[b_wco, b_cs], wr=[pbb])
                        kb.op("pe", lambda e: [e.matmul(pgk[:, 0:n], wg1[:, kc, dc * 128:(dc + 1) * 128],
                                                        uT[:, kc, t0:t0 + n], start=(kc == 0), stop=(kc == 7))
                                               for kc in range(8)],
                              rd=[b_wg1] + uT_bufs(t0, n), wr=[pgb])
                        q = it % 2
                        it += 1
                        kb.op("act", lambda e: e.activation(out=sg[q][:, 0:n], in_=pgk[:, 0:n], func=AF.Sigmoid),
                              rd=[pgb], wr=[b_sg[q]])
                        kb.op("dve", lambda e: e.tensor_tensor(out=m1[p][:, dc, 0:n], in0=pbk[:, 0:n],
                                                               in1=sg[q][:, 0:n], op=ALU.mult),
                              rd=[pbb, b_sg[q]], wr=[b_m1[p]])
                        pg2, pg2b = bank()
                        kb.op("pe", lambda e: [e.matmul(pg2[:, 0:n], wg2[:, kc, dc * 128:(dc + 1) * 128],
                                                        uT[:, kc, t0:t0 + n], start=(kc == 0), stop=(kc == 7))
                                               for kc in range(8)],
                              rd=[b_wg2] + uT_bufs(t0, n), wr=[pg2b])
                        kb.op("act", lambda e: e.activation(out=g2[p][:, dc, 0:n], in_=pg2[:, 0:n], func=AF.Sigmoid),
                              rd=[pg2b], wr=[b_g2[p]])
                    kb.dma("sp", lambda qq: qq.dma_start(out=m1_d[:, :, t0:t0 + n], in_=m1[p][:, :, 0:n]),
                           rd=[b_m1[p]], wr=[b_m1d[ti]])
                    kb.dma("sp", lambda qq: qq.dma_start(out=g2_d[:, :, t0:t0 + n], in_=g2[p][:, :, 0:n]),
                           rd=[b_g2[p]], wr=[b_g2d[ti]])
                    itc[0] = it

                b_LN(0)
                for ti in range(len(TBS)):
                    if ti + 1 < len(TBS):
                        b_LN(ti + 1)
                    b_MM(ti)

            kb.barrier()
            with ExitStack() as sc:
                kb.enabled = "C" in run
                wz = sb("wz", [128, 8, 2048], BF16, sc)
                wdt = sb("wdt", [128, 8, 64], BF16, sc)
                b_wz, b_wdt = Buf(), Buf()
                for qq_ in range(4):
                    kb.dma("pool", lambda q: q.dma_start(out=wz[:, :, qq_ * 512:(qq_ + 1) * 512],
                                                         in_=win_v[:, :, 2048 + qq_ * 512:2048 + (qq_ + 1) * 512]),
                           wr=[b_wz])
                kb.dma("pool", lambda q: q.dma_start(out=wdt[:], in_=win_v[:, :, 7168:7232]), wr=[b_wdt])
                dtb = sb("dtb", [128, 64], F32, sc)
                b_dtb = Buf()
                kb.dma("sp", lambda q: q.dma_start(out=dtb[:], in_=bc_d[:, B_DTB:B_DTB + 64]), wr=[b_dtb])
                szt = [sb("szt%d" % i, [128, 2048], BF16, sc) for i in range(2)]
                b_szt = [Buf(), Buf()]
                b_szd = [Buf() for _ in range(NT)]
                dte = sb("dte", [128, 64], F32, sc)
                b_dte = Buf()
                for i in range(NT):
                    p = i % 2
                    for qz in range(4):
                        pz, pzb = bank()
                        kb.op("pe", lambda e: [e.matmul(pz[:, :], uT[:, kc, i * 128:(i + 1) * 128],
                                                        wz[:, kc, qz * 512:(qz + 1) * 512],
                                                        start=(kc == 0), stop=(kc == 7)) for kc in range(8)],
                              rd=[b_wz, b_uT[i]], wr=[pzb])
                        kb.op("act", lambda e: e.activation(out=szt[p][:, qz * 512:(qz + 1) * 512], in_=pz[:, :],
                                                            func=AF.Silu), rd=[pzb], wr=[b_szt[p]])
                    kb.dma("sp", lambda q: q.dma_start(out=sz_d[i], in_=szt[p][:]), rd=[b_szt[p]], wr=[b_szd[i]])
                    pd, pdb = bank()
                    kb.op("pe", lambda e: [e.matmul(pd[:, 0:64], uT[:, kc, i * 128:(i + 1) * 128], wdt[:, kc, :],
                                                    start=(kc == 0), stop=(kc == 7)) for kc in range(8)],
                          rd=[b_wdt, b_uT[i]], wr=[pdb])
                    kb.op("dve", lambda e: e.tensor_tensor(out=dte[:], in0=pd[:, 0:64], in1=dtb[:], op=ALU.add),
                          rd=[pdb, b_dtb], wr=[b_dte])
                    kb.op("act", lambda e: e.activation(out=dte[:], in_=dte[:], func=AF.Exp), rd=[b_dte], wr=[b_dte])
                    kb.op("act", lambda e: e.activation(out=dtall[:, i, :], in_=dte[:], func=AF.Ln, bias=1.0),
                          rd=[b_dte], wr=[b_dt[i]])
                    if i == 0:
                        kb.op("dve", lambda e: e.memset(dtall[0:96, 0, :], 0.0), wr=[b_dt[0]])
                        kb.op("dve", lambda e: e.memset(dtall[96:112, 0, :], 0.0), wr=[b_dt[0]])
                if debug:
                    kb.dma("sp", lambda q: q.dma_start(out=dt_dbg, in_=dtall[:]), rd=b_dt, wr=[Buf()])
                wX = [sb("wX%d" % i, [128, 8, 128], BF16, sc) for i in range(2)]
                b_wX = [Buf(), Buf()]
                xT = [sb("xT%d" % i, [128, T + 6], BF16, sc) for i in range(2)]
                b_xT = [Buf(), Buf()]
                dg7 = [sb("dg7_%d" % i, [128, 7, 128], BF16, sc) for i in range(2)]
                b_dg7 = [Buf(), Buf()]
                xc = [sb("xc%d" % i, [128, 512], BF16, sc) for i in range(2)]
                b_xc = [Buf(), Buf()]
                b_xbcd = [[Buf() for _ in TBS] for _ in range(24)]
                for i in range(2):
                    kb.op("pool", lambda e: e.memset(xT[i][:], 0.0), wr=[b_xT[i]])
                it = 0
                for j in range(24):
                    p = j % 2
                    kb.dma("pool", lambda q: q.dma_start(out=wX[p][:], in_=win_v[:, :, 4096 + j * 128:4096 + (j + 1) * 128]),
                           wr=[b_wX[p]])
                    for k in range(7):
                        kb.op("dve", lambda e: e.tensor_scalar(out=dg7[p][:, k, :], in0=identb,
                                                               scalar1=sm[:, S_WSSM + j * 7 + k:S_WSSM + j * 7 + k + 1],
                                                               scalar2=None, op0=ALU.mult),
                              rd=[b_cbf, b_sm], wr=[b_dg7[p]])
                    for (t0, n) in TBS:
                        px, pxb = bank()
                        kb.op("pe", lambda e: [e.matmul(px[:, 0:n], wX[p][:, kc, :], uT[:, kc, t0:t0 + n],
                                                        start=(kc == 0), stop=(kc == 7)) for kc in range(8)],
                              rd=[b_wX[p]] + uT_bufs(t0, n), wr=[pxb])
                        kb.op("act", lambda e: e.activation(out=xT[p][:, 3 + t0:3 + t0 + n], in_=px[:, 0:n], func=AF.Copy),
                              rd=[pxb], wr=[b_xT[p]])
                    for ti, (t0, n) in enumerate(TBS):
                        pc, pcb = bank()
                        kb.op("pe", lambda e: [e.matmul(pc[:, 0:n], dg7[p][:, k, :], xT[p][:, t0 + k:t0 + k + n],
                                                        start=(k == 0), stop=(k == 6)) for k in range(7)],
                              rd=[b_dg7[p], b_xT[p]], wr=[pcb])
                        q = it % 2
                        it += 1
                        kb.op("act", lambda e: e.activation(out=xc[q][:, 0:n], in_=pc[:, 0:n], func=AF.Silu,
                                                            bias=sm[:, S_BSSM + j:S_BSSM + j + 1]),
                              rd=[pcb, b_sm], wr=[b_xc[q]])
                        if ti == 0:
                            kb.op("dve", lambda e: e.memset(xc[q][:, 0:112], 0.0), wr=[b_xc[q]])
                        kb.dma("sp", lambda qq: qq.dma_start(out=xbc_d[:, j, t0:t0 + n], in_=xc[q][:, 0:n]),
                               rd=[b_xc[q]], wr=[b_xbcd[j][ti]])

        kb.barrier()
        hnrow = Buf()
        hacc = Buf()
        b_ynd = [Buf() for _ in range(NT)]
        with ExitStack() as sd:
            kb.enabled = ("D" in run) or ("E" in run)
            abc = sb("abc", [128, 160], F32, sd)
            b_abc = Buf()
            kb.dma("sp", lambda q: q.dma_start(out=abc[:], in_=bc_d[:, B_DTB:B_DTB + 160]), wr=[b_abc])
            Aneg = sb("Aneg", [128, 64], F32, sd)
            b_A = Buf()
            kb.op("act", lambda e: e.activation(out=Aneg[:], in_=abc[:, 64:128], func=AF.Exp), rd=[b_abc], wr=[b_A])
            kb.op("dve", lambda e: e.tensor_scalar(out=Aneg[:], in0=Aneg[:], scalar1=-1.0, scalar2=None, op0=ALU.mult),
                  rd=[b_A], wr=[b_A])

            def dbl(name, shape, dtype):
                return [sb("%s_%d" % (name, i), shape, dtype, sd) for i in range(2)], [Buf(), Buf()]

            XT, b_XT = dbl("XT", [128, 24, 128], BF16)
            xs_tm2, b_xs2 = dbl("xs_tm", [128, 2048], BF16)
            b_tm2, b_btm2 = dbl("b_tm", [128, 512], BF16)
            cbm2, b_cbm2 = dbl("cbm", [128, 4, 128], BF16)
            a322, b_a322 = dbl("a32", [128, 32], F32)
            rhsA2, b_rhsA2 = dbl("rhsA", [128, 32, 128], F32)
            E2_, b_E2 = dbl("E", [128, 32, 128], BF16)
            dec2, b_dec2 = dbl("dec", [128, 96], F32)
            w22, b_w22 = dbl("w2", [128, 32], F32)
            xd2, b_xd2 = dbl("xd", [128, 32, 64], BF16)
            xdd2, b_xdd2 = dbl("xdd", [128, 32, 64], BF16)
            yacc2, b_yacc2 = dbl("yacc", [128, 2048], F32)
            S32 = sb("S32", [128, 2048], F32, sd)
            Sbf = sb("Sbf", [128, 2048], BF16, sd)
            b_S32, b_Sbf = Buf(), Buf()
            tmp = [sb("tmpo%d" % i, [128, 512], F32, sd) for i in range(2)]
            b_tmp = [Buf(), Buf()]
            b_ybd = [Buf() for _ in range(NT)]
            nchunk = [0]

            class Ctx:
                pass

            SEGR = os.environ.get("SEGR", "1") == "1"
            LR = sb("LR", [128, 2, 128], F32R, sd)
            b_LR = Buf()
            if SEGR:
                kb.op("dve", lambda e: e.tensor_copy(out=LR[:, 0, :], in_=cf[:, C_LF:C_LF + 128]), rd=[b_cf], wr=[b_LR])
                kb.op("dve", lambda e: e.tensor_copy(out=LR[:, 1, :], in_=cf[:, C_LB:C_LB + 128]), rd=[b_cf], wr=[b_LR])
            S_ENG = os.environ.get("S_ENG", "pool")
            GT_POOL = int(os.environ.get("GT_POOL", 0))

            def front1(c, d, preload=None):
                k = Ctx()
                p = nchunk[0] % 2
                nchunk[0] += 1
                k.p, k.c, k.d = p, c, d
                k.xs_tm, k.b_xs = xs_tm2[p], b_xs2[p]
                k.b_tm, k.b_btm = b_tm2[p], b_btm2[p]
                k.cbm, k.b_cbm = cbm2[p], b_cbm2[p]
                k.a32, k.b_a32 = a322[p], b_a322[p]
                k.rhsA, k.b_rhsA = rhsA2[p], b_rhsA2[p]
                k.E, k.b_E = E2_[p], b_E2[p]
                k.dec, k.b_dec = dec2[p], b_dec2[p]
                k.w2, k.b_w2 = w22[p], b_w22[p]
                k.xd, k.b_xd = xd2[p], b_xd2[p]
                k.xdd, k.b_xdd = xdd2[p], b_xdd2[p]
                k.yacc, k.b_yacc = yacc2[p], b_yacc2[p]
                k.xs3 = k.xs_tm[:].rearrange("p (h d) -> p h d", d=64)
                k.add_prev = preload is not None
                xs_tm, b_xs, b_tm, b_btm, cbm, b_cbm = k.xs_tm, k.b_xs, k.b_tm, k.b_btm, k.cbm, k.b_cbm
                a32, b_a32, rhsA, b_rhsA, E, b_E, dec, b_dec = k.a32, k.b_a32, k.rhsA, k.b_rhsA, k.E, k.b_E, k.dec, k.b_dec
                w2, b_w2, xd, b_xd, xdd, b_xdd, xs3 = k.w2, k.b_w2, k.xd, k.b_xd, k.xdd, k.b_xdd, k.xs3
                k.ybl = None
                if k.add_prev:
                    preload(k)
                rd_x = [b_xbcd[j][min(c // 4, 8)] for j in range(24)]
                kb.dma("sp", lambda q: q.dma_start(out=XT[p][:], in_=xbc_d[:, :, c * 128:(c + 1) * 128]),
                       rd=rd_x, wr=[b_XT[p]])
                X = XT[p]
                bX = b_XT[p]
                k.X, k.bX = X, bX
                U = cf[:, (C_UF if d == 0 else C_UB):(C_UF if d == 0 else C_UB) + 128]
                L = cf[:, (C_LF if d == 0 else C_LB):(C_LF if d == 0 else C_LB) + 128]
                onesf = cf[:, C_ONES:C_ONES + 128]
                Lr = LR[:, d, :] if SEGR else L
                dtc = dtall[:, c, d * 32:(d + 1) * 32]
                kb.op("dve", lambda e: e.tensor_tensor(out=a32[:], in0=dtc, in1=Aneg[:, d * 32:(d + 1) * 32],
                                                       op=ALU.mult), rd=[b_dt[c], b_A], wr=[b_a32])
                kb.op("pool", lambda e: e.tensor_tensor(out=(rhsA[:].bitcast(F32R) if SEGR else rhsA[:]), in0=a32[:].unsqueeze(2).broadcast_to([128, 32, 128]),
                                                        in1=U.unsqueeze(1).broadcast_to([128, 32, 128]), op=ALU.mult),
                      rd=[b_a32, b_cf], wr=[b_rhsA])
                psm, psmb = bank()
                kb.op("pe", lambda e: [e.matmul(psm[:, 0:32], U, a32[:], start=True, stop=True),
                                       e.matmul(psm[:, 32:64], L, a32[:], start=True, stop=True),
                                       e.matmul(psm[:, 64:96], onesf, a32[:], start=True, stop=True)],
                      rd=[b_cf, b_a32], wr=[psmb])
                kb.op("act", lambda e: e.activation(out=dec[:], in_=psm[:, 0:96], func=AF.Exp), rd=[psmb], wr=[b_dec])
                kb.op("dve", lambda e: e.tensor_tensor(out=w2[:], in0=dtc, in1=dec[:, 32:64], op=ALU.mult),
                      rd=[b_dt[c], b_dec], wr=[b_w2])
                for half in range(2):
                    pt, pb = bank()
                    ptb = pt[:].bitcast(BF16)
                    kb.op("pe", lambda e: [e.transpose(ptb[:, kk * 128:(kk + 1) * 128], X[:, half * 8 + kk, :], identb)
                                           for kk in range(8)], rd=[bX, b_cbf], wr=[pb])
                    kb.op("act", lambda e: e.activation(out=xs_tm[:, half * 1024:(half + 1) * 1024], in_=ptb,
                                                        func=AF.Copy), rd=[pb], wr=[b_xs])
                pt, pb = bank()
                ptb = pt[:].bitcast(BF16)
                kb.op("pe", lambda e: [e.transpose(ptb[:, kk * 128:(kk + 1) * 128], X[:, 16 + kk, :], identb)
                                       for kk in range(4)], rd=[bX, b_cbf], wr=[pb])
                kb.op("act", lambda e: e.activation(out=b_tm[:], in_=ptb[:, 0:512], func=AF.Copy), rd=[pb], wr=[b_btm])
                pcb_, pcbb = bank()
                kb.op("pe", lambda e: [e.matmul(pcb_[:, g * 128:(g + 1) * 128], X[:, 16 + g, :], X[:, 20 + g, :],
                                                start=True, stop=True) for g in range(4)], rd=[bX], wr=[pcbb])
                kb.op("dve", lambda e: e.tensor_tensor(out=cbm[:], in0=pcb_[:].rearrange("p (g l) -> p g l", g=4),
                                                       in1=U.unsqueeze(1).broadcast_to([128, 4, 128]), op=ALU.mult),
                      rd=[pcbb, b_cf], wr=[b_cbm])
                kb.op("pool", lambda e: e.tensor_tensor(out=xd[:], in0=xs3, in1=dtc.unsqueeze(2).broadcast_to([128, 32, 64]),
                                                        op=ALU.mult), rd=[b_xs, b_dt[c]], wr=[b_xd])
                kb.op("pool", lambda e: e.tensor_tensor(out=xdd[:], in0=xs3,
                                                        in1=w2[:].unsqueeze(2).broadcast_to([128, 32, 64]), op=ALU.mult),
                      rd=[b_xs, b_w2], wr=[b_xdd])
                for q8 in range(8):
                    pse, pseb = bank()
                    kb.op("pe", lambda e: e.matmul(pse[:, :], Lr, (rhsA[:, q8 * 4:(q8 + 1) * 4, :].bitcast(F32R) if SEGR else rhsA[:, q8 * 4:(q8 + 1) * 4, :]),
                                                   start=True, stop=True),
                          rd=[b_cf, b_rhsA, b_LR], wr=[pseb])
                    kb.op("act", lambda e: e.activation(out=E[:, q8 * 4:(q8 + 1) * 4, :],
                                                        in_=pse[:].rearrange("p (h l) -> p h l", h=4), func=AF.Exp),
                          rd=[pseb], wr=[b_E])
                return k

            def front2(k):
                for g in range(4):
                    kb.op("pool" if g < GT_POOL else "dve", lambda e: e.tensor_tensor(out=k.E[:, g * 8:(g + 1) * 8, :], in0=k.E[:, g * 8:(g + 1) * 8, :],
                                                           in1=k.cbm[:, g:g + 1, :].broadcast_to([128, 8, 128]),
                                                           op=ALU.mult), rd=[k.b_cbm], wr=[k.b_E])

            def back(k):
                GT, b_GT, xd, b_xd, xdd, b_xdd = k.E, k.b_E, k.xd, k.b_xd, k.xdd, k.b_xdd
                X, bX, b_tm, b_btm, dec, b_dec = k.X, k.bX, k.b_tm, k.b_btm, k.dec, k.b_dec
                yacc, b_yacc = k.yacc, k.b_yacc
                for g in range(4):
                    py, pyb = bank()
                    po, pob = bank()
                    pst, pstb = bank()
                    if k.add_prev:
                        kb.op("pe", lambda e: [e.matmul(py[:, :], identb, k.ybl[:, g * 512:(g + 1) * 512], start=True, stop=False)]
                              + [e.matmul(py[:, hh * 64:(hh + 1) * 64], DI[:, g * 8 + hh, :],
                                          k.xs_tm[:, (g * 8 + hh) * 64:(g * 8 + hh + 1) * 64], start=False, stop=False)
                                 for hh in range(8)]
                              + [e.matmul(py[:, hh * 64:(hh + 1) * 64], GT[:, g * 8 + hh, :], xd[:, g * 8 + hh, :],
                                          start=False, stop=(hh == 7)) for hh in range(8)],
                              rd=[b_GT, b_xd, k.b_ybl, k.b_xs, b_DI, b_cbf], wr=[pyb])
                    else:
                        kb.op("pe", lambda e: [e.matmul(py[:, hh * 64:(hh + 1) * 64], GT[:, g * 8 + hh, :], xd[:, g * 8 + hh, :],
                                                        start=True, stop=True) for hh in range(8)],
                              rd=[b_GT, b_xd], wr=[pyb])
                    kb.op("pe", lambda e: e.matmul(po[:, :], X[:, 20 + g, :], Sbf[:, g * 512:(g + 1) * 512],
                                                   start=True, stop=True), rd=[bX, b_Sbf], wr=[pob])
                    kb.op("pe", lambda e: e.matmul(pst[:, :], b_tm[:, g * 128:(g + 1) * 128],
                                                   xdd[:, g * 8:(g + 1) * 8, :], start=True, stop=True),
                          rd=[b_btm, b_xdd], wr=[pstb])
                    q = g % 2
                    kb.op("dve", lambda e: e.tensor_tensor(
                        out=tmp[q][:].rearrange("p (h d) -> p h d", d=64),
                        in0=po[:].rearrange("p (h d) -> p h d", d=64),
                        in1=dec[:, g * 8:(g + 1) * 8].unsqueeze(2).broadcast_to([128, 8, 64]), op=ALU.mult),
                        rd=[pob, b_dec], wr=[b_tmp[q]])
                    kb.op("dve", lambda e: e.tensor_tensor(out=yacc[:, g * 512:(g + 1) * 512], in0=py[:, :], in1=tmp[q][:],
                                                           op=ALU.add), rd=[pyb, b_tmp[q]], wr=[b_yacc])
                    kb.op(S_ENG, lambda e: e.tensor_tensor(
                        out=S32[:, g * 512:(g + 1) * 512].rearrange("p (h d) -> p h d", d=64),
                        in0=S32[:, g * 512:(g + 1) * 512].rearrange("p (h d) -> p h d", d=64),
                        in1=dec[:, 64 + g * 8:64 + (g + 1) * 8].unsqueeze(2).broadcast_to([128, 8, 64]), op=ALU.mult),
                        rd=[b_dec], wr=[b_S32])
                    kb.op("dve", lambda e: e.tensor_tensor(out=S32[:, g * 512:(g + 1) * 512], in0=pst[:, :],
                                                           in1=S32[:, g * 512:(g + 1) * 512], op=ALU.add),
                          rd=[pstb], wr=[b_S32])
                    kb.op("act", lambda e: e.activation(out=Sbf[:, g * 512:(g + 1) * 512], in_=S32[:, g * 512:(g + 1) * 512],
                                                        func=AF.Copy), rd=[b_S32], wr=[b_Sbf])

            def run_pass(order, d, preload_fn, post_fn):
                k = front1(order[0], d, preload_fn(order[0]) if preload_fn else None)
                front2(k)
                for i, c in enumerate(order):
                    kn = None
                    if i + 1 < len(order):
                        cn = order[i + 1]
                        kn = front1(cn, d, preload_fn(cn) if preload_fn else None)
                    back(k)
                    post_fn(k)
                    if kn is not None:
                        front2(kn)
                    k = kn

            kb.enabled = "D" in run
            kb.op("dve", lambda e: e.memset(S32[:], 0.0), wr=[b_S32])
            kb.op("dve", lambda e: e.memset(Sbf[:], 0.0), wr=[b_Sbf])
            run_pass(list(range(NT - 1, -1, -1)), 1, None,
                     lambda k: kb.dma("pool", lambda q: q.dma_start(out=yb_d[k.c], in_=k.yacc[:]), rd=[k.b_yacc], wr=[b_ybd[k.c]]))

            kb.enabled = "E" in run
            kb.op("dve", lambda e: e.memset(S32[:], 0.0), wr=[b_S32])
            kb.op("dve", lambda e: e.memset(Sbf[:], 0.0), wr=[b_Sbf])
            wns = sb("wns", [128, 2048], F32, sd)
            b_wns = Buf()
            kb.dma("sp", lambda q: q.dma_start(out=wns[:], in_=bc_d[:, B_WSSM:B_WSSM + 2048]), wr=[b_wns])
            szl2, b_szl2 = dbl("szl", [128, 2048], BF16)
            yn2, b_yn2 = dbl("yn", [128, 2048], BF16)
            ybl2, b_ybl2 = dbl("ybl", [128, 2048], BF16)
            DI = sb("DI", [128, 32, 128], BF16, sd)
            b_DI = Buf()
            for hh_ in range(32):
                kb.op("dve", lambda e: e.tensor_scalar(out=DI[:, hh_, :], in0=identb, scalar1=abc[:, 128 + hh_:129 + hh_],
                                                       scalar2=None, op0=ALU.mult), rd=[b_cbf, b_abc], wr=[b_DI])
            ssf2, b_ssf2 = dbl("ssf", [128, 1], F32)
            rsf2, b_rsf2 = dbl("rsf", [128, 1], F32)

            def fwd_preload(c):
                def f(k):
                    k.ybl, k.b_ybl = ybl2[k.p], b_ybl2[k.p]
                    kb.dma("sp", lambda q: q.dma_start(out=k.ybl[:], in_=yb_d[c]), rd=[b_ybd[c]], wr=[k.b_ybl])
                return f

            def fwd_post(k):
                p, c = k.p, k.c
                yacc, b_yacc = k.yacc, k.b_yacc
                kb.dma("sp", lambda q: q.dma_start(out=szl2[p][:], in_=sz_d[c]), rd=[b_szd[c]], wr=[b_szl2[p]])
                kb.op("pool", lambda e: e.tensor_tensor(out=yacc[:], in0=yacc[:], in1=szl2[p][:], op=ALU.mult),
                      rd=[b_szl2[p]], wr=[b_yacc])
                rms_rstd(yacc[:], b_yacc, yn2[p][:], b_yn2[p], ssf2[p], b_ssf2[p], rsf2[p], b_rsf2[p], 2048)
                kb.op("dve", lambda e: e.scalar_tensor_tensor(out=yn2[p][:], in0=yacc[:], scalar=rsf2[p][:, 0:1], in1=wns[:],
                                                              op0=ALU.mult, op1=ALU.mult),
                      rd=[b_yacc, b_rsf2[p], b_wns], wr=[b_yn2[p]])
                kb.dma("sp", lambda q: q.dma_start(out=yn_d[c], in_=yn2[p][:]), rd=[b_yn2[p]], wr=[b_ynd[c]])

            run_pass(list(range(NT)), 0, fwd_preload, fwd_post)

        kb.barrier()
        affTM = sb("affTM", [128, NT, NE], F32)
        b_affTM = Buf()
        affT = sb("affT", [NE, T], F32)
        b_affT = Buf()
        with ExitStack() as se:
            kb.enabled = "E" in run
            wso = sb("wso", [128, 16, D], BF16, se)
            wout = sb("wout", [128, 8, D], BF16, se)
            wr_ = sb("wr_", [128, 8, NE], BF16, se)
            b_wso, b_wout, b_wr = Buf(), Buf(), Buf()
            kb.dma("pool", lambda q: q.dma_start(out=wso[:], in_=w_so.rearrange("(kc p) n -> p kc n", p=128)), wr=[b_wso])
            kb.dma("pool", lambda q: q.dma_start(out=wout[:], in_=w_o.rearrange("(kc p) n -> p kc n", p=128)), wr=[b_wout])
            kb.dma("pool", lambda q: q.dma_start(out=wr_[:], in_=w_r.rearrange("(kc p) n -> p kc n", p=128)), wr=[b_wr])
            wnf = sb("wnf", [128, D], F32, se)
            b_wnf = Buf()
            kb.dma("sp", lambda q: q.dma_start(out=wnf[:], in_=bc_d[:, B_WFFN:B_WFFN + D]), wr=[b_wnf])

            def nbuf(name, shape, dtype, n):
                return [sb("%s_%d" % (name, i), shape, dtype, se) for i in range(n)], [Buf() for _ in range(n)]

            ynl1, b_ynl1 = nbuf("ynl", [128, 4, 2048], BF16, 1)
            ynT2, b_ynT2 = nbuf("ynT", [128, 16, 512], BF16, 1)
            ynT2, b_ynT2 = ynT2 * 2, b_ynT2 * 2
            m1l1, b_m1l1 = nbuf("m1l", [128, 8, 512], BF16, 1)
            g2l1, b_g2l1 = nbuf("g2l", [128, 8, 512], BF16, 1)
            mT2, b_mT2 = nbuf("mT", [128, 8, 512], BF16, 2)
            xinF4, b_xinF4 = nbuf("xinF", [128, D], F32, 4)
            hT4, b_hT4 = nbuf("h_t", [128, D], F32, 4)
            hnb4, b_hnb4 = nbuf("hnb", [128, D], BF16, 4)
            hnT4, b_hnT4 = nbuf("hnT", [128, 8, 128], BF16, 4)
            junkE2, b_junkE2 = nbuf("junkE", [128, D], BF16, 2)
            ssE4, b_ssE4 = nbuf("ssE", [128, 1], F32, 4)
            rsE4, b_rsE4 = nbuf("rsE", [128, 1], F32, 4)
            lg4, b_lg4 = nbuf("lg", [128, NE], F32, 4)
            mx4, b_mx4 = nbuf("mx", [128, 1], F32, 4)
            sme4, b_sme4 = nbuf("sme", [128, 1], F32, 4)

            def stage_TS(ti):
                t0, n = TBS[ti]
                pb_ = ti % 2
                nt_ = n // 128
                c0 = t0 // 128
                ynl, b_ynl = ynl1[0], b_ynl1[0]
                ynT, b_ynT = ynT2[pb_], b_ynT2[pb_]
                m1l, b_m1l = m1l1[0], b_m1l1[0]
                g2l, b_g2l = g2l1[0], b_g2l1[0]
                mT, b_mT = mT2[pb_], b_mT2[pb_]
                kb.dma("sp", lambda q: q.dma_start(out=ynl[:, 0:nt_, :], in_=yn_d[c0:c0 + nt_].rearrange("t p f -> p t f")),
                       rd=b_ynd[c0:c0 + nt_], wr=[b_ynl])
                kb.dma("sp", lambda q: q.dma_start(out=m1l[:, :, 0:n], in_=m1_d[:, :, t0:t0 + n]), rd=[b_m1d[ti]], wr=[b_m1l])
                kb.dma("sp", lambda q: q.dma_start(out=g2l[:, :, 0:n], in_=g2_d[:, :, t0:t0 + n]), rd=[b_g2d[ti]], wr=[b_g2l])
                for tt in range(nt_):
                    for half in range(2):
                        pt, pb = bank()
                        ptb = pt[:].bitcast(BF16)
                        kb.op("pe", lambda e: [e.transpose(ptb[:, k * 128:(k + 1) * 128],
                                                           ynl[:, tt, (half * 8 + k) * 128:(half * 8 + k + 1) * 128], identb)
                                               for k in range(8)], rd=[b_ynl, b_cbf], wr=[pb])
                        kb.op("act", lambda e: e.activation(out=ynT[:, half * 8:(half + 1) * 8, tt * 128:(tt + 1) * 128],
                                                            in_=ptb.rearrange("p (k t) -> p k t", k=8), func=AF.Copy),
                              rd=[pb], wr=[b_ynT])
                for dc in range(8):
                    pbs, pbsb = bank()
                    kb.op("pe", lambda e: [e.matmul(pbs[:, 0:n], wso[:, kc, dc * 128:(dc + 1) * 128], ynT[:, kc, 0:n],
                                                    start=(kc == 0), stop=(kc == 15)) for kc in range(16)],
                          rd=[b_wso, b_ynT], wr=[pbsb])
                    kb.op("dve", lambda e: e.tensor_tensor(out=mT[:, dc, 0:n], in0=pbs[:, 0:n], in1=g2l[:, dc, 0:n],
                                                           op=ALU.mult), rd=[pbsb, b_g2l], wr=[b_mT])
                    kb.op("pool", lambda e: e.tensor_tensor(out=mT[:, dc, 0:n], in0=mT[:, dc, 0:n], in1=m1l[:, dc, 0:n],
                                                            op=ALU.add), rd=[b_m1l], wr=[b_mT])

            def stage_W(ti):
                t0, n = TBS[ti]
                pb_ = ti % 2
                nt_ = n // 128
                c0 = t0 // 128
                mT, b_mT = mT2[pb_], b_mT2[pb_]
                for tt in range(nt_):
                    c = c0 + tt
                    load_x_tile(c, xinF4[tt], b_xinF4[tt])
                    for dh in range(2):
                        ph, phb = bank()
                        kb.op("pe", lambda e: [e.matmul(ph[:, :], mT[:, kc, tt * 128:(tt + 1) * 128],
                                                        wout[:, kc, dh * 512:(dh + 1) * 512],
                                                        start=(kc == 0), stop=(kc == 7)) for kc in range(8)],
                              rd=[b_wout, b_mT], wr=[phb])
                        kb.op("dve", lambda e: e.tensor_tensor(out=hT4[tt][:, dh * 512:(dh + 1) * 512], in0=ph[:, :],
                                                               in1=xinF4[tt][:, dh * 512:(dh + 1) * 512], op=ALU.add),
                              rd=[phb, b_xinF4[tt]], wr=[b_hT4[tt]])
                    kb.dma("sp", lambda q: q.dma_start(out=hacc_d[c * 128:(c + 1) * 128, :], in_=hT4[tt][:]),
                           rd=[b_hT4[tt]], wr=[hacc])
                for tt in range(nt_):
                    kb.op("act", lambda e: e.activation(out=junkE2[tt % 2][:], in_=hT4[tt][:], func=AF.Square,
                                                        accum_out=ssE4[tt][:]),
                          rd=[b_hT4[tt]], wr=[b_junkE2[tt % 2], b_ssE4[tt]])
                for tt in range(nt_):
                    kb.op("act", lambda e: e.activation(out=ssE4[tt][:], in_=ssE4[tt][:], func=AF.Sqrt, bias=EPS, scale=1.0 / D),
                          rd=[b_ssE4[tt]], wr=[b_ssE4[tt]])
                for tt in range(nt_):
                    kb.op("dve", lambda e: e.reciprocal(out=rsE4[tt][:], in_=ssE4[tt][:]), rd=[b_ssE4[tt]], wr=[b_rsE4[tt]])
                for tt in range(nt_):
                    c = c0 + tt
                    kb.op("dve", lambda e: e.scalar_tensor_tensor(out=hnb4[tt][:], in0=hT4[tt][:], scalar=rsE4[tt][:, 0:1],
                                                                  in1=wnf[:], op0=ALU.mult, op1=ALU.mult),
                          rd=[b_hT4[tt], b_rsE4[tt], b_wnf], wr=[b_hnb4[tt]])
                    kb.dma("sp", lambda q: q.dma_start(out=hn_d[c * 128:(c + 1) * 128, :], in_=hnb4[tt][:]),
                           rd=[b_hnb4[tt]], wr=[hnrow])

            def stage_R(ti):
                t0, n = TBS[ti]
                nt_ = n // 128
                c0 = t0 // 128
                prs = []
                for tt in range(nt_):
                    pt, pb = bank()
                    ptb = pt[:].bitcast(BF16)
                    kb.op("pe", lambda e: [e.transpose(ptb[:, k * 128:(k + 1) * 128], hnb4[tt][:, k * 128:(k + 1) * 128], identb)
                                           for k in range(8)], rd=[b_hnb4[tt], b_cbf], wr=[pb])
                    kb.op("act", lambda e: e.activation(out=hnT4[tt][:], in_=ptb.rearrange("p (k t) -> p k t", k=8), func=AF.Copy),
                          rd=[pb], wr=[b_hnT4[tt]])
                for tt in range(nt_):
                    pr, prb = bank()
                    prs.append((pr, prb))
                    kb.op("pe", lambda e: [e.matmul(pr[:, 0:NE], hnT4[tt][:, kc, :], wr_[:, kc, :], start=(kc == 0), stop=(kc == 7))
                                           for kc in range(8)], rd=[b_hnT4[tt], b_wr], wr=[prb])
                for tt in range(nt_):
                    pr, prb = prs[tt]
                    kb.op("dve", lambda e: e.reduce_max(out=mx4[tt][:], in_=pr[:, 0:NE], axis=AX.X), rd=[prb], wr=[b_mx4[tt]])
                for tt in range(nt_):
                    kb.op("dve", lambda e: e.tensor_scalar(out=mx4[tt][:], in0=mx4[tt][:], scalar1=-1.0, scalar2=None, op0=ALU.mult),
                          rd=[b_mx4[tt]], wr=[b_mx4[tt]])
                for tt in range(nt_):
                    pr, prb = prs[tt]
                    kb.op("act", lambda e: e.activation(out=lg4[tt][:], in_=pr[:, 0:NE], func=AF.Exp, bias=mx4[tt][:, 0:1],
                                                        accum_out=sme4[tt][:]), rd=[prb, b_mx4[tt]], wr=[b_lg4[tt], b_sme4[tt]])
                for tt in range(nt_):
                    kb.op("dve", lambda e: e.reciprocal(out=sme4[tt][:], in_=sme4[tt][:]), rd=[b_sme4[tt]], wr=[b_sme4[tt]])
                for tt in range(nt_):
                    c = c0 + tt
                    kb.op("dve", lambda e: e.tensor_scalar(out=affTM[:, c, :], in0=lg4[tt][:], scalar1=sme4[tt][:, 0:1], scalar2=None,
                                                           op0=ALU.mult), rd=[b_lg4[tt], b_sme4[tt]], wr=[b_affTM])
                for tt in range(nt_):
                    c = c0 + tt
                    pa_, pab_ = bank()
                    kb.op("pe", lambda e: e.transpose(pa_[0:NE, 0:128], affTM[:, c, :], identf), rd=[b_affTM, b_cf], wr=[pab_])
                    kb.op("act", lambda e: e.activation(out=affT[:, c * 128:(c + 1) * 128], in_=pa_[0:NE, 0:128], func=AF.Copy),
                          rd=[pab_], wr=[b_affT])

            stage_TS(0)
            for ti in range(len(TBS)):
                stage_W(ti)
                if ti + 1 < len(TBS):
                    stage_TS(ti + 1)
                stage_R(ti)

        kb.barrier()
        if debug:
            kb.enabled = "E" in run
            kb.dma("sp", lambda q: q.dma_start(out=aff_dbg, in_=affTM[:]), rd=[b_affTM], wr=[Buf()])
        with ExitStack() as sf:
            kb.enabled = "F" in run
            kb.op("dve", lambda e: e.memset(affT[:, 0:112], 0.0), wr=[b_affT])
            posmTM = sf.enter_context(nc.sbuf_tensor("posmTM", [128, NT, NE], F32))
            RT = sf.enter_context(nc.sbuf_tensor("RT", [128, NT, NE, 4], BF16))
            sr = ExitStack()
            lo = sr.enter_context(nc.sbuf_tensor("lo", [NE, 1], F32))
            hi = sr.enter_context(nc.sbuf_tensor("hi", [NE, 1], F32))
            mid = sr.enter_context(nc.sbuf_tensor("mid", [NE, 1], F32))
            cnt = sr.enter_context(nc.sbuf_tensor("cnt", [NE, 1], F32))
            ge = sr.enter_context(nc.sbuf_tensor("ge", [NE, 1], F32))
            dl = sr.enter_context(nc.sbuf_tensor("dl", [NE, 1], F32))
            jk = sr.enter_context(nc.sbuf_tensor("jk", [NE, T], BF16))
            maskT = sr.enter_context(nc.sbuf_tensor("maskT", [NE, T], F32))
            csum = sr.enter_context(nc.sbuf_tensor("csum", [NE, T], F32))
            onesT = sr.enter_context(nc.sbuf_tensor("onesT", [NE, T], F32))
            ahi = sr.enter_context(nc.sbuf_tensor("ahi", [128, NT, NE], BF16))
            alo = sr.enter_context(nc.sbuf_tensor("alo", [128, NT, NE], F32))
            b_r = Buf()
            b_posm = Buf()
            b_RT = Buf()
            kb.op("dve", lambda e: e.memset(lo[:], 0.0), wr=[b_r])
            kb.op("dve", lambda e: e.memset(hi[:], 1.0), wr=[b_r])
            kb.op("dve", lambda e: e.memset(onesT[:], 1.0), wr=[b_r])
            for itn in range(28):
                kb.op("dve", lambda e: e.tensor_tensor(out=mid[:], in0=lo[:], in1=hi[:], op=ALU.add), rd=[b_r], wr=[b_r])
                kb.op("dve", lambda e: e.tensor_scalar(out=mid[:], in0=mid[:], scalar1=0.5, scalar2=None, op0=ALU.mult),
                      rd=[b_r], wr=[b_r])
                kb.op("dve", lambda e: e.tensor_scalar(out=jk[:], in0=affT[:], scalar1=mid[:, 0:1], scalar2=None,
                                                       op0=ALU.is_gt, op1=ALU.add, accum_out=cnt[:]),
                      rd=[b_r, b_affT], wr=[b_r])
                kb.op("dve", lambda e: e.tensor_scalar(out=ge[:], in0=cnt[:], scalar1=float(CAP) - 0.5, scalar2=None,
                                                       op0=ALU.is_gt), rd=[b_r], wr=[b_r])
                kb.op("dve", lambda e: e.tensor_tensor(out=dl[:], in0=mid[:], in1=lo[:], op=ALU.subtract), rd=[b_r], wr=[b_r])
                kb.op("dve", lambda e: e.tensor_tensor(out=dl[:], in0=dl[:], in1=ge[:], op=ALU.mult), rd=[b_r], wr=[b_r])
                kb.op("dve", lambda e: e.tensor_tensor(out=lo[:], in0=lo[:], in1=dl[:], op=ALU.add), rd=[b_r], wr=[b_r])
                kb.op("dve", lambda e: e.tensor_tensor(out=dl[:], in0=hi[:], in1=mid[:], op=ALU.subtract), rd=[b_r], wr=[b_r])
                kb.op("dve", lambda e: e.tensor_tensor(out=dl[:], in0=dl[:], in1=ge[:], op=ALU.mult), rd=[b_r], wr=[b_r])
                kb.op("dve", lambda e: e.tensor_tensor(out=hi[:], in0=mid[:], in1=dl[:], op=ALU.add), rd=[b_r], wr=[b_r])
            kb.op("dve", lambda e: e.tensor_scalar(out=maskT[:], in0=affT[:], scalar1=lo[:, 0:1], scalar2=None,
                                                   op0=ALU.is_gt), rd=[b_r, b_affT], wr=[b_r])
            kb.op("dve", lambda e: e.tensor_tensor_scan(out=csum[:], data0=onesT[:], data1=maskT[:], initial=0.0,
                                                        op0=ALU.mult, op1=ALU.add), rd=[b_r], wr=[b_r])
            kb.op("dve", lambda e: e.tensor_tensor(out=csum[:], in0=csum[:], in1=maskT[:], op=ALU.mult), rd=[b_r], wr=[b_r])
            kb.op("dve", lambda e: e.tensor_scalar(out=csum[:], in0=csum[:], scalar1=-1.0, scalar2=None, op0=ALU.add),
                  rd=[b_r], wr=[b_r])
            for c in range(NT):
                pp, ppb = bank()
                kb.op("pe", lambda e: e.transpose(pp[:, 0:NE], csum[:, c * 128:(c + 1) * 128], identf[0:NE, 0:NE]),
                      rd=[b_r, b_cf], wr=[ppb])
                kb.op("act", lambda e: e.activation(out=posmTM[:, c, :], in_=pp[:, 0:NE], func=AF.Copy),
                      rd=[ppb], wr=[b_posm])
            kb.op("dve", lambda e: e.tensor_copy(out=ahi[:], in_=affTM[:]), rd=[b_affTM], wr=[b_RT])
            kb.op("dve", lambda e: e.tensor_tensor(out=alo[:], in0=affTM[:], in1=ahi[:], op=ALU.subtract),
                  rd=[b_affTM], wr=[b_RT])
            kb.op("dve", lambda e: e.tensor_copy(out=RT[:, :, :, 2], in_=ahi[:]), wr=[b_RT])
            kb.op("dve", lambda e: e.tensor_copy(out=RT[:, :, :, 3], in_=alo[:]), wr=[b_RT])
            tv = cbf[:, CB_TV:CB_TV + 66].rearrange("p (t two) -> p t two", two=2)
            kb.op("dve", lambda e: e.tensor_copy(out=RT[:, :, :, 0], in_=tv[:, :, 0:1].broadcast_to([128, NT, NE])),
                  rd=[b_cbf], wr=[b_RT])
            kb.op("dve", lambda e: e.tensor_copy(out=RT[:, :, :, 1], in_=tv[:, :, 1:2].broadcast_to([128, NT, NE])),
                  rd=[b_cbf], wr=[b_RT])

            kb.barrier()
            sr.close()

            if debug:
                kb.enabled = "F" in run
                kb.dma("sp", lambda q: q.dma_start(out=posm_dbg, in_=posmTM[:]), rd=[b_posm], wr=[Buf()])
            kb.enabled = "G" in run
            NSLOT = 8
            wring = [sf.enter_context(nc.sbuf_tensor("wring%d" % i, [128, 4096], BF16)) for i in range(NSLOT)]
            b_wring = [Buf() for _ in range(NSLOT)]
            units = []
            for e_ in range(NE):
                for q4 in range(4):
                    units.append(("g", e_, q4))
                    units.append(("u", e_, q4))
                for q4 in range(4):
                    units.append(("d", e_, q4))
            slot_of = {}

            def issue_unit(ui):
                kind, e_, q4 = units[ui]
                s_ = ui % NSLOT
                slot_of[(kind, e_, q4)] = s_
                if not kb.enabled or os.environ.get("GNOW") == "1":
                    return
                if kind in ("g", "u"):
                    src = (w_eg if kind == "g" else w_eu)[e_].rearrange("(kc p) n -> p kc n", p=128)[:, :, q4 * 512:(q4 + 1) * 512]
                    dst = wring[s_][:].rearrange("p (kc n) -> p kc n", kc=8)
                else:
                    src = w_ed[e_].rearrange("(fc p) n -> p fc n", p=128)[:, q4 * 4:(q4 + 1) * 4, :]
                    dst = wring[s_][:].rearrange("p (fc n) -> p fc n", fc=4)
                MAXOUT = int(os.environ.get("MAXOUT", 2))
                if len(unit_toks) >= MAXOUT:
                    kb.wait("pool", [unit_toks[-MAXOUT]])
                unit_toks.append(kb.dma("pool", lambda q: q.dma_start(out=dst, in_=src), wr=[b_wring[s_]]))

            unit_toks = []
            PREF = 6
            nissued = [0]

            def ensure(ui):
                while nissued[0] <= min(ui + PREF, len(units) - 1):
                    issue_unit(nissued[0])
                    nissued[0] += 1

            sel = [sf.enter_context(nc.sbuf_tensor("sel%d" % i, [128, 516], BF16)) for i in range(3)]
            b_sel = [Buf() for _ in range(3)]
            iq = sf.enter_context(nc.sbuf_tensor("iq", [4, 516], F32))
            b_iq = Buf()
            iqT = sf.enter_context(nc.sbuf_tensor("iqT", [128, 5, 4], F32))
            b_iqT = Buf()
            idxf = sf.enter_context(nc.sbuf_tensor("idxf", [128, 5], F32))
            idxi = [sf.enter_context(nc.sbuf_tensor("idxi%d" % i, [128, 5], I32)) for i in range(2)]
            afs = [sf.enter_context(nc.sbuf_tensor("afs%d" % i, [128, 5], F32)) for i in range(2)]
            b_idx = [Buf(), Buf()]
            xg2 = [sf.enter_context(nc.sbuf_tensor("xg%d" % i, [128, 5, D], BF16)) for i in range(2)]
            b_xg2 = [Buf(), Buf()]
            xgT2 = [sf.enter_context(nc.sbuf_tensor("xgT%d" % i, [128, 8, 640], BF16)) for i in range(2)]
            b_xgT2 = [Buf(), Buf()]
            hTe = sf.enter_context(nc.sbuf_tensor("hTe", [128, 16, 640], BF16))
            b_hTe = Buf()
            sgl = [sf.enter_context(nc.sbuf_tensor("sgl%d" % i, [128, 257], BF16)) for i in range(2)]
            b_sgl = [Buf(), Buf()]
            yw = [sf.enter_context(nc.sbuf_tensor("yw%d" % i, [128, D], F32)) for i in range(2)]
            b_yw = [Buf(), Buf()]
            hg = [sf.enter_context(nc.sbuf_tensor("hg%d" % i, [128, D], F32)) for i in range(2)]
            b_hg = [Buf(), Buf()]
            for i in range(2):
                kb.op("pool", lambda e: e.memset(xg2[i][:], 0.0), wr=[b_xg2[i]])
            iota = cf[:, C_IOTA:C_IOTA + 516]
            uic = [0]
            ityc = [0]

            def prepA(e_):
                ip = e_ % 2
                pi1, pi1b = bank()
                pi2, pi2b = bank()
                for c in range(NT):
                    s3 = c % 3
                    kb.op("dve", lambda e: e.tensor_scalar(out=sel[s3][:], in0=iota, scalar1=posmTM[:, c, e_:e_ + 1],
                                                           scalar2=None, op0=ALU.is_equal),
                          rd=[b_cf, b_posm], wr=[b_sel[s3]])
                    kb.op("pe", lambda e: [e.matmul(pi1[0:4, :], RT[:, c, e_, :], sel[s3][:, 0:512],
                                                    start=(c == 0), stop=(c == NT - 1)),
                                           e.matmul(pi2[0:4, 0:4], RT[:, c, e_, :], sel[s3][:, 512:516],
                                                    start=(c == 0), stop=(c == NT - 1))],
                          rd=[b_RT, b_sel[s3]], wr=[pi1b, pi2b])
                kb.op("act", lambda e: e.activation(out=iq[:, 0:512], in_=pi1[0:4, :], func=AF.Copy), rd=[pi1b], wr=[b_iq])
                kb.op("act", lambda e: e.activation(out=iq[:, 512:516], in_=pi2[0:4, 0:4], func=AF.Copy), rd=[pi2b], wr=[b_iq])
                pq, pqb = bank()
                kb.op("pe", lambda e: ([e.transpose(pq[:, jb * 4:(jb + 1) * 4], iq[:, jb * 128:(jb + 1) * 128],
                                                    identf[0:4, 0:4]) for jb in range(4)]
                                       + [e.transpose(pq[0:4, 16:20], iq[:, 512:516], identf[0:4, 0:4])]),
                      rd=[b_iq, b_cf], wr=[pqb])
                kb.op("dve", lambda e: e.memset(iqT[:], 0.0), wr=[b_iqT])
                kb.op("act", lambda e: e.activation(out=iqT[:, 0:4, :], in_=pq[:, 0:16].rearrange("p (j f) -> p j f", f=4),
                                                    func=AF.Copy), rd=[pqb], wr=[b_iqT])
                kb.op("act", lambda e: e.activation(out=iqT[0:4, 4, :], in_=pq[0:4, 16:20], func=AF.Copy),
                      rd=[pqb], wr=[b_iqT])
                kb.op("dve", lambda e: e.scalar_tensor_tensor(out=idxf[:], in0=iqT[:, :, 0], scalar=128.0, in1=iqT[:, :, 1],
                                                              op0=ALU.mult, op1=ALU.add), rd=[b_iqT], wr=[b_idx[ip]])
                kb.op("dve", lambda e: e.tensor_copy(out=idxi[ip][:], in_=idxf[:]), wr=[b_idx[ip]])
                kb.op("dve", lambda e: e.tensor_tensor(out=afs[ip][:], in0=iqT[:, :, 2], in1=iqT[:, :, 3], op=ALU.add),
                      rd=[b_iqT], wr=[b_idx[ip]])
                for jb in range(5):
                    M = 128 if jb < 4 else 2
                    kb.dma("pool", lambda q: q.indirect_dma_start(
                        out=xg2[ip][0:M, jb, :], out_offset=None, in_=hn_d[:, :],
                        in_offset=bass.IndirectOffsetOnAxis(ap=idxi[ip][0:M, jb:jb + 1], axis=0)),
                        rd=[b_idx[ip], hnrow], wr=[b_xg2[ip]])

            def prepB(e_):
                ip = e_ % 2
                for jb in range(5):
                    pt, pb = bank()
                    ptb = pt[:].bitcast(BF16)
                    kb.op("pe", lambda e: [e.transpose(ptb[:, kk * 128:(kk + 1) * 128], xg2[ip][:, jb, kk * 128:(kk + 1) * 128], identb)
                                           for kk in range(8)], rd=[b_xg2[ip], b_cbf], wr=[pb])
                    kb.op("act", lambda e: e.activation(out=xgT2[ip][:, :, jb * 128:(jb + 1) * 128],
                                                        in_=ptb.rearrange("p (k t) -> p k t", k=8), func=AF.Copy),
                          rd=[pb], wr=[b_xgT2[ip]])

            def gateup(e_, q4s):
                ip = e_ % 2
                xgT, b_xgT = xgT2[ip], b_xgT2[ip]
                for q4 in q4s:
                    ensure(uic[0])
                    sg_ = slot_of[("g", e_, q4)]
                    su_ = slot_of[("u", e_, q4)]
                    wgv = wring[sg_][:].rearrange("p (kc n) -> p kc n", kc=8)
                    wuv = wring[su_][:].rearrange("p (kc n) -> p kc n", kc=8)
                    for f4 in range(4):
                        fc = q4 * 4 + f4
                        for hf in range(2):
                            c0 = hf * 257
                            pgk, pgb = bank()
                            puk, pub = bank()
                            kb.op("pe", lambda e: [e.matmul(pgk[:, 0:257], wgv[:, kc, f4 * 128:(f4 + 1) * 128],
                                                            xgT[:, kc, c0:c0 + 257], start=(kc == 0), stop=(kc == 7))
                                                   for kc in range(8)], rd=[b_wring[sg_], b_xgT], wr=[pgb])
                            kb.op("pe", lambda e: [e.matmul(puk[:, 0:257], wuv[:, kc, f4 * 128:(f4 + 1) * 128],
                                                            xgT[:, kc, c0:c0 + 257], start=(kc == 0), stop=(kc == 7))
                                                   for kc in range(8)], rd=[b_wring[su_], b_xgT], wr=[pub])
                            qy = ityc[0] % 2
                            ityc[0] += 1
                            kb.op("act", lambda e: e.activation(out=sgl[qy][:], in_=pgk[:, 0:257], func=AF.Silu),
                                  rd=[pgb], wr=[b_sgl[qy]])
                            kb.op("dve", lambda e: e.tensor_tensor(out=hTe[:, fc, c0:c0 + 257], in0=puk[:, 0:257],
                                                                   in1=sgl[qy][:], op=ALU.mult),
                                  rd=[pub, b_sgl[qy]], wr=[b_hTe])
                    uic[0] += 2
                    ensure(uic[0])

            def down(e_):
                ip = e_ % 2
                sd_ = [slot_of[("d", e_, q4)] for q4 in range(4)]
                for jb in range(5):
                    M = 128 if jb < 4 else 2
                    qy = ityc[0] % 2
                    ityc[0] += 1
                    kb.dma("pool", lambda q: q.indirect_dma_start(
                        out=hg[qy][0:M, :], out_offset=None, in_=hacc_d[:, :],
                        in_offset=bass.IndirectOffsetOnAxis(ap=idxi[ip][0:M, jb:jb + 1], axis=0)),
                        rd=[b_idx[ip], hacc], wr=[b_hg[qy]])
                    for dh in range(2):
                        pdn, pdnb = bank()
                        kb.op("pe", lambda e: [e.matmul(pdn[0:M, :], hTe[:, fc, jb * 128:jb * 128 + M],
                                                        wring[sd_[fc // 4]][:].rearrange("p (f n) -> p f n", f=4)[:, fc % 4, dh * 512:(dh + 1) * 512],
                                                        start=(fc == 0), stop=(fc == 15)) for fc in range(16)],
                              rd=[b_wring[s_] for s_ in sd_] + [b_hTe], wr=[pdnb])
                        kb.op("dve", lambda e: e.scalar_tensor_tensor(
                            out=yw[qy][0:M, dh * 512:(dh + 1) * 512], in0=pdn[0:M, :], scalar=afs[ip][0:M, jb:jb + 1],
                            in1=hg[qy][0:M, dh * 512:(dh + 1) * 512], op0=ALU.mult, op1=ALU.add),
                            rd=[pdnb, b_idx[ip], b_hg[qy]], wr=[b_yw[qy]])
                    kb.dma("pool", lambda q: q.indirect_dma_start(
                        out=hacc_d[:, :], out_offset=bass.IndirectOffsetOnAxis(ap=idxi[ip][0:M, jb:jb + 1], axis=0),
                        in_=yw[qy][0:M, :], in_offset=None),
                        rd=[b_yw[qy], b_idx[ip]], wr=[hacc])
                uic[0] += 4

            ensure(0)
            prepA(0)
            prepB(0)
            for e_ in range(NE):
                gateup(e_, [0, 1])
                if e_ + 1 < NE:
                    prepA(e_ + 1)
                gateup(e_, [2, 3])
                if e_ + 1 < NE:
                    prepB(e_ + 1)
                down(e_)

        kb.barrier()
        with ExitStack() as sh:
            kb.enabled = "H" in run
            wfin = sb("wfin", [128, D], F32, sh)
            b_wfin = Buf()
            kb.dma("sp", lambda q: q.dma_start(out=wfin[:], in_=bc_d[:, B_WFIN:B_WFIN + D]), wr=[b_wfin])
            hl = [sb("hl%d" % i, [128, D], F32, sh) for i in range(4)]
            b_hl = [Buf() for _ in range(4)]
            ol = [sb("ol%d" % i, [128, D], F32, sh) for i in range(4)]
            b_ol = [Buf() for _ in range(4)]
            jf = [sb("jf%d" % i, [128, D], BF16, sh) for i in range(2)]
            b_jf = [Buf(), Buf()]
            s1_ = [sb("s1_%d" % i, [128, 1], F32, sh) for i in range(4)]
            r1_ = [sb("r1_%d" % i, [128, 1], F32, sh) for i in range(4)]
            b_s1 = [Buf() for _ in range(4)]
            b_r1 = [Buf() for _ in range(4)]
            outb = Buf()
            for c0 in range(1, NT, 4):
                cs_ = list(range(c0, min(c0 + 4, NT)))
                for i, c in enumerate(cs_):
                    kb.dma("sp", lambda q: q.dma_start(out=hl[i][:], in_=hacc_d[c * 128:(c + 1) * 128, :]), rd=[hacc], wr=[b_hl[i]])
                for i, c in enumerate(cs_):
                    kb.op("act", lambda e: e.activation(out=jf[i % 2][:], in_=hl[i][:], func=AF.Square, accum_out=s1_[i][:]),
                          rd=[b_hl[i]], wr=[b_jf[i % 2], b_s1[i]])
                for i, c in enumerate(cs_):
                    kb.op("act", lambda e: e.activation(out=s1_[i][:], in_=s1_[i][:], func=AF.Sqrt, bias=EPS, scale=1.0 / D),
                          rd=[b_s1[i]], wr=[b_s1[i]])
                for i, c in enumerate(cs_):
                    kb.op("dve", lambda e: e.reciprocal(out=r1_[i][:], in_=s1_[i][:]), rd=[b_s1[i]], wr=[b_r1[i]])
                for i, c in enumerate(cs_):
                    kb.op("dve" if i % 2 == 0 else "pool", lambda e: e.scalar_tensor_tensor(out=ol[i][:], in0=hl[i][:], scalar=r1_[i][:, 0:1], in1=wfin[:],
                                                                  op0=ALU.mult, op1=ALU.mult),
                          rd=[b_hl[i], b_r1[i], b_wfin], wr=[b_ol[i]]) if False else \
                        kb.op("dve", lambda e: e.scalar_tensor_tensor(out=ol[i][:], in0=hl[i][:], scalar=r1_[i][:, 0:1], in1=wfin[:],
                                                                      op0=ALU.mult, op1=ALU.mult),
                              rd=[b_hl[i], b_r1[i], b_wfin], wr=[b_ol[i]])
                    kb.dma("sp", lambda q: q.dma_start(out=out_d[(c - 1) * 128:c * 128, :], in_=ol[i][:]), rd=[b_ol[i]], wr=[outb])
            kb.enabled = True
            kb.barrier()
    return nc


def _host_consts():
    l = np.arange(128)
    Uf = (l[:, None] <= l[None, :]).astype(np.float32)
    Lf = (l[:, None] > l[None, :]).astype(np.float32)
    Ub = (l[:, None] >= l[None, :]).astype(np.float32)
    Lb = (l[:, None] < l[None, :]).astype(np.float32)
    cf = np.zeros((128, C_N), np.float32)
    cf[:, C_UF:C_UF + 128] = Uf
    cf[:, C_LF:C_LF + 128] = Lf
    cf[:, C_UB:C_UB + 128] = Ub
    cf[:, C_LB:C_LB + 128] = Lb
    cf[:, C_ONES:C_ONES + 128] = 1.0
    cf[:, C_ID:C_ID + 128] = np.eye(128, dtype=np.float32)
    cf[:, C_IOTA:C_IOTA + 516] = np.arange(516, dtype=np.float32)[None, :]
    cb = np.zeros((128, CB_N), np.float32)
    cb[:, CB_ID:CB_ID + 128] = np.eye(128)
    cb[:, CB_ONES:CB_ONES + 128] = 1.0
    tv = np.zeros((128, NT, 2), np.float32)
    tv[:, :, 0] = np.arange(NT)[None, :]
    tv[:, :, 1] = np.arange(128)[:, None]
    cb[:, CB_TV:CB_TV + 66] = tv.reshape(128, 66)
    return cf, cb.astype(ml_dtypes.bfloat16)


_NC_CACHE = {}


def kernel(x, meta_tokens, w_norm_mix, w_in, w_conf_dw, b_conf_dw, conf_ln_g, conf_ln_b, w_conf_out,
           w_ssm_conv, b_ssm_conv, ssm_dt_bias, ssm_a_log, ssm_d, w_ssm_norm, w_ssm_out, w_out,
           w_norm_ffn, w_router, w_exp_gate, w_exp_up, w_exp_down, w_norm_final):
    f = lambda a: np.ascontiguousarray(np.asarray(a, dtype=np.float32))
    x = f(x)
    small = np.zeros((128, S_N), np.float32)
    small[:, S_BCONF:S_BCONF + 8] = f(b_conf_dw)[0].reshape(8, 128).T
    small[:, S_LNG:S_LNG + 8] = f(conf_ln_g)[0].reshape(8, 128).T
    small[:, S_LNB:S_LNB + 8] = f(conf_ln_b)[0].reshape(8, 128).T
    small[:, S_WCONF:S_WCONF + 248] = f(w_conf_dw)[0].T.reshape(8, 128, 31).transpose(1, 0, 2).reshape(128, 248)
    small[:, S_WSSM:S_WSSM + 168] = f(w_ssm_conv)[0].T.reshape(24, 128, 7).transpose(1, 0, 2).reshape(128, 168)
    small[:, S_BSSM:S_BSSM + 24] = f(b_ssm_conv)[0].reshape(24, 128).T
    row = np.concatenate([f(w_norm_mix)[0], f(w_ssm_norm)[0], f(w_norm_ffn)[0], f(w_norm_final),
                          f(ssm_dt_bias)[0].reshape(64), f(ssm_a_log)[0].reshape(64), f(ssm_d)[0]])
    bcast = np.ascontiguousarray(np.broadcast_to(row[None, :], (128, B_N)))
    cf, cb = _host_consts()
    if "nc" not in _NC_CACHE:
        _NC_CACHE["nc"] = build_program()
    nc = _NC_CACHE["nc"]
    shared = {
        "meta": f(meta_tokens), "w_in": f(w_in)[0], "w_conf_out": f(w_conf_out)[0], "w_ssm_out": f(w_ssm_out)[0],
        "w_out": f(w_out)[0], "w_router": f(w_router)[0], "w_eg": f(w_exp_gate)[0], "w_eu": f(w_exp_up)[0],
        "w_ed": f(w_exp_down)[0], "smallT": small, "bcast": bcast, "cf32": cf, "cbf": cb,
    }
    in_maps = [dict(shared, x=x[b]) for b in range(8)]
    res = run_bass_kernel_spmd(nc, in_maps, core_ids=list(range(8)))
    return np.stack([np.asarray(r["out"], dtype=np.float32) for r in res.results], axis=0)
```

```python
import os
import numpy as np
import ml_dtypes
from contextlib import ExitStack
import concourse.bass as bass
import concourse.mybir as mybir
from concourse.bass_utils import run_bass_kernel_spmd

F32 = mybir.dt.float32
F32R = mybir.dt.float32r
BF16 = mybir.dt.bfloat16
I32 = mybir.dt.int32
AF = mybir.ActivationFunctionType
ALU = mybir.AluOpType
AX = mybir.AxisListType

T = 4224
NT = 33
D = 1024
CAP = 514
NE = 16
TBS = [(i * 512, 512) for i in range(8)] + [(4096, 128)]
EPS = 1e-6
S_BCONF, S_LNG, S_LNB, S_WCONF, S_WSSM, S_BSSM, S_N = 0, 8, 16, 24, 272, 440, 464
B_WMIX, B_WSSM, B_WFFN, B_WFIN, B_DTB, B_ALOG, B_DSK, B_N = 0, 1024, 3072, 4096, 5120, 5184, 5248, 5280
C_UF, C_LF, C_UB, C_LB, C_ONES, C_ID, C_IOTA, C_N = 0, 128, 256, 384, 512, 640, 768, 768 + 516
CB_ID, CB_ONES, CB_TV, CB_N = 0, 128, 256, 256 + 66


class Buf:
    __slots__ = ("w", "r")

    def __init__(self):
        self.w = None
        self.r = {}


class KB:
    EPOCH = 2048

    def __init__(self, nc, st):
        self.nc = nc
        self.st = st
        self.eng = dict(pe=nc.tensor, act=nc.scalar, dve=nc.vector, pool=nc.gpsimd, sp=nc.sync)
        self.cnt = {e: 0 for e in self.eng}
        self.sems = {e: [] for e in self.eng}
        self.known = {e: {} for e in self.eng}
        self.semh = []
        self.origin = []
        self.dq = {}
        self.enabled = True

    def newsem(self, origin):
        h = self.st.enter_context(self.nc.semaphore("s%d" % len(self.semh)))
        self.semh.append(h)
        self.origin.append(origin)
        return len(self.semh) - 1

    def wait(self, e, toks, keep_one=False):
        need = {}
        kn = self.known[e]
        for sid, val in toks:
            if e == "pe" and self.origin[sid] == "pe":
                continue
            if kn.get(sid, 0) < val and need.get(sid, 0) < val:
                need[sid] = val
        items = list(need.items())
        attach = items.pop() if (keep_one and items) else None
        for sid, val in items:
            self.eng[e].wait_ge(self.semh[sid], val)
            kn[sid] = val
        if attach is not None:
            kn[attach[0]] = attach[1]
        return attach

    def _deps(self, rd, wr):
        toks = []
        for b in rd:
            if b.w is not None:
                toks.append(b.w)
        for b in wr:
            if b.w is not None:
                toks.append(b.w)
            toks.extend(b.r.items())
        return toks

    def _mark(self, tok, rd, wr):
        sid, val = tok
        for b in rd:
            if b.r.get(sid, 0) < val:
                b.r[sid] = val
        for b in wr:
            b.w = tok
            b.r = {}

    def op(self, e, fn, rd=(), wr=()):
        if not self.enabled:
            return None
        attach = self.wait(e, self._deps(rd, wr), keep_one=True)
        ins = fn(self.eng[e])
        if isinstance(ins, (list, tuple)):
            first, ins = ins[0], ins[-1]
        else:
            first = ins
        if attach is not None:
            first.wait_op(self.semh[attach[0]], attach[1], "sem-ge")
        self.cnt[e] += 1
        k = self.cnt[e]
        ep = (k - 1) // self.EPOCH
        while len(self.sems[e]) <= ep:
            self.sems[e].append(self.newsem(e))
        sid = self.sems[e][ep]
        val = (k - 1) % self.EPOCH + 1
        ins.then_inc(self.semh[sid], 1)
        tok = (sid, val)
        self._mark(tok, rd, wr)
        return tok

    def barrier(self):
        toks = []
        for e in self.eng:
            if self.cnt[e] > 0:
                k = self.cnt[e]
                toks.append((self.sems[e][(k - 1) // self.EPOCH], (k - 1) % self.EPOCH + 1))
        for pool in self.dq.values():
            toks.extend((sid, val) for sid, val in zip(pool["sids"], pool["vals"]) if val > 0)
        for e in self.eng:
            self.wait(e, toks)

    def dma(self, q, fn, rd=(), wr=(), nslots=16):
        if not self.enabled:
            return None
        self.wait(q, self._deps(rd, wr))
        pool = self.dq.setdefault(q, dict(sids=[], vals=[], n=0))
        i = pool["n"] % nslots
        pool["n"] += 1
        if len(pool["sids"]) <= i:
            pool["sids"].append(self.newsem("dma"))
            pool["vals"].append(0)
        sid = pool["sids"][i]
        if pool["vals"][i] > 0:
            self.wait(q, [(sid, pool["vals"][i])])
        ins = fn(self.eng[q])
        val = pool["vals"][i] + 16
        pool["vals"][i] = val
        ins.then_inc(self.semh[sid], 16)
        tok = (sid, val)
        self._mark(tok, rd, wr)
        return tok


def build_program(run="ABbCDEFGH", debug=False):
    nc = bass.Bass("TRN2", target_bir_lowering=False)
    dt_ = nc.dram_tensor
    x_d = dt_("x", [4096, D], F32, kind="ExternalInput").ap()
    meta_d = dt_("meta", [16, D], F32, kind="ExternalInput").ap()
    w_in = dt_("w_in", [D, 9280], F32, kind="ExternalInput").ap()
    w_co = dt_("w_conf_out", [D, D], F32, kind="ExternalInput").ap()
    w_so = dt_("w_ssm_out", [2048, D], F32, kind="ExternalInput").ap()
    w_o = dt_("w_out", [D, D], F32, kind="ExternalInput").ap()
    w_r = dt_("w_router", [D, NE], F32, kind="ExternalInput").ap()
    ned = NE if ("G" in run or os.environ.get("FORCE_NE") == "1") else 1
    w_eg = dt_("w_eg", [ned, D, 2048], F32, kind="ExternalInput").ap()
    w_eu = dt_("w_eu", [ned, D, 2048], F32, kind="ExternalInput").ap()
    w_ed = dt_("w_ed", [ned, 2048, D], F32, kind="ExternalInput").ap()
    small_d = dt_("smallT", [128, S_N], F32, kind="ExternalInput").ap()
    bc_d = dt_("bcast", [128, B_N], F32, kind="ExternalInput").ap()
    cf_d = dt_("cf32", [128, C_N], F32, kind="ExternalInput").ap()
    cb_d = dt_("cbf", [128, CB_N], BF16, kind="ExternalInput").ap()
    out_d = dt_("out", [4096, D], F32, kind="ExternalOutput").ap()
    skw = dict(kind="ExternalOutput") if debug else {}
    conv_d = dt_("conv_s", [128, 8, T], BF16, **skw).ap()
    m1_d = dt_("m1_s", [128, 8, T], BF16, **skw).ap()
    g2_d = dt_("g2_s", [128, 8, T], BF16, **skw).ap()
    xbc_d = dt_("xbc_s", [128, 24, T], BF16, **skw).ap()
    sz_d = dt_("sz_s", [NT, 128, 2048], BF16, **skw).ap()
    yb_d = dt_("yb_s", [NT, 128, 2048], BF16, **skw).ap()
    yn_d = dt_("yn_s", [NT, 128, 2048], BF16, **skw).ap()
    hn_d = dt_("hn_s", [T, D], BF16, **skw).ap()
    hacc_d = dt_("hacc_s", [T, D], F32, **skw).ap()
    uT_dbg = dt_("uT_s", [128, 8, T], BF16, **skw).ap() if debug else None
    dt_dbg = dt_("dt_s", [128, NT, 64], F32, **skw).ap() if debug else None
    aff_dbg = dt_("aff_s", [128, NT, NE], F32, **skw).ap() if debug else None
    posm_dbg = dt_("posm_s", [128, NT, NE], F32, **skw).ap() if debug else None

    win_v = w_in.rearrange("(kc p) n -> p kc n", p=128)

    with ExitStack() as st:
        kb = KB(nc, st)

        def sb(name, shape, dtype, stack=None):
            return (stack or st).enter_context(nc.sbuf_tensor("sb_" + name, shape, dtype))

        ps = [st.enter_context(nc.psum_tensor("ps%d" % i, [128, 512], F32)) for i in range(8)]
        psb = [Buf() for _ in range(8)]
        psn = [0]

        def bank():
            i = psn[0] % 8
            psn[0] += 1
            return ps[i], psb[i]

        cf = sb("cf", [128, C_N], F32)
        cbf = sb("cbf", [128, CB_N], BF16)
        sm = sb("sm", [128, S_N], F32)
        b_cf, b_cbf, b_sm = Buf(), Buf(), Buf()
        kb.dma("sp", lambda q: q.dma_start(out=cf[:], in_=cf_d), wr=[b_cf])
        kb.dma("sp", lambda q: q.dma_start(out=cbf[:], in_=cb_d), wr=[b_cbf])
        kb.dma("sp", lambda q: q.dma_start(out=sm[:], in_=small_d), wr=[b_sm])
        identb = cbf[:, CB_ID:CB_ID + 128]
        onesb = cbf[:, CB_ONES:CB_ONES + 128]
        identf = cf[:, C_ID:C_ID + 128]

        dtall = sb("dtall", [128, NT, 64], F32)
        b_dt = [Buf() for _ in range(NT)]

        def load_x_tile(i, xin, b_xin):
            if i == 0:
                kb.op("dve", lambda e: e.memset(xin[:], 0.0), wr=[b_xin])
                kb.dma("sp", lambda q: q.dma_start(out=xin[112:128, :], in_=meta_d), wr=[b_xin])
            else:
                kb.dma("sp", lambda q: q.dma_start(out=xin[:], in_=x_d[(i - 1) * 128:i * 128, :]), wr=[b_xin])

        def rms_rstd(src, b_src, junk, b_junk, ss, b_ss, rstd, b_rstd, n):
            kb.op("act", lambda e: e.activation(out=junk, in_=src, func=AF.Square, accum_out=ss[:]),
                  rd=(b_src if isinstance(b_src, list) else [b_src]), wr=[b_junk, b_ss])
            kb.op("act", lambda e: e.activation(out=ss[:], in_=ss[:], func=AF.Sqrt, bias=EPS, scale=1.0 / n),
                  rd=[b_ss], wr=[b_ss])
            kb.op("dve", lambda e: e.reciprocal(out=rstd[:], in_=ss[:]), rd=[b_ss], wr=[b_rstd])

        with ExitStack() as s1:
            uT = sb("uT", [128, 8, T], BF16, s1)
            b_uT = [Buf() for _ in range(NT)]

            def uT_bufs(t0, n):
                return b_uT[t0 // 128:(t0 + n) // 128]

            with ExitStack() as sa:
                kb.enabled = "A" in run
                wmix = sb("wmix", [128, D], F32, sa)
                b_wmix = Buf()
                kb.dma("sp", lambda q: q.dma_start(out=wmix[:], in_=bc_d[:, B_WMIX:B_WMIX + D]), wr=[b_wmix])
                xin = [sb("xinA%d" % i, [128, D], F32, sa) for i in range(4)]
                b_xin = [Buf() for _ in range(4)]
                junk = [sb("junkA%d" % i, [128, D], BF16, sa) for i in range(2)]
                b_junk = [Buf(), Buf()]
                ub = [sb("ubA%d" % i, [128, D], BF16, sa) for i in range(4)]
                b_ub = [Buf() for _ in range(4)]
                ss = [sb("ssA%d" % i, [128, 1], F32, sa) for i in range(4)]
                rs = [sb("rsA%d" % i, [128, 1], F32, sa) for i in range(4)]
                b_ss = [Buf() for _ in range(4)]
                b_rs = [Buf() for _ in range(4)]
                for i0 in range(0, NT, 4):
                    tiles = list(range(i0, min(i0 + 4, NT)))
                    for p, i in enumerate(tiles):
                        load_x_tile(i, xin[p], b_xin[p])
                    for p, i in enumerate(tiles):
                        kb.op("act", lambda e: e.activation(out=junk[p % 2][:], in_=xin[p][:], func=AF.Square, accum_out=ss[p][:]),
                              rd=[b_xin[p]], wr=[b_junk[p % 2], b_ss[p]])
                    for p, i in enumerate(tiles):
                        kb.op("act", lambda e: e.activation(out=ss[p][:], in_=ss[p][:], func=AF.Sqrt, bias=EPS, scale=1.0 / D),
                              rd=[b_ss[p]], wr=[b_ss[p]])
                    for p, i in enumerate(tiles):
                        kb.op("dve", lambda e: e.reciprocal(out=rs[p][:], in_=ss[p][:]), rd=[b_ss[p]], wr=[b_rs[p]])
                    for p, i in enumerate(tiles):
                        kb.op("dve", lambda e: e.scalar_tensor_tensor(out=ub[p][:], in0=xin[p][:], scalar=rs[p][:, 0:1],
                                                                       in1=wmix[:], op0=ALU.mult, op1=ALU.mult),
                              rd=[b_xin[p], b_rs[p], b_wmix], wr=[b_ub[p]])
                    for p, i in enumerate(tiles):
                        pt, pb = bank()
                        ptb = pt[:].bitcast(BF16)
                        kb.op("pe", lambda e: [e.transpose(ptb[:, kc * 128:(kc + 1) * 128],
                                                           ub[p][:, kc * 128:(kc + 1) * 128], identb)
                                               for kc in range(8)],
                              rd=[b_ub[p], b_cbf], wr=[pb])
                        kb.op("act", lambda e: e.activation(out=uT[:, :, i * 128:(i + 1) * 128],
                                                            in_=ptb.rearrange("p (k t) -> p k t", k=8), func=AF.Copy),
                              rd=[pb], wr=[b_uT[i]])

            kb.barrier()
            if debug:
                kb.enabled = "A" in run
                kb.dma("sp", lambda q: q.dma_start(out=uT_dbg, in_=uT[:]), rd=b_uT, wr=[Buf()])
            with ExitStack() as sbk:
                kb.enabled = "B" in run
                wA = [sb("wA%d" % i, [128, 8, 256], BF16, sbk) for i in range(2)]
                b_wA = [Buf(), Buf()]
                cT = [sb("cT%d" % i, [128, T + 30], BF16, sbk) for i in range(2)]
                b_cT = [Buf(), Buf()]
                dg = [sb("dg%d" % i, [128, 31, 128], BF16, sbk) for i in range(2)]
                b_dg = [Buf(), Buf()]
                sgt = [sb("sgt%d" % i, [128, 512], BF16, sbk) for i in range(2)]
                b_sgt = [Buf(), Buf()]
                cv = [sb("cv%d" % i, [128, 512], BF16, sbk) for i in range(2)]
                b_cv = [Buf(), Buf()]
                b_conv = [[Buf() for _ in TBS] for _ in range(8)]
                for i in range(2):
                    kb.op("pool", lambda e: e.memset(cT[i][:], 0.0), wr=[b_cT[i]])
                it = 0
                for j in range(8):
                    p = j % 2
                    kb.dma("pool", lambda q: q.dma_start(out=wA[p][:, :, 0:128], in_=win_v[:, :, j * 128:(j + 1) * 128]),
                           wr=[b_wA[p]])
                    kb.dma("pool", lambda q: q.dma_start(out=wA[p][:, :, 128:256],
                                                         in_=win_v[:, :, 1024 + j * 128:1024 + (j + 1) * 128]),
                           wr=[b_wA[p]])
                    for k in range(31):
                        kb.op("dve", lambda e: e.tensor_scalar(out=dg[p][:, k, :], in0=identb,
                                                               scalar1=sm[:, S_WCONF + j * 31 + k:S_WCONF + j * 31 + k + 1],
                                                               scalar2=None, op0=ALU.mult),
                              rd=[b_cbf, b_sm], wr=[b_dg[p]])
                    for (t0, n) in TBS:
                        pa, pab = bank()
                        pg, pgb = bank()
                        kb.op("pe", lambda e: [e.matmul(pa[:, 0:n], wA[p][:, kc, 0:128], uT[:, kc, t0:t0 + n],
                                                        start=(kc == 0), stop=(kc == 7)) for kc in range(8)],
                              rd=[b_wA[p]] + uT_bufs(t0, n), wr=[pab])
                        kb.op("pe", lambda e: [e.matmul(pg[:, 0:n], wA[p][:, kc, 128:256], uT[:, kc, t0:t0 + n],
                                                        start=(kc == 0), stop=(kc == 7)) for kc in range(8)],
                              rd=[b_wA[p]] + uT_bufs(t0, n), wr=[pgb])
                        q = it % 2
                        it += 1
                        kb.op("act", lambda e: e.activation(out=sgt[q][:, 0:n], in_=pg[:, 0:n], func=AF.Sigmoid),
                              rd=[pgb], wr=[b_sgt[q]])
                        kb.op("dve", lambda e: e.tensor_tensor(out=cT[p][:, 15 + t0:15 + t0 + n], in0=pa[:, 0:n],
                                                               in1=sgt[q][:, 0:n], op=ALU.mult),
                              rd=[pab, b_sgt[q]], wr=[b_cT[p]])
                    for ti, (t0, n) in enumerate(TBS):
                        pc, pcb = bank()
                        kb.op("pe", lambda e: [e.matmul(pc[:, 0:n], dg[p][:, k, :], cT[p][:, t0 + k:t0 + k + n],
                                                        start=(k == 0), stop=(k == 30)) for k in range(31)],
                              rd=[b_dg[p], b_cT[p]], wr=[pcb])
                        q = it % 2
                        it += 1
                        kb.op("act", lambda e: e.activation(out=cv[q][:, 0:n], in_=pc[:, 0:n], func=AF.Identity,
                                                            bias=sm[:, S_BCONF + j:S_BCONF + j + 1]),
                              rd=[pcb, b_sm], wr=[b_cv[q]])
                        kb.dma("sp", lambda qq: qq.dma_start(out=conv_d[:, j, t0:t0 + n], in_=cv[q][:, 0:n]),
                               rd=[b_cv[q]], wr=[b_conv[j][ti]])

            kb.barrier()
            with ExitStack() as sb2:
                kb.enabled = "b" in run
                wco = sb("wco", [128, 8, D], BF16, sb2)
                wg1 = sb("wg1", [128, 8, D], BF16, sb2)
                wg2 = sb("wg2", [128, 8, D], BF16, sb2)
                b_wco, b_wg1, b_wg2 = Buf(), Buf(), Buf()
                kb.dma("pool", lambda q: q.dma_start(out=wco[:], in_=w_co.rearrange("(kc p) n -> p kc n", p=128)),
                       wr=[b_wco])
                kb.dma("pool", lambda q: q.dma_start(out=wg1[:], in_=win_v[:, :, 7232:7232 + D]), wr=[b_wg1])
                kb.dma("pool", lambda q: q.dma_start(out=wg2[:], in_=win_v[:, :, 8256:8256 + D]), wr=[b_wg2])
                cvb = [sb("cvb%d" % i, [128, 8, 512], BF16, sb2) for i in range(2)]
                b_cvb = [Buf(), Buf()]
                sq = sb("sq", [128, 8, 512], BF16, sb2)
                b_sq = Buf()
                mean = sb("mean", [128, 512], F32, sb2)
                msq = sb("msq", [128, 512], F32, sb2)
                rstd = sb("rstdb", [128, 512], F32, sb2)
                nmr = sb("nmr", [128, 512], F32, sb2)
                b_mean, b_msq, b_rstd, b_nmr = Buf(), Buf(), Buf(), Buf()
                t2 = [sb("t2_%d" % i, [128, 512], F32, sb2) for i in range(2)]
                b_t2 = [Buf(), Buf()]
                cs2 = [sb("cs%d" % i, [128, 8, 512], BF16, sb2) for i in range(2)]
                b_cs2 = [Buf(), Buf()]
                sg = [sb("sg%d" % i, [128, 512], BF16, sb2) for i in range(2)]
                b_sg = [Buf(), Buf()]
                m1 = [sb("m1_0", [128, 8, 512], BF16, sb2)] * 2
                b_m1 = [Buf()] * 2
                g2 = [sb("g2_0", [128, 8, 512], BF16, sb2)] * 2
                b_g2 = [Buf()] * 2
                b_m1d = [Buf() for _ in TBS]
                b_g2d = [Buf() for _ in TBS]
                itc = [0]

                def b_LN(ti):
                    t0, n = TBS[ti]
                    it = itc[0]
                    p = ti % 2
                    cs, b_cs = cs2[p], b_cs2[p]
                    kb.dma("sp", lambda q: q.dma_start(out=cvb[p][:, :, 0:n], in_=conv_d[:, :, t0:t0 + n]),
                           rd=[b_conv[j][ti] for j in range(8)], wr=[b_cvb[p]])
                    kb.op("pool", lambda e: e.tensor_tensor(out=sq[:, :, 0:n], in0=cvb[p][:, :, 0:n],
                                                            in1=cvb[p][:, :, 0:n], op=ALU.mult),
                          rd=[b_cvb[p]], wr=[b_sq])
                    p1, p1b = bank()
                    p2, p2b = bank()
                    kb.op("pe", lambda e: [e.matmul(p1[:, 0:n], onesb, cvb[p][:, j, 0:n], start=(j == 0), stop=(j == 7))
                                           for j in range(8)], rd=[b_cbf, b_cvb[p]], wr=[p1b])
                    kb.op("pe", lambda e: [e.matmul(p2[:, 0:n], onesb, sq[:, j, 0:n], start=(j == 0), stop=(j == 7))
                                           for j in range(8)], rd=[b_cbf, b_sq], wr=[p2b])
                    kb.op("dve", lambda e: e.tensor_scalar(out=mean[:, 0:n], in0=p1[:, 0:n], scalar1=1.0 / D,
                                                           scalar2=None, op0=ALU.mult), rd=[p1b], wr=[b_mean])
                    kb.op("dve", lambda e: e.tensor_tensor(out=msq[:, 0:n], in0=mean[:, 0:n], in1=mean[:, 0:n],
                                                           op=ALU.mult), rd=[b_mean], wr=[b_msq])
                    kb.op("dve", lambda e: e.scalar_tensor_tensor(out=msq[:, 0:n], in0=p2[:, 0:n], scalar=1.0 / D,
                                                                  in1=msq[:, 0:n], op0=ALU.mult, op1=ALU.subtract),
                          rd=[p2b, b_msq], wr=[b_msq])
                    kb.op("act", lambda e: e.activation(out=msq[:, 0:n], in_=msq[:, 0:n], func=AF.Sqrt, bias=EPS),
                          rd=[b_msq], wr=[b_msq])
                    kb.op("dve", lambda e: e.reciprocal(out=rstd[:, 0:n], in_=msq[:, 0:n]), rd=[b_msq], wr=[b_rstd])
                    kb.op("dve", lambda e: e.scalar_tensor_tensor(out=nmr[:, 0:n], in0=mean[:, 0:n], scalar=-1.0,
                                                                  in1=rstd[:, 0:n], op0=ALU.mult, op1=ALU.mult),
                          rd=[b_mean, b_rstd], wr=[b_nmr])
                    for j in range(8):
                        q = it % 2
                        it += 1
                        kb.op("dve", lambda e: e.tensor_tensor(out=t2[q][:, 0:n], in0=cvb[p][:, j, 0:n],
                                                               in1=rstd[:, 0:n], op=ALU.mult),
                              rd=[b_cvb[p], b_rstd], wr=[b_t2[q]])
                        kb.op("dve", lambda e: e.tensor_tensor(out=t2[q][:, 0:n], in0=t2[q][:, 0:n],
                                                               in1=nmr[:, 0:n], op=ALU.add),
                              rd=[b_nmr], wr=[b_t2[q]])
                        kb.op("act", lambda e: e.activation(out=cs[:, j, 0:n], in_=t2[q][:, 0:n], func=AF.Silu,
                                                            scale=sm[:, S_LNG + j:S_LNG + j + 1],
                                                            bias=sm[:, S_LNB + j:S_LNB + j + 1]),
                              rd=[b_t2[q], b_sm], wr=[b_cs])
                    itc[0] = it

                def b_MM(ti):
                    t0, n = TBS[ti]
                    it = itc[0]
                    p = ti % 2
                    cs, b_cs = cs2[p], b_cs2[p]
                    for dc in range(8):
                        pbk, pbb = bank()
                        pgk, pgb = bank()
                        kb.op("pe", lambda e: [e.matmul(pbk[:, 0:n], wco[:, kc, dc * 128:(dc + 1) * 128], cs[:, kc, 0:n],
                                                        start=(kc == 0), stop=(kc == 7)) for kc in range(8)],
                              rd=[b_wco, b_cs], wr=[pbb])
                        kb.op("pe", lambda e: [e.matmul(pgk[:, 0:n], wg1[:, kc, dc * 128:(dc + 1) * 128],
                                                        uT[:, kc, t0:t0 + n], start=(kc == 0), stop=(kc == 7))
                                               for kc in range(8)],
                              rd=[b_wg1] + uT_bufs(t0, n), wr=[pgb])
                        q = it % 2
                        it += 1
                        kb.op("act", lambda e: e.activation(out=sg[q][:, 0:n], in_=pgk[:, 0:n], func=AF.Sigmoid),
                              rd=[pgb], wr=[b_sg[q]])
                        kb.op("dve", lambda e: e.tensor_tensor(out=m1[p][:, dc, 0:n], in0=pbk[:, 0:n],
                                                               in1=sg[q][:, 0:n], op=ALU.mult),
                              rd=[pbb, b_sg[q]], wr=[b_m1[p]])
                        pg2, pg2b = bank()
                        kb.op("pe", lambda e: [e.matmul(pg2[:, 0:n], wg2[:, kc, dc * 128:(dc + 1) * 128],
                                                        uT[:, kc, t0:t0 + n], start=(kc == 0), stop=(kc == 7))
                                               for kc in range(8)],
                              rd=[b_wg2] + uT_bufs(t0, n), wr=[pg2b])
                        kb.op("act", lambda e: e.activation(out=g2[p][:, dc, 0:n], in_=pg2[:, 0:n], func=AF.Sigmoid),
                              rd=[pg2b], wr=[b_g2[p]])
                    kb.dma("sp", lambda qq: qq.dma_start(out=m1_d[:, :, t0:t0 + n], in_=m1[p][:, :, 0:n]),
                           rd=[b_m1[p]], wr=[b_m1d[ti]])
                    kb.dma("sp", lambda qq: qq.dma_start(out=g2_d[:, :, t0:t0 + n], in_=g2[p][:, :, 0:n]),
                           rd=[b_g2[p]], wr=[b_g2d[ti]])
                    itc[0] = it

                b_LN(0)
                for ti in range(len(TBS)):
                    if ti + 1 < len(TBS):
                        b_LN(ti + 1)
                    b_MM(ti)

            kb.barrier()
            with ExitStack() as sc:
                kb.enabled = "C" in run
                wz = sb("wz", [128, 8, 2048], BF16, sc)
                wdt = sb("wdt", [128, 8, 64], BF16, sc)
                b_wz, b_wdt = Buf(), Buf()
                for qq_ in range(4):
                    kb.dma("pool", lambda q: q.dma_start(out=wz[:, :, qq_ * 512:(qq_ + 1) * 512],
                                                         in_=win_v[:, :, 2048 + qq_ * 512:2048 + (qq_ + 1) * 512]),
                           wr=[b_wz])
                kb.dma("pool", lambda q: q.dma_start(out=wdt[:], in_=win_v[:, :, 7168:7232]), wr=[b_wdt])
                dtb = sb("dtb", [128, 64], F32, sc)
                b_dtb = Buf()
                kb.dma("sp", lambda q: q.dma_start(out=dtb[:], in_=bc_d[:, B_DTB:B_DTB + 64]), wr=[b_dtb])
                szt = [sb("szt%d" % i, [128, 2048], BF16, sc) for i in range(2)]
                b_szt = [Buf(), Buf()]
                b_szd = [Buf() for _ in range(NT)]
                dte = sb("dte", [128, 64], F32, sc)
                b_dte = Buf()
                for i in range(NT):
                    p = i % 2
                    for qz in range(4):
                        pz, pzb = bank()
                        kb.op("pe", lambda e: [e.matmul(pz[:, :], uT[:, kc, i * 128:(i + 1) * 128],
                                                        wz[:, kc, qz * 512:(qz + 1) * 512],
                                                        start=(kc == 0), stop=(kc == 7)) for kc in range(8)],
                              rd=[b_wz, b_uT[i]], wr=[pzb])
                        kb.op("act", lambda e: e.activation(out=szt[p][:, qz * 512:(qz + 1) * 512], in_=pz[:, :],
                                                            func=AF.Silu), rd=[pzb], wr=[b_szt[p]])
                    kb.dma("sp", lambda q: q.dma_start(out=sz_d[i], in_=szt[p][:]), rd=[b_szt[p]], wr=[b_szd[i]])
                    pd, pdb = bank()
                    kb.op("pe", lambda e: [e.matmul(pd[:, 0:64], uT[:, kc, i * 128:(i + 1) * 128], wdt[:, kc, :],
                                                    start=(kc == 0), stop=(kc == 7)) for kc in range(8)],
                          rd=[b_wdt, b_uT[i]], wr=[pdb])
                    kb.op("dve", lambda e: e.tensor_tensor(out=dte[:], in0=pd[:, 0:64], in1=dtb[:], op=ALU.add),
                          rd=[pdb, b_dtb], wr=[b_dte])
                    kb.op("act", lambda e: e.activation(out=dte[:], in_=dte[:], func=AF.Exp), rd=[b_dte], wr=[b_dte])
                    kb.op("act", lambda e: e.activation(out=dtall[:, i, :], in_=dte[:], func=AF.Ln, bias=1.0),
                          rd=[b_dte], wr=[b_dt[i]])
                    if i == 0:
                        kb.op("dve", lambda e: e.memset(dtall[0:96, 0, :], 0.0), wr=[b_dt[0]])
                        kb.op("dve", lambda e: e.memset(dtall[96:112, 0, :], 0.0), wr=[b_dt[0]])
                if debug:
                    kb.dma("sp", lambda q: q.dma_start(out=dt_dbg, in_=dtall[:]), rd=b_dt, wr=[Buf()])
                wX = [sb("wX%d" % i, [128, 8, 128], BF16, sc) for i in range(2)]
                b_wX = [Buf(), Buf()]
                xT = [sb("xT%d" % i, [128, T + 6], BF16, sc) for i in range(2)]
                b_xT = [Buf(), Buf()]
                dg7 = [sb("dg7_%d" % i, [128, 7, 128], BF16, sc) for i in range(2)]
                b_dg7 = [Buf(), Buf()]
                xc = [sb("xc%d" % i, [128, 512], BF16, sc) for i in range(2)]
                b_xc = [Buf(), Buf()]
                b_xbcd = [[Buf() for _ in TBS] for _ in range(24)]
                for i in range(2):
                    kb.op("pool", lambda e: e.memset(xT[i][:], 0.0), wr=[b_xT[i]])
                it = 0
                for j in range(24):
                    p = j % 2
                    kb.dma("pool", lambda q: q.dma_start(out=wX[p][:], in_=win_v[:, :, 4096 + j * 128:4096 + (j + 1) * 128]),
                           wr=[b_wX[p]])
                    for k in range(7):
                        kb.op("dve", lambda e: e.tensor_scalar(out=dg7[p][:, k, :], in0=identb,
                                                               scalar1=sm[:, S_WSSM + j * 7 + k:S_WSSM + j * 7 + k + 1],
                                                               scalar2=None, op0=ALU.mult),
                              rd=[b_cbf, b_sm], wr=[b_dg7[p]])
                    for (t0, n) in TBS:
                        px, pxb = bank()
                        kb.op("pe", lambda e: [e.matmul(px[:, 0:n], wX[p][:, kc, :], uT[:, kc, t0:t0 + n],
                                                        start=(kc == 0), stop=(kc == 7)) for kc in range(8)],
                              rd=[b_wX[p]] + uT_bufs(t0, n), wr=[pxb])
                        kb.op("act", lambda e: e.activation(out=xT[p][:, 3 + t0:3 + t0 + n], in_=px[:, 0:n], func=AF.Copy),
                              rd=[pxb], wr=[b_xT[p]])
                    for ti, (t0, n) in enumerate(TBS):
                        pc, pcb = bank()
                        kb.op("pe", lambda e: [e.matmul(pc[:, 0:n], dg7[p][:, k, :], xT[p][:, t0 + k:t0 + k + n],
                                                        start=(k == 0), stop=(k == 6)) for k in range(7)],
                              rd=[b_dg7[p], b_xT[p]], wr=[pcb])
                        q = it % 2
                        it += 1
                        kb.op("act", lambda e: e.activation(out=xc[q][:, 0:n], in_=pc[:, 0:n], func=AF.Silu,
                                                            bias=sm[:, S_BSSM + j:S_BSSM + j + 1]),
                              rd=[pcb, b_sm], wr=[b_xc[q]])
                        if ti == 0:
                            kb.op("dve", lambda e: e.memset(xc[q][:, 0:112], 0.0), wr=[b_xc[q]])
                        kb.dma("sp", lambda qq: qq.dma_start(out=xbc_d[:, j, t0:t0 + n], in_=xc[q][:, 0:n]),
                               rd=[b_xc[q]], wr=[b_xbcd[j][ti]])

        kb.barrier()
        hnrow = Buf()
        hacc = Buf()
        b_ynd = [Buf() for _ in range(NT)]
        with ExitStack() as sd:
            kb.enabled = ("D" in run) or ("E" in run)
            abc = sb("abc", [128, 160], F32, sd)
            b_abc = Buf()
            kb.dma("sp", lambda q: q.dma_start(out=abc[:], in_=bc_d[:, B_DTB:B_DTB + 160]), wr=[b_abc])
            Aneg = sb("Aneg", [128, 64], F32, sd)
            b_A = Buf()
            kb.op("act", lambda e: e.activation(out=Aneg[:], in_=abc[:, 64:128], func=AF.Exp), rd=[b_abc], wr=[b_A])
            kb.op("dve", lambda e: e.tensor_scalar(out=Aneg[:], in0=Aneg[:], scalar1=-1.0, scalar2=None, op0=ALU.mult),
                  rd=[b_A], wr=[b_A])

            def dbl(name, shape, dtype):
                return [sb("%s_%d" % (name, i), shape, dtype, sd) for i in range(2)], [Buf(), Buf()]

            XT, b_XT = dbl("XT", [128, 24, 128], BF16)
            xs_tm2, b_xs2 = dbl("xs_tm", [128, 2048], BF16)
            b_tm2, b_btm2 = dbl("b_tm", [128, 512], BF16)
            cbm2, b_cbm2 = dbl("cbm", [128, 4, 128], BF16)
            a322, b_a322 = dbl("a32", [128, 32], F32)
            rhsA2, b_rhsA2 = dbl("rhsA", [128, 32, 128], F32)
            E2_, b_E2 = dbl("E", [128, 32, 128], BF16)
            dec2, b_dec2 = dbl("dec", [128, 96], F32)
            w22, b_w22 = dbl("w2", [128, 32], F32)
            xd2, b_xd2 = dbl("xd", [128, 32, 64], BF16)
            xdd2, b_xdd2 = dbl("xdd", [128, 32, 64], BF16)
            yacc2, b_yacc2 = dbl("yacc", [128, 2048], F32)
            S32 = sb("S32", [128, 2048], F32, sd)
            Sbf = sb("Sbf", [128, 2048], BF16, sd)
            b_S32g = [Buf() for _ in range(4)]
            b_Sbfg = [Buf() for _ in range(4)]
            b_Eg2 = [[Buf() for _ in range(4)] for _ in range(2)]
            b_yaccg2 = [[Buf() for _ in range(4)] for _ in range(2)]
            tmp = [sb("tmpo%d" % i, [128, 512], F32, sd) for i in range(2)]
            b_tmp = [Buf(), Buf()]
            b_ybd = [Buf() for _ in range(NT)]
            nchunk = [0]

            class Ctx:
                pass

            SEGR = os.environ.get("SEGR", "1") == "1"
            LR = sb("LR", [128, 2, 128], F32R, sd)
            b_LR = Buf()
            if SEGR:
                kb.op("dve", lambda e: e.tensor_copy(out=LR[:, 0, :], in_=cf[:, C_LF:C_LF + 128]), rd=[b_cf], wr=[b_LR])
                kb.op("dve", lambda e: e.tensor_copy(out=LR[:, 1, :], in_=cf[:, C_LB:C_LB + 128]), rd=[b_cf], wr=[b_LR])
            S_ENG = os.environ.get("S_ENG", "pool")
            GT_POOL = int(os.environ.get("GT_POOL", 0))

            def front1(c, d, preload=None):
                k = Ctx()
                p = nchunk[0] % 2
                nchunk[0] += 1
                k.p, k.c, k.d = p, c, d
                k.xs_tm, k.b_xs = xs_tm2[p], b_xs2[p]
                k.b_tm, k.b_btm = b_tm2[p], b_btm2[p]
                k.cbm, k.b_cbm = cbm2[p], b_cbm2[p]
                k.a32, k.b_a32 = a322[p], b_a322[p]
                k.rhsA, k.b_rhsA = rhsA2[p], b_rhsA2[p]
                k.E, k.b_E = E2_[p], b_E2[p]
                k.dec, k.b_dec = dec2[p], b_dec2[p]
                k.w2, k.b_w2 = w22[p], b_w22[p]
                k.xd, k.b_xd = xd2[p], b_xd2[p]
                k.xdd, k.b_xdd = xdd2[p], b_xdd2[p]
                k.yacc, k.b_yacc = yacc2[p], b_yaccg2[p]
                k.b_Eg = b_Eg2[p]
                k.xs3 = k.xs_tm[:].rearrange("p (h d) -> p h d", d=64)
                k.add_prev = preload is not None
                xs_tm, b_xs, b_tm, b_btm, cbm, b_cbm = k.xs_tm, k.b_xs, k.b_tm, k.b_btm, k.cbm, k.b_cbm
                a32, b_a32, rhsA, b_rhsA, E, b_E, dec, b_dec = k.a32, k.b_a32, k.rhsA, k.b_rhsA, k.E, k.b_E, k.dec, k.b_dec
                w2, b_w2, xd, b_xd, xdd, b_xdd, xs3 = k.w2, k.b_w2, k.xd, k.b_xd, k.xdd, k.b_xdd, k.xs3
                k.ybl = None
                if k.add_prev:
                    preload(k)
                rd_x = [b_xbcd[j][min(c // 4, 8)] for j in range(24)]
                kb.dma("sp", lambda q: q.dma_start(out=XT[p][:], in_=xbc_d[:, :, c * 128:(c + 1) * 128]),
                       rd=rd_x, wr=[b_XT[p]])
                X = XT[p]
                bX = b_XT[p]
                k.X, k.bX = X, bX
                U = cf[:, (C_UF if d == 0 else C_UB):(C_UF if d == 0 else C_UB) + 128]
                L = cf[:, (C_LF if d == 0 else C_LB):(C_LF if d == 0 else C_LB) + 128]
                onesf = cf[:, C_ONES:C_ONES + 128]
                Lr = LR[:, d, :] if SEGR else L
                dtc = dtall[:, c, d * 32:(d + 1) * 32]
                kb.op("dve", lambda e: e.tensor_tensor(out=a32[:], in0=dtc, in1=Aneg[:, d * 32:(d + 1) * 32],
                                                       op=ALU.mult), rd=[b_dt[c], b_A], wr=[b_a32])
                kb.op("pool", lambda e: e.tensor_tensor(out=(rhsA[:].bitcast(F32R) if SEGR else rhsA[:]), in0=a32[:].unsqueeze(2).broadcast_to([128, 32, 128]),
                                                        in1=U.unsqueeze(1).broadcast_to([128, 32, 128]), op=ALU.mult),
                      rd=[b_a32, b_cf], wr=[b_rhsA])
                psm, psmb = bank()
                kb.op("pe", lambda e: [e.matmul(psm[:, 0:32], U, a32[:], start=True, stop=True),
                                       e.matmul(psm[:, 32:64], L, a32[:], start=True, stop=True),
                                       e.matmul(psm[:, 64:96], onesf, a32[:], start=True, stop=True)],
                      rd=[b_cf, b_a32], wr=[psmb])
                kb.op("act", lambda e: e.activation(out=dec[:], in_=psm[:, 0:96], func=AF.Exp), rd=[psmb], wr=[b_dec])
                kb.op("dve", lambda e: e.tensor_tensor(out=w2[:], in0=dtc, in1=dec[:, 32:64], op=ALU.mult),
                      rd=[b_dt[c], b_dec], wr=[b_w2])
                for half in range(2):
                    pt, pb = bank()
                    ptb = pt[:].bitcast(BF16)
                    kb.op("pe", lambda e: [e.transpose(ptb[:, kk * 128:(kk + 1) * 128], X[:, half * 8 + kk, :], identb)
                                           for kk in range(8)], rd=[bX, b_cbf], wr=[pb])
                    kb.op("act", lambda e: e.activation(out=xs_tm[:, half * 1024:(half + 1) * 1024], in_=ptb,
                                                        func=AF.Copy), rd=[pb], wr=[b_xs])
                pt, pb = bank()
                ptb = pt[:].bitcast(BF16)
                kb.op("pe", lambda e: [e.transpose(ptb[:, kk * 128:(kk + 1) * 128], X[:, 16 + kk, :], identb)
                                       for kk in range(4)], rd=[bX, b_cbf], wr=[pb])
                kb.op("act", lambda e: e.activation(out=b_tm[:], in_=ptb[:, 0:512], func=AF.Copy), rd=[pb], wr=[b_btm])
                pcb_, pcbb = bank()
                kb.op("pe", lambda e: [e.matmul(pcb_[:, g * 128:(g + 1) * 128], X[:, 16 + g, :], X[:, 20 + g, :],
                                                start=True, stop=True) for g in range(4)], rd=[bX], wr=[pcbb])
                kb.op("dve", lambda e: e.tensor_tensor(out=cbm[:], in0=pcb_[:].rearrange("p (g l) -> p g l", g=4),
                                                       in1=U.unsqueeze(1).broadcast_to([128, 4, 128]), op=ALU.mult),
                      rd=[pcbb, b_cf], wr=[b_cbm])
                kb.op("pool", lambda e: e.tensor_tensor(out=xd[:], in0=xs3, in1=dtc.unsqueeze(2).broadcast_to([128, 32, 64]),
                                                        op=ALU.mult), rd=[b_xs, b_dt[c]], wr=[b_xd])
                kb.op("pool", lambda e: e.tensor_tensor(out=xdd[:], in0=xs3,
                                                        in1=w2[:].unsqueeze(2).broadcast_to([128, 32, 64]), op=ALU.mult),
                      rd=[b_xs, b_w2], wr=[b_xdd])
                for q8 in range(8):
                    pse, pseb = bank()
                    kb.op("pe", lambda e: e.matmul(pse[:, :], Lr, (rhsA[:, q8 * 4:(q8 + 1) * 4, :].bitcast(F32R) if SEGR else rhsA[:, q8 * 4:(q8 + 1) * 4, :]),
                                                   start=True, stop=True),
                          rd=[b_cf, b_rhsA, b_LR], wr=[pseb])
                    kb.op("act", lambda e: e.activation(out=E[:, q8 * 4:(q8 + 1) * 4, :],
                                                        in_=pse[:].rearrange("p (h l) -> p h l", h=4), func=AF.Exp),
                          rd=[pseb], wr=[k.b_Eg[q8 // 2]])
                return k

            def front2(k):
                for g in range(4):
                    kb.op("pool" if g < GT_POOL else "dve", lambda e: e.tensor_tensor(out=k.E[:, g * 8:(g + 1) * 8, :], in0=k.E[:, g * 8:(g + 1) * 8, :],
                                                           in1=k.cbm[:, g:g + 1, :].broadcast_to([128, 8, 128]),
                                                           op=ALU.mult), rd=[k.b_cbm], wr=[k.b_Eg[g]])

            def back(k):
                GT, b_GT, xd, b_xd, xdd, b_xdd = k.E, k.b_E, k.xd, k.b_xd, k.xdd, k.b_xdd
                X, bX, b_tm, b_btm, dec, b_dec = k.X, k.bX, k.b_tm, k.b_btm, k.dec, k.b_dec
                yacc, b_yacc = k.yacc, k.b_yacc
                for g in range(4):
                    py, pyb = bank()
                    po, pob = bank()
                    pst, pstb = bank()
                    if k.add_prev:
                        kb.op("pe", lambda e: [e.matmul(py[:, :], identb, k.ybl[:, g * 512:(g + 1) * 512], start=True, stop=False)]
                              + [e.matmul(py[:, hh * 64:(hh + 1) * 64], DI[:, g * 8 + hh, :],
                                          k.xs_tm[:, (g * 8 + hh) * 64:(g * 8 + hh + 1) * 64], start=False, stop=False)
                                 for hh in range(8)]
                              + [e.matmul(py[:, hh * 64:(hh + 1) * 64], GT[:, g * 8 + hh, :], xd[:, g * 8 + hh, :],
                                          start=False, stop=(hh == 7)) for hh in range(8)],
                              rd=[k.b_Eg[g], b_xd, k.b_ybl, k.b_xs, b_DI, b_cbf], wr=[pyb])
                    else:
                        kb.op("pe", lambda e: [e.matmul(py[:, hh * 64:(hh + 1) * 64], GT[:, g * 8 + hh, :], xd[:, g * 8 + hh, :],
                                                        start=True, stop=True) for hh in range(8)],
                              rd=[k.b_Eg[g], b_xd], wr=[pyb])
                    kb.op("pe", lambda e: e.matmul(po[:, :], X[:, 20 + g, :], Sbf[:, g * 512:(g + 1) * 512],
                                                   start=True, stop=True), rd=[bX, b_Sbfg[g]], wr=[pob])
                    kb.op("pe", lambda e: e.matmul(pst[:, :], b_tm[:, g * 128:(g + 1) * 128],
                                                   xdd[:, g * 8:(g + 1) * 8, :], start=True, stop=True),
                          rd=[b_btm, b_xdd], wr=[pstb])
                    q = g % 2
                    kb.op("dve", lambda e: e.tensor_tensor(
                        out=tmp[q][:].rearrange("p (h d) -> p h d", d=64),
                        in0=po[:].rearrange("p (h d) -> p h d", d=64),
                        in1=dec[:, g * 8:(g + 1) * 8].unsqueeze(2).broadcast_to([128, 8, 64]), op=ALU.mult),
                        rd=[pob, b_dec], wr=[b_tmp[q]])
                    kb.op("dve", lambda e: e.tensor_tensor(out=yacc[:, g * 512:(g + 1) * 512], in0=py[:, :], in1=tmp[q][:],
                                                           op=ALU.add), rd=[pyb, b_tmp[q]], wr=[b_yacc[g]])
                    kb.op(S_ENG, lambda e: e.tensor_tensor(
                        out=S32[:, g * 512:(g + 1) * 512].rearrange("p (h d) -> p h d", d=64),
                        in0=S32[:, g * 512:(g + 1) * 512].rearrange("p (h d) -> p h d", d=64),
                        in1=dec[:, 64 + g * 8:64 + (g + 1) * 8].unsqueeze(2).broadcast_to([128, 8, 64]), op=ALU.mult),
                        rd=[b_dec], wr=[b_S32g[g]])
                    kb.op("dve", lambda e: e.tensor_tensor(out=S32[:, g * 512:(g + 1) * 512], in0=pst[:, :],
                                                           in1=S32[:, g * 512:(g + 1) * 512], op=ALU.add),
                          rd=[pstb], wr=[b_S32g[g]])
                    kb.op("act", lambda e: e.activation(out=Sbf[:, g * 512:(g + 1) * 512], in_=S32[:, g * 512:(g + 1) * 512],
                                                        func=AF.Copy), rd=[b_S32g[g]], wr=[b_Sbfg[g]])

            def run_pass(order, d, preload_fn, post_fn):
                k = front1(order[0], d, preload_fn(order[0]) if preload_fn else None)
                front2(k)
                for i, c in enumerate(order):
                    kn = None
                    if i + 1 < len(order):
                        cn = order[i + 1]
                        kn = front1(cn, d, preload_fn(cn) if preload_fn else None)
                    back(k)
                    post_fn(k)
                    if kn is not None:
                        front2(kn)
                    k = kn

            kb.enabled = "D" in run
            kb.op("dve", lambda e: e.memset(S32[:], 0.0), wr=b_S32g)
            kb.op("dve", lambda e: e.memset(Sbf[:], 0.0), wr=b_Sbfg)
            run_pass(list(range(NT - 1, -1, -1)), 1, None,
                     lambda k: kb.dma("pool", lambda q: q.dma_start(out=yb_d[k.c], in_=k.yacc[:]), rd=k.b_yacc, wr=[b_ybd[k.c]]))

            kb.enabled = "E" in run
            kb.op("dve", lambda e: e.memset(S32[:], 0.0), wr=b_S32g)
            kb.op("dve", lambda e: e.memset(Sbf[:], 0.0), wr=b_Sbfg)
            wns = sb("wns", [128, 2048], F32, sd)
            b_wns = Buf()
            kb.dma("sp", lambda q: q.dma_start(out=wns[:], in_=bc_d[:, B_WSSM:B_WSSM + 2048]), wr=[b_wns])
            szl2, b_szl2 = dbl("szl", [128, 2048], BF16)
            yn2, b_yn2 = dbl("yn", [128, 2048], BF16)
            ybl2, b_ybl2 = dbl("ybl", [128, 2048], BF16)
            DI = sb("DI", [128, 32, 128], BF16, sd)
            b_DI = Buf()
            for hh_ in range(32):
                kb.op("dve", lambda e: e.tensor_scalar(out=DI[:, hh_, :], in0=identb, scalar1=abc[:, 128 + hh_:129 + hh_],
                                                       scalar2=None, op0=ALU.mult), rd=[b_cbf, b_abc], wr=[b_DI])
            ssf2, b_ssf2 = dbl("ssf", [128, 1], F32)
            rsf2, b_rsf2 = dbl("rsf", [128, 1], F32)

            def fwd_preload(c):
                def f(k):
                    k.ybl, k.b_ybl = ybl2[k.p], b_ybl2[k.p]
                    kb.dma("sp", lambda q: q.dma_start(out=k.ybl[:], in_=yb_d[c]), rd=[b_ybd[c]], wr=[k.b_ybl])
                return f

            def fwd_post(k):
                p, c = k.p, k.c
                yacc, b_yacc = k.yacc, k.b_yacc
                kb.dma("sp", lambda q: q.dma_start(out=szl2[p][:], in_=sz_d[c]), rd=[b_szd[c]], wr=[b_szl2[p]])
                kb.op("pool", lambda e: e.tensor_tensor(out=yacc[:], in0=yacc[:], in1=szl2[p][:], op=ALU.mult),
                      rd=[b_szl2[p]], wr=b_yacc)
                rms_rstd(yacc[:], b_yacc, yn2[p][:], b_yn2[p], ssf2[p], b_ssf2[p], rsf2[p], b_rsf2[p], 2048)
                kb.op("dve", lambda e: e.scalar_tensor_tensor(out=yn2[p][:], in0=yacc[:], scalar=rsf2[p][:, 0:1], in1=wns[:],
                                                              op0=ALU.mult, op1=ALU.mult),
                      rd=b_yacc + [b_rsf2[p], b_wns], wr=[b_yn2[p]])
                kb.dma("sp", lambda q: q.dma_start(out=yn_d[c], in_=yn2[p][:]), rd=[b_yn2[p]], wr=[b_ynd[c]])

            run_pass(list(range(NT)), 0, fwd_preload, fwd_post)

        kb.barrier()
        affTM = sb("affTM", [128, NT, NE], F32)
        b_affTM = Buf()
        affT = sb("affT", [NE, T], F32)
        b_affT = Buf()
        with ExitStack() as se:
            kb.enabled = "E" in run
            wso = sb("wso", [128, 16, D], BF16, se)
            wout = sb("wout", [128, 8, D], BF16, se)
            wr_ = sb("wr_", [128, 8, NE], BF16, se)
            b_wso, b_wout, b_wr = Buf(), Buf(), Buf()
            kb.dma("pool", lambda q: q.dma_start(out=wso[:], in_=w_so.rearrange("(kc p) n -> p kc n", p=128)), wr=[b_wso])
            kb.dma("pool", lambda q: q.dma_start(out=wout[:], in_=w_o.rearrange("(kc p) n -> p kc n", p=128)), wr=[b_wout])
            kb.dma("pool", lambda q: q.dma_start(out=wr_[:], in_=w_r.rearrange("(kc p) n -> p kc n", p=128)), wr=[b_wr])
            wnf = sb("wnf", [128, D], F32, se)
            b_wnf = Buf()
            kb.dma("sp", lambda q: q.dma_start(out=wnf[:], in_=bc_d[:, B_WFFN:B_WFFN + D]), wr=[b_wnf])

            def nbuf(name, shape, dtype, n):
                return [sb("%s_%d" % (name, i), shape, dtype, se) for i in range(n)], [Buf() for _ in range(n)]

            ynl1, b_ynl1 = nbuf("ynl", [128, 4, 2048], BF16, 1)
            ynT2, b_ynT2 = nbuf("ynT", [128, 16, 512], BF16, 1)
            ynT2, b_ynT2 = ynT2 * 2, b_ynT2 * 2
            m1l1, b_m1l1 = nbuf("m1l", [128, 8, 512], BF16, 1)
            g2l1, b_g2l1 = nbuf("g2l", [128, 8, 512], BF16, 1)
            mT2, b_mT2 = nbuf("mT", [128, 8, 512], BF16, 2)
            xinF4, b_xinF4 = nbuf("xinF", [128, D], F32, 4)
            hT4, b_hT4 = nbuf("h_t", [128, D], F32, 4)
            hnb4, b_hnb4 = nbuf("hnb", [128, D], BF16, 4)
            hnT4, b_hnT4 = nbuf("hnT", [128, 8, 128], BF16, 4)
            junkE2, b_junkE2 = nbuf("junkE", [128, D], BF16, 2)
            ssE4, b_ssE4 = nbuf("ssE", [128, 1], F32, 4)
            rsE4, b_rsE4 = nbuf("rsE", [128, 1], F32, 4)
            lg4, b_lg4 = nbuf("lg", [128, NE], F32, 4)
            mx4, b_mx4 = nbuf("mx", [128, 1], F32, 4)
            sme4, b_sme4 = nbuf("sme", [128, 1], F32, 4)

            def stage_TS(ti):
                t0, n = TBS[ti]
                pb_ = ti % 2
                nt_ = n // 128
                c0 = t0 // 128
                ynl, b_ynl = ynl1[0], b_ynl1[0]
                ynT, b_ynT = ynT2[pb_], b_ynT2[pb_]
                m1l, b_m1l = m1l1[0], b_m1l1[0]
                g2l, b_g2l = g2l1[0], b_g2l1[0]
                mT, b_mT = mT2[pb_], b_mT2[pb_]
                kb.dma("sp", lambda q: q.dma_start(out=ynl[:, 0:nt_, :], in_=yn_d[c0:c0 + nt_].rearrange("t p f -> p t f")),
                       rd=b_ynd[c0:c0 + nt_], wr=[b_ynl])
                kb.dma("sp", lambda q: q.dma_start(out=m1l[:, :, 0:n], in_=m1_d[:, :, t0:t0 + n]), rd=[b_m1d[ti]], wr=[b_m1l])
                kb.dma("sp", lambda q: q.dma_start(out=g2l[:, :, 0:n], in_=g2_d[:, :, t0:t0 + n]), rd=[b_g2d[ti]], wr=[b_g2l])
                for tt in range(nt_):
                    for half in range(2):
                        pt, pb = bank()
                        ptb = pt[:].bitcast(BF16)
                        kb.op("pe", lambda e: [e.transpose(ptb[:, k * 128:(k + 1) * 128],
                                                           ynl[:, tt, (half * 8 + k) * 128:(half * 8 + k + 1) * 128], identb)
                                               for k in range(8)], rd=[b_ynl, b_cbf], wr=[pb])
                        kb.op("act", lambda e: e.activation(out=ynT[:, half * 8:(half + 1) * 8, tt * 128:(tt + 1) * 128],
                                                            in_=ptb.rearrange("p (k t) -> p k t", k=8), func=AF.Copy),
                              rd=[pb], wr=[b_ynT])
                for dc in range(8):
                    pbs, pbsb = bank()
                    kb.op("pe", lambda e: [e.matmul(pbs[:, 0:n], wso[:, kc, dc * 128:(dc + 1) * 128], ynT[:, kc, 0:n],
                                                    start=(kc == 0), stop=(kc == 15)) for kc in range(16)],
                          rd=[b_wso, b_ynT], wr=[pbsb])
                    kb.op("dve", lambda e: e.tensor_tensor(out=mT[:, dc, 0:n], in0=pbs[:, 0:n], in1=g2l[:, dc, 0:n],
                                                           op=ALU.mult), rd=[pbsb, b_g2l], wr=[b_mT])
                    kb.op("pool", lambda e: e.tensor_tensor(out=mT[:, dc, 0:n], in0=mT[:, dc, 0:n], in1=m1l[:, dc, 0:n],
                                                            op=ALU.add), rd=[b_m1l], wr=[b_mT])

            def stage_W(ti):
                t0, n = TBS[ti]
                pb_ = ti % 2
                nt_ = n // 128
                c0 = t0 // 128
                mT, b_mT = mT2[pb_], b_mT2[pb_]
                for tt in range(nt_):
                    c = c0 + tt
                    load_x_tile(c, xinF4[tt], b_xinF4[tt])
                    for dh in range(2):
                        ph, phb = bank()
                        kb.op("pe", lambda e: [e.matmul(ph[:, :], mT[:, kc, tt * 128:(tt + 1) * 128],
                                                        wout[:, kc, dh * 512:(dh + 1) * 512],
                                                        start=(kc == 0), stop=(kc == 7)) for kc in range(8)],
                              rd=[b_wout, b_mT], wr=[phb])
                        kb.op("dve", lambda e: e.tensor_tensor(out=hT4[tt][:, dh * 512:(dh + 1) * 512], in0=ph[:, :],
                                                               in1=xinF4[tt][:, dh * 512:(dh + 1) * 512], op=ALU.add),
                              rd=[phb, b_xinF4[tt]], wr=[b_hT4[tt]])
                    kb.dma("sp", lambda q: q.dma_start(out=hacc_d[c * 128:(c + 1) * 128, :], in_=hT4[tt][:]),
                           rd=[b_hT4[tt]], wr=[hacc])
                for tt in range(nt_):
                    kb.op("act", lambda e: e.activation(out=junkE2[tt % 2][:], in_=hT4[tt][:], func=AF.Square,
                                                        accum_out=ssE4[tt][:]),
                          rd=[b_hT4[tt]], wr=[b_junkE2[tt % 2], b_ssE4[tt]])
                for tt in range(nt_):
                    kb.op("act", lambda e: e.activation(out=ssE4[tt][:], in_=ssE4[tt][:], func=AF.Sqrt, bias=EPS, scale=1.0 / D),
                          rd=[b_ssE4[tt]], wr=[b_ssE4[tt]])
                for tt in range(nt_):
                    kb.op("dve", lambda e: e.reciprocal(out=rsE4[tt][:], in_=ssE4[tt][:]), rd=[b_ssE4[tt]], wr=[b_rsE4[tt]])
                for tt in range(nt_):
                    c = c0 + tt
                    kb.op("dve", lambda e: e.scalar_tensor_tensor(out=hnb4[tt][:], in0=hT4[tt][:], scalar=rsE4[tt][:, 0:1],
                                                                  in1=wnf[:], op0=ALU.mult, op1=ALU.mult),
                          rd=[b_hT4[tt], b_rsE4[tt], b_wnf], wr=[b_hnb4[tt]])
                    kb.dma("sp", lambda q: q.dma_start(out=hn_d[c * 128:(c + 1) * 128, :], in_=hnb4[tt][:]),
                           rd=[b_hnb4[tt]], wr=[hnrow])

            def stage_R(ti):
                t0, n = TBS[ti]
                nt_ = n // 128
                c0 = t0 // 128
                prs = []
                for tt in range(nt_):
                    pt, pb = bank()
                    ptb = pt[:].bitcast(BF16)
                    kb.op("pe", lambda e: [e.transpose(ptb[:, k * 128:(k + 1) * 128], hnb4[tt][:, k * 128:(k + 1) * 128], identb)
                                           for k in range(8)], rd=[b_hnb4[tt], b_cbf], wr=[pb])
                    kb.op("act", lambda e: e.activation(out=hnT4[tt][:], in_=ptb.rearrange("p (k t) -> p k t", k=8), func=AF.Copy),
                          rd=[pb], wr=[b_hnT4[tt]])
                for tt in range(nt_):
                    pr, prb = bank()
                    prs.append((pr, prb))
                    kb.op("pe", lambda e: [e.matmul(pr[:, 0:NE], hnT4[tt][:, kc, :], wr_[:, kc, :], start=(kc == 0), stop=(kc == 7))
                                           for kc in range(8)], rd=[b_hnT4[tt], b_wr], wr=[prb])
                for tt in range(nt_):
                    pr, prb = prs[tt]
                    kb.op("dve", lambda e: e.reduce_max(out=mx4[tt][:], in_=pr[:, 0:NE], axis=AX.X), rd=[prb], wr=[b_mx4[tt]])
                for tt in range(nt_):
                    kb.op("dve", lambda e: e.tensor_scalar(out=mx4[tt][:], in0=mx4[tt][:], scalar1=-1.0, scalar2=None, op0=ALU.mult),
                          rd=[b_mx4[tt]], wr=[b_mx4[tt]])
                for tt in range(nt_):
                    pr, prb = prs[tt]
                    kb.op("act", lambda e: e.activation(out=lg4[tt][:], in_=pr[:, 0:NE], func=AF.Exp, bias=mx4[tt][:, 0:1],
                                                        accum_out=sme4[tt][:]), rd=[prb, b_mx4[tt]], wr=[b_lg4[tt], b_sme4[tt]])
                for tt in range(nt_):
                    kb.op("dve", lambda e: e.reciprocal(out=sme4[tt][:], in_=sme4[tt][:]), rd=[b_sme4[tt]], wr=[b_sme4[tt]])
                for tt in range(nt_):
                    c = c0 + tt
                    kb.op("dve", lambda e: e.tensor_scalar(out=affTM[:, c, :], in0=lg4[tt][:], scalar1=sme4[tt][:, 0:1], scalar2=None,
                                                           op0=ALU.mult), rd=[b_lg4[tt], b_sme4[tt]], wr=[b_affTM])
                for tt in range(nt_):
                    c = c0 + tt
                    pa_, pab_ = bank()
                    kb.op("pe", lambda e: e.transpose(pa_[0:NE, 0:128], affTM[:, c, :], identf), rd=[b_affTM, b_cf], wr=[pab_])
                    kb.op("act", lambda e: e.activation(out=affT[:, c * 128:(c + 1) * 128], in_=pa_[0:NE, 0:128], func=AF.Copy),
                          rd=[pab_], wr=[b_affT])

            stage_TS(0)
            for ti in range(len(TBS)):
                stage_W(ti)
                if ti + 1 < len(TBS):
                    stage_TS(ti + 1)
                stage_R(ti)

        kb.barrier()
        if debug:
            kb.enabled = "E" in run
            kb.dma("sp", lambda q: q.dma_start(out=aff_dbg, in_=affTM[:]), rd=[b_affTM], wr=[Buf()])
        with ExitStack() as sf:
            kb.enabled = "F" in run
            kb.op("dve", lambda e: e.memset(affT[:, 0:112], 0.0), wr=[b_affT])
            posmTM = sf.enter_context(nc.sbuf_tensor("posmTM", [128, NT, NE], F32))
            RT = sf.enter_context(nc.sbuf_tensor("RT", [128, NT, NE, 4], BF16))
            sr = ExitStack()
            lo = sr.enter_context(nc.sbuf_tensor("lo", [NE, 1], F32))
            hi = sr.enter_context(nc.sbuf_tensor("hi", [NE, 1], F32))
            mid = sr.enter_context(nc.sbuf_tensor("mid", [NE, 1], F32))
            cnt = sr.enter_context(nc.sbuf_tensor("cnt", [NE, 1], F32))
            ge = sr.enter_context(nc.sbuf_tensor("ge", [NE, 1], F32))
            dl = sr.enter_context(nc.sbuf_tensor("dl", [NE, 1], F32))
            jk = sr.enter_context(nc.sbuf_tensor("jk", [NE, T], BF16))
            maskT = sr.enter_context(nc.sbuf_tensor("maskT", [NE, T], F32))
            csum = sr.enter_context(nc.sbuf_tensor("csum", [NE, T], F32))
            onesT = sr.enter_context(nc.sbuf_tensor("onesT", [NE, T], F32))
            ahi = sr.enter_context(nc.sbuf_tensor("ahi", [128, NT, NE], BF16))
            alo = sr.enter_context(nc.sbuf_tensor("alo", [128, NT, NE], F32))
            b_r = Buf()
            b_posm = Buf()
            b_RT = Buf()
            kb.op("dve", lambda e: e.memset(lo[:], 0.0), wr=[b_r])
            kb.op("dve", lambda e: e.memset(hi[:], 1.0), wr=[b_r])
            kb.op("dve", lambda e: e.memset(onesT[:], 1.0), wr=[b_r])
            for itn in range(28):
                kb.op("dve", lambda e: e.tensor_tensor(out=mid[:], in0=lo[:], in1=hi[:], op=ALU.add), rd=[b_r], wr=[b_r])
                kb.op("dve", lambda e: e.tensor_scalar(out=mid[:], in0=mid[:], scalar1=0.5, scalar2=None, op0=ALU.mult),
                      rd=[b_r], wr=[b_r])
                kb.op("dve", lambda e: e.tensor_scalar(out=jk[:], in0=affT[:], scalar1=mid[:, 0:1], scalar2=None,
                                                       op0=ALU.is_gt, op1=ALU.add, accum_out=cnt[:]),
                      rd=[b_r, b_affT], wr=[b_r])
                kb.op("dve", lambda e: e.tensor_scalar(out=ge[:], in0=cnt[:], scalar1=float(CAP) - 0.5, scalar2=None,
                                                       op0=ALU.is_gt), rd=[b_r], wr=[b_r])
                kb.op("dve", lambda e: e.tensor_tensor(out=dl[:], in0=mid[:], in1=lo[:], op=ALU.subtract), rd=[b_r], wr=[b_r])
                kb.op("dve", lambda e: e.tensor_tensor(out=dl[:], in0=dl[:], in1=ge[:], op=ALU.mult), rd=[b_r], wr=[b_r])
                kb.op("dve", lambda e: e.tensor_tensor(out=lo[:], in0=lo[:], in1=dl[:], op=ALU.add), rd=[b_r], wr=[b_r])
                kb.op("dve", lambda e: e.tensor_tensor(out=dl[:], in0=hi[:], in1=mid[:], op=ALU.subtract), rd=[b_r], wr=[b_r])
                kb.op("dve", lambda e: e.tensor_tensor(out=dl[:], in0=dl[:], in1=ge[:], op=ALU.mult), rd=[b_r], wr=[b_r])
                kb.op("dve", lambda e: e.tensor_tensor(out=hi[:], in0=mid[:], in1=dl[:], op=ALU.add), rd=[b_r], wr=[b_r])
            kb.op("dve", lambda e: e.tensor_scalar(out=maskT[:], in0=affT[:], scalar1=lo[:, 0:1], scalar2=None,
                                                   op0=ALU.is_gt), rd=[b_r, b_affT], wr=[b_r])
            kb.op("dve", lambda e: e.tensor_tensor_scan(out=csum[:], data0=onesT[:], data1=maskT[:], initial=0.0,
                                                        op0=ALU.mult, op1=ALU.add), rd=[b_r], wr=[b_r])
            kb.op("dve", lambda e: e.tensor_tensor(out=csum[:], in0=csum[:], in1=maskT[:], op=ALU.mult), rd=[b_r], wr=[b_r])
            kb.op("dve", lambda e: e.tensor_scalar(out=csum[:], in0=csum[:], scalar1=-1.0, scalar2=None, op0=ALU.add),
                  rd=[b_r], wr=[b_r])
            for c in range(NT):
                pp, ppb = bank()
                kb.op("pe", lambda e: e.transpose(pp[:, 0:NE], csum[:, c * 128:(c + 1) * 128], identf[0:NE, 0:NE]),
                      rd=[b_r, b_cf], wr=[ppb])
                kb.op("act", lambda e: e.activation(out=posmTM[:, c, :], in_=pp[:, 0:NE], func=AF.Copy),
                      rd=[ppb], wr=[b_posm])
            kb.op("dve", lambda e: e.tensor_copy(out=ahi[:], in_=affTM[:]), rd=[b_affTM], wr=[b_RT])
            kb.op("dve", lambda e: e.tensor_tensor(out=alo[:], in0=affTM[:], in1=ahi[:], op=ALU.subtract),
                  rd=[b_affTM], wr=[b_RT])
            kb.op("dve", lambda e: e.tensor_copy(out=RT[:, :, :, 2], in_=ahi[:]), wr=[b_RT])
            kb.op("dve", lambda e: e.tensor_copy(out=RT[:, :, :, 3], in_=alo[:]), wr=[b_RT])
            tv = cbf[:, CB_TV:CB_TV + 66].rearrange("p (t two) -> p t two", two=2)
            kb.op("dve", lambda e: e.tensor_copy(out=RT[:, :, :, 0], in_=tv[:, :, 0:1].broadcast_to([128, NT, NE])),
                  rd=[b_cbf], wr=[b_RT])
            kb.op("dve", lambda e: e.tensor_copy(out=RT[:, :, :, 1], in_=tv[:, :, 1:2].broadcast_to([128, NT, NE])),
                  rd=[b_cbf], wr=[b_RT])

            kb.barrier()
            sr.close()

            if debug:
                kb.enabled = "F" in run
                kb.dma("sp", lambda q: q.dma_start(out=posm_dbg, in_=posmTM[:]), rd=[b_posm], wr=[Buf()])
            kb.enabled = "G" in run
            NSLOT = 8
            wring = [sf.enter_context(nc.sbuf_tensor("wring%d" % i, [128, 4096], BF16)) for i in range(NSLOT)]
            b_wring = [Buf() for _ in range(NSLOT)]
            units = []
            for e_ in range(NE):
                for q4 in range(4):
                    units.append(("g", e_, q4))
                    units.append(("u", e_, q4))
                for q4 in range(4):
                    units.append(("d", e_, q4))
            slot_of = {}

            def issue_unit(ui):
                kind, e_, q4 = units[ui]
                s_ = ui % NSLOT
                slot_of[(kind, e_, q4)] = s_
                if not kb.enabled or os.environ.get("GNOW") == "1":
                    return
                if kind in ("g", "u"):
                    src = (w_eg if kind == "g" else w_eu)[e_].rearrange("(kc p) n -> p kc n", p=128)[:, :, q4 * 512:(q4 + 1) * 512]
                    dst = wring[s_][:].rearrange("p (kc n) -> p kc n", kc=8)
                else:
                    src = w_ed[e_].rearrange("(fc p) n -> p fc n", p=128)[:, q4 * 4:(q4 + 1) * 4, :]
                    dst = wring[s_][:].rearrange("p (fc n) -> p fc n", fc=4)
                MAXOUT = int(os.environ.get("MAXOUT", 2))
                if len(unit_toks) >= MAXOUT:
                    kb.wait("pool", [unit_toks[-MAXOUT]])
                unit_toks.append(kb.dma("pool", lambda q: q.dma_start(out=dst, in_=src), wr=[b_wring[s_]]))

            unit_toks = []
            PREF = 6
            nissued = [0]

            def ensure(ui):
                while nissued[0] <= min(ui + PREF, len(units) - 1):
                    issue_unit(nissued[0])
                    nissued[0] += 1

            sel = [sf.enter_context(nc.sbuf_tensor("sel%d" % i, [128, 516], BF16)) for i in range(3)]
            b_sel = [Buf() for _ in range(3)]
            iq = sf.enter_context(nc.sbuf_tensor("iq", [4, 516], F32))
            b_iq = Buf()
            iqT = sf.enter_context(nc.sbuf_tensor("iqT", [128, 5, 4], F32))
            b_iqT = Buf()
            idxf = sf.enter_context(nc.sbuf_tensor("idxf", [128, 5], F32))
            idxi = [sf.enter_context(nc.sbuf_tensor("idxi%d" % i, [128, 5], I32)) for i in range(2)]
            afs = [sf.enter_context(nc.sbuf_tensor("afs%d" % i, [128, 5], F32)) for i in range(2)]
            b_idx = [Buf(), Buf()]
            xg2 = [sf.enter_context(nc.sbuf_tensor("xg%d" % i, [128, 5, D], BF16)) for i in range(2)]
            b_xg2 = [Buf(), Buf()]
            xgT2 = [sf.enter_context(nc.sbuf_tensor("xgT%d" % i, [128, 8, 640], BF16)) for i in range(2)]
            b_xgT2 = [Buf(), Buf()]
            hTe = sf.enter_context(nc.sbuf_tensor("hTe", [128, 16, 640], BF16))
            b_hTe = Buf()
            sgl = [sf.enter_context(nc.sbuf_tensor("sgl%d" % i, [128, 257], BF16)) for i in range(2)]
            b_sgl = [Buf(), Buf()]
            yw = [sf.enter_context(nc.sbuf_tensor("yw%d" % i, [128, D], F32)) for i in range(2)]
            b_yw = [Buf(), Buf()]
            hg = [sf.enter_context(nc.sbuf_tensor("hg%d" % i, [128, D], F32)) for i in range(2)]
            b_hg = [Buf(), Buf()]
            for i in range(2):
                kb.op("pool", lambda e: e.memset(xg2[i][:], 0.0), wr=[b_xg2[i]])
            iota = cf[:, C_IOTA:C_IOTA + 516]
            uic = [0]
            ityc = [0]

            def prepA(e_):
                ip = e_ % 2
                pi1, pi1b = bank()
                pi2, pi2b = bank()
                for c in range(NT):
                    s3 = c % 3
                    kb.op("dve", lambda e: e.tensor_scalar(out=sel[s3][:], in0=iota, scalar1=posmTM[:, c, e_:e_ + 1],
                                                           scalar2=None, op0=ALU.is_equal),
                          rd=[b_cf, b_posm], wr=[b_sel[s3]])
                    kb.op("pe", lambda e: [e.matmul(pi1[0:4, :], RT[:, c, e_, :], sel[s3][:, 0:512],
                                                    start=(c == 0), stop=(c == NT - 1)),
                                           e.matmul(pi2[0:4, 0:4], RT[:, c, e_, :], sel[s3][:, 512:516],
                                                    start=(c == 0), stop=(c == NT - 1))],
                          rd=[b_RT, b_sel[s3]], wr=[pi1b, pi2b])
                kb.op("act", lambda e: e.activation(out=iq[:, 0:512], in_=pi1[0:4, :], func=AF.Copy), rd=[pi1b], wr=[b_iq])
                kb.op("act", lambda e: e.activation(out=iq[:, 512:516], in_=pi2[0:4, 0:4], func=AF.Copy), rd=[pi2b], wr=[b_iq])
                pq, pqb = bank()
                kb.op("pe", lambda e: ([e.transpose(pq[:, jb * 4:(jb + 1) * 4], iq[:, jb * 128:(jb + 1) * 128],
                                                    identf[0:4, 0:4]) for jb in range(4)]
                                       + [e.transpose(pq[0:4, 16:20], iq[:, 512:516], identf[0:4, 0:4])]),
                      rd=[b_iq, b_cf], wr=[pqb])
                kb.op("dve", lambda e: e.memset(iqT[:], 0.0), wr=[b_iqT])
                kb.op("act", lambda e: e.activation(out=iqT[:, 0:4, :], in_=pq[:, 0:16].rearrange("p (j f) -> p j f", f=4),
                                                    func=AF.Copy), rd=[pqb], wr=[b_iqT])
                kb.op("act", lambda e: e.activation(out=iqT[0:4, 4, :], in_=pq[0:4, 16:20], func=AF.Copy),
                      rd=[pqb], wr=[b_iqT])
                kb.op("dve", lambda e: e.scalar_tensor_tensor(out=idxf[:], in0=iqT[:, :, 0], scalar=128.0, in1=iqT[:, :, 1],
                                                              op0=ALU.mult, op1=ALU.add), rd=[b_iqT], wr=[b_idx[ip]])
                kb.op("dve", lambda e: e.tensor_copy(out=idxi[ip][:], in_=idxf[:]), wr=[b_idx[ip]])
                kb.op("dve", lambda e: e.tensor_tensor(out=afs[ip][:], in0=iqT[:, :, 2], in1=iqT[:, :, 3], op=ALU.add),
                      rd=[b_iqT], wr=[b_idx[ip]])
                for jb in range(5):
                    M = 128 if jb < 4 else 2
                    kb.dma("pool", lambda q: q.indirect_dma_start(
                        out=xg2[ip][0:M, jb, :], out_offset=None, in_=hn_d[:, :],
                        in_offset=bass.IndirectOffsetOnAxis(ap=idxi[ip][0:M, jb:jb + 1], axis=0)),
                        rd=[b_idx[ip], hnrow], wr=[b_xg2[ip]])

            def prepB(e_):
                ip = e_ % 2
                for jb in range(5):
                    pt, pb = bank()
                    ptb = pt[:].bitcast(BF16)
                    kb.op("pe", lambda e: [e.transpose(ptb[:, kk * 128:(kk + 1) * 128], xg2[ip][:, jb, kk * 128:(kk + 1) * 128], identb)
                                           for kk in range(8)], rd=[b_xg2[ip], b_cbf], wr=[pb])
                    kb.op("act", lambda e: e.activation(out=xgT2[ip][:, :, jb * 128:(jb + 1) * 128],
                                                        in_=ptb.rearrange("p (k t) -> p k t", k=8), func=AF.Copy),
                          rd=[pb], wr=[b_xgT2[ip]])

            def gateup(e_, q4s):
                ip = e_ % 2
                xgT, b_xgT = xgT2[ip], b_xgT2[ip]
                for q4 in q4s:
                    ensure(uic[0])
                    sg_ = slot_of[("g", e_, q4)]
                    su_ = slot_of[("u", e_, q4)]
                    wgv = wring[sg_][:].rearrange("p (kc n) -> p kc n", kc=8)
                    wuv = wring[su_][:].rearrange("p (kc n) -> p kc n", kc=8)
                    for f4 in range(4):
                        fc = q4 * 4 + f4
                        for hf in range(2):
                            c0 = hf * 257
                            pgk, pgb = bank()
                            puk, pub = bank()
                            kb.op("pe", lambda e: [e.matmul(pgk[:, 0:257], wgv[:, kc, f4 * 128:(f4 + 1) * 128],
                                                            xgT[:, kc, c0:c0 + 257], start=(kc == 0), stop=(kc == 7))
                                                   for kc in range(8)], rd=[b_wring[sg_], b_xgT], wr=[pgb])
                            kb.op("pe", lambda e: [e.matmul(puk[:, 0:257], wuv[:, kc, f4 * 128:(f4 + 1) * 128],
                                                            xgT[:, kc, c0:c0 + 257], start=(kc == 0), stop=(kc == 7))
                                                   for kc in range(8)], rd=[b_wring[su_], b_xgT], wr=[pub])
                            qy = ityc[0] % 2
                            ityc[0] += 1
                            kb.op("act", lambda e: e.activation(out=sgl[qy][:], in_=pgk[:, 0:257], func=AF.Silu),
                                  rd=[pgb], wr=[b_sgl[qy]])
                            kb.op("dve", lambda e: e.tensor_tensor(out=hTe[:, fc, c0:c0 + 257], in0=puk[:, 0:257],
                                                                   in1=sgl[qy][:], op=ALU.mult),
                                  rd=[pub, b_sgl[qy]], wr=[b_hTe])
                    uic[0] += 2
                    ensure(uic[0])

            def down(e_):
                ip = e_ % 2
                sd_ = [slot_of[("d", e_, q4)] for q4 in range(4)]
                for jb in range(5):
                    M = 128 if jb < 4 else 2
                    qy = ityc[0] % 2
                    ityc[0] += 1
                    kb.dma("pool", lambda q: q.indirect_dma_start(
                        out=hg[qy][0:M, :], out_offset=None, in_=hacc_d[:, :],
                        in_offset=bass.IndirectOffsetOnAxis(ap=idxi[ip][0:M, jb:jb + 1], axis=0)),
                        rd=[b_idx[ip], hacc], wr=[b_hg[qy]])
                    for dh in range(2):
                        pdn, pdnb = bank()
                        kb.op("pe", lambda e: [e.matmul(pdn[0:M, :], hTe[:, fc, jb * 128:jb * 128 + M],
                                                        wring[sd_[fc // 4]][:].rearrange("p (f n) -> p f n", f=4)[:, fc % 4, dh * 512:(dh + 1) * 512],
                                                        start=(fc == 0), stop=(fc == 15)) for fc in range(16)],
                              rd=[b_wring[s_] for s_ in sd_] + [b_hTe], wr=[pdnb])
                        kb.op("dve", lambda e: e.scalar_tensor_tensor(
                            out=yw[qy][0:M, dh * 512:(dh + 1) * 512], in0=pdn[0:M, :], scalar=afs[ip][0:M, jb:jb + 1],
                            in1=hg[qy][0:M, dh * 512:(dh + 1) * 512], op0=ALU.mult, op1=ALU.add),
                            rd=[pdnb, b_idx[ip], b_hg[qy]], wr=[b_yw[qy]])
                    kb.dma("pool", lambda q: q.indirect_dma_start(
                        out=hacc_d[:, :], out_offset=bass.IndirectOffsetOnAxis(ap=idxi[ip][0:M, jb:jb + 1], axis=0),
                        in_=yw[qy][0:M, :], in_offset=None),
                        rd=[b_yw[qy], b_idx[ip]], wr=[hacc])
                uic[0] += 4

            ensure(0)
            prepA(0)
            prepB(0)
            for e_ in range(NE):
                gateup(e_, [0, 1])
                if e_ + 1 < NE:
                    prepA(e_ + 1)
                gateup(e_, [2, 3])
                if e_ + 1 < NE:
                    prepB(e_ + 1)
                down(e_)

        kb.barrier()
        with ExitStack() as sh:
            kb.enabled = "H" in run
            wfin = sb("wfin", [128, D], F32, sh)
            b_wfin = Buf()
            kb.dma("sp", lambda q: q.dma_start(out=wfin[:], in_=bc_d[:, B_WFIN:B_WFIN + D]), wr=[b_wfin])
            hl = [sb("hl%d" % i, [128, D], F32, sh) for i in range(4)]
            b_hl = [Buf() for _ in range(4)]
            ol = [sb("ol%d" % i, [128, D], F32, sh) for i in range(4)]
            b_ol = [Buf() for _ in range(4)]
            jf = [sb("jf%d" % i, [128, D], BF16, sh) for i in range(2)]
            b_jf = [Buf(), Buf()]
            s1_ = [sb("s1_%d" % i, [128, 1], F32, sh) for i in range(4)]
            r1_ = [sb("r1_%d" % i, [128, 1], F32, sh) for i in range(4)]
            b_s1 = [Buf() for _ in range(4)]
            b_r1 = [Buf() for _ in range(4)]
            outb = Buf()
            for c0 in range(1, NT, 4):
                cs_ = list(range(c0, min(c0 + 4, NT)))
                for i, c in enumerate(cs_):
                    kb.dma("sp", lambda q: q.dma_start(out=hl[i][:], in_=hacc_d[c * 128:(c + 1) * 128, :]), rd=[hacc], wr=[b_hl[i]])
                for i, c in enumerate(cs_):
                    kb.op("act", lambda e: e.activation(out=jf[i % 2][:], in_=hl[i][:], func=AF.Square, accum_out=s1_[i][:]),
                          rd=[b_hl[i]], wr=[b_jf[i % 2], b_s1[i]])
                for i, c in enumerate(cs_):
                    kb.op("act", lambda e: e.activation(out=s1_[i][:], in_=s1_[i][:], func=AF.Sqrt, bias=EPS, scale=1.0 / D),
                          rd=[b_s1[i]], wr=[b_s1[i]])
                for i, c in enumerate(cs_):
                    kb.op("dve", lambda e: e.reciprocal(out=r1_[i][:], in_=s1_[i][:]), rd=[b_s1[i]], wr=[b_r1[i]])
                for i, c in enumerate(cs_):
                    kb.op("dve" if i % 2 == 0 else "pool", lambda e: e.scalar_tensor_tensor(out=ol[i][:], in0=hl[i][:], scalar=r1_[i][:, 0:1], in1=wfin[:],
                                                                  op0=ALU.mult, op1=ALU.mult),
                          rd=[b_hl[i], b_r1[i], b_wfin], wr=[b_ol[i]]) if False else \
                        kb.op("dve", lambda e: e.scalar_tensor_tensor(out=ol[i][:], in0=hl[i][:], scalar=r1_[i][:, 0:1], in1=wfin[:],
                                                                      op0=ALU.mult, op1=ALU.mult),
                              rd=[b_hl[i], b_r1[i], b_wfin], wr=[b_ol[i]])
                    kb.dma("sp", lambda q: q.dma_start(out=out_d[(c - 1) * 128:c * 128, :], in_=ol[i][:]), rd=[b_ol[i]], wr=[outb])
            kb.enabled = True
            kb.barrier()
    return nc


def _host_consts():
    l = np.arange(128)
    Uf = (l[:, None] <= l[None, :]).astype(np.float32)
    Lf = (l[:, None] > l[None, :]).astype(np.float32)
    Ub = (l[:, None] >= l[None, :]).astype(np.float32)
    Lb = (l[:, None] < l[None, :]).astype(np.float32)
    cf = np.zeros((128, C_N), np.float32)
    cf[:, C_UF:C_UF + 128] = Uf
    cf[:, C_LF:C_LF + 128] = Lf
    cf[:, C_UB:C_UB + 128] = Ub
    cf[:, C_LB:C_LB + 128] = Lb
    cf[:, C_ONES:C_ONES + 128] = 1.0
    cf[:, C_ID:C_ID + 128] = np.eye(128, dtype=np.float32)
    cf[:, C_IOTA:C_IOTA + 516] = np.arange(516, dtype=np.float32)[None, :]
    cb = np.zeros((128, CB_N), np.float32)
    cb[:, CB_ID:CB_ID + 128] = np.eye(128)
    cb[:, CB_ONES:CB_ONES + 128] = 1.0
    tv = np.zeros((128, NT, 2), np.float32)
    tv[:, :, 0] = np.arange(NT)[None, :]
    tv[:, :, 1] = np.arange(128)[:, None]
    cb[:, CB_TV:CB_TV + 66] = tv.reshape(128, 66)
    return cf, cb.astype(ml_dtypes.bfloat16)


_NC_CACHE = {}


def kernel(x, meta_tokens, w_norm_mix, w_in, w_conf_dw, b_conf_dw, conf_ln_g, conf_ln_b, w_conf_out,
           w_ssm_conv, b_ssm_conv, ssm_dt_bias, ssm_a_log, ssm_d, w_ssm_norm, w_ssm_out, w_out,
           w_norm_ffn, w_router, w_exp_gate, w_exp_up, w_exp_down, w_norm_final):
    f = lambda a: np.ascontiguousarray(np.asarray(a, dtype=np.float32))
    x = f(x)
    small = np.zeros((128, S_N), np.float32)
    small[:, S_BCONF:S_BCONF + 8] = f(b_conf_dw)[0].reshape(8, 128).T
    small[:, S_LNG:S_LNG + 8] = f(conf_ln_g)[0].reshape(8, 128).T
    small[:, S_LNB:S_LNB + 8] = f(conf_ln_b)[0].reshape(8, 128).T
    small[:, S_WCONF:S_WCONF + 248] = f(w_conf_dw)[0].T.reshape(8, 128, 31).transpose(1, 0, 2).reshape(128, 248)
    small[:, S_WSSM:S_WSSM + 168] = f(w_ssm_conv)[0].T.reshape(24, 128, 7).transpose(1, 0, 2).reshape(128, 168)
    small[:, S_BSSM:S_BSSM + 24] = f(b_ssm_conv)[0].reshape(24, 128).T
    row = np.concatenate([f(w_norm_mix)[0], f(w_ssm_norm)[0], f(w_norm_ffn)[0], f(w_norm_final),
                          f(ssm_dt_bias)[0].reshape(64), f(ssm_a_log)[0].reshape(64), f(ssm_d)[0]])
    bcast = np.ascontiguousarray(np.broadcast_to(row[None, :], (128, B_N)))
    cf, cb = _host_consts()
    if "nc" not in _NC_CACHE:
        _NC_CACHE["nc"] = build_program()
    nc = _NC_CACHE["nc"]
    shared = {
        "meta": f(meta_tokens), "w_in": f(w_in)[0], "w_conf_out": f(w_conf_out)[0], "w_ssm_out": f(w_ssm_out)[0],
        "w_out": f(w_out)[0], "w_router": f(w_router)[0], "w_eg": f(w_exp_gate)[0], "w_eu": f(w_exp_up)[0],
        "w_ed": f(w_exp_down)[0], "smallT": small, "bcast": bcast, "cf32": cf, "cbf": cb,
    }
    in_maps = [dict(shared, x=x[b]) for b in range(8)]
    res = run_bass_kernel_spmd(nc, in_maps, core_ids=list(range(8)))
    return np.stack([np.asarray(r["out"], dtype=np.float32) for r in res.results], axis=0)
```

```python
import os
import numpy as np
import ml_dtypes
from contextlib import ExitStack
import concourse.bass as bass
import concourse.mybir as mybir
from concourse.bass_utils import run_bass_kernel_spmd

F32 = mybir.dt.float32
F32R = mybir.dt.float32r
BF16 = mybir.dt.bfloat16
I32 = mybir.dt.int32
AF = mybir.ActivationFunctionType
ALU = mybir.AluOpType
AX = mybir.AxisListType

T = 4224
NT = 33
D = 1024
CAP = 514
NE = 16
TBS = [(i * 512, 512) for i in range(8)] + [(4096, 128)]
EPS = 1e-6
S_BCONF, S_LNG, S_LNB, S_WCONF, S_WSSM, S_BSSM, S_N = 0, 8, 16, 24, 272, 440, 464
B_WMIX, B_WSSM, B_WFFN, B_WFIN, B_DTB, B_ALOG, B_DSK, B_N = 0, 1024, 3072, 4096, 5120, 5184, 5248, 5280
C_UF, C_LF, C_UB, C_LB, C_ONES, C_ID, C_IOTA, C_N = 0, 128, 256, 384, 512, 640, 768, 768 + 516
CB_ID, CB_ONES, CB_TV, CB_N = 0, 128, 256, 256 + 66


class Buf:
    __slots__ = ("w", "r")

    def __init__(self):
        self.w = None
        self.r = {}


class KB:
    EPOCH = 2048

    def __init__(self, nc, st):
        self.nc = nc
        self.st = st
        self.eng = dict(pe=nc.tensor, act=nc.scalar, dve=nc.vector, pool=nc.gpsimd, sp=nc.sync)
        self.cnt = {e: 0 for e in self.eng}
        self.sems = {e: [] for e in self.eng}
        self.known = {e: {} for e in self.eng}
        self.semh = []
        self.origin = []
        self.dq = {}
        self.enabled = True

    def newsem(self, origin):
        h = self.st.enter_context(self.nc.semaphore("s%d" % len(self.semh)))
        self.semh.append(h)
        self.origin.append(origin)
        return len(self.semh) - 1

    def wait(self, e, toks, keep_one=False):
        need = {}
        kn = self.known[e]
        for sid, val in toks:
            if e == "pe" and self.origin[sid] == "pe":
                continue
            if kn.get(sid, 0) < val and need.get(sid, 0) < val:
                need[sid] = val
        items = list(need.items())
        attach = items.pop() if (keep_one and items) else None
        for sid, val in items:
            self.eng[e].wait_ge(self.semh[sid], val)
            kn[sid] = val
        if attach is not None:
            kn[attach[0]] = attach[1]
        return attach

    def _deps(self, rd, wr):
        toks = []
        for b in rd:
            if b.w is not None:
                toks.append(b.w)
        for b in wr:
            if b.w is not None:
                toks.append(b.w)
            toks.extend(b.r.items())
        return toks

    def _mark(self, tok, rd, wr):
        sid, val = tok
        for b in rd:
            if b.r.get(sid, 0) < val:
                b.r[sid] = val
        for b in wr:
            b.w = tok
            b.r = {}

    def op(self, e, fn, rd=(), wr=()):
        if not self.enabled:
            return None
        attach = self.wait(e, self._deps(rd, wr), keep_one=True)
        ins = fn(self.eng[e])
        if isinstance(ins, (list, tuple)):
            first, ins = ins[0], ins[-1]
        else:
            first = ins
        if attach is not None:
            first.wait_op(self.semh[attach[0]], attach[1], "sem-ge")
        self.cnt[e] += 1
        k = self.cnt[e]
        ep = (k - 1) // self.EPOCH
        while len(self.sems[e]) <= ep:
            self.sems[e].append(self.newsem(e))
        sid = self.sems[e][ep]
        val = (k - 1) % self.EPOCH + 1
        ins.then_inc(self.semh[sid], 1)
        tok = (sid, val)
        self._mark(tok, rd, wr)
        return tok

    def barrier(self):
        toks = []
        for e in self.eng:
            if self.cnt[e] > 0:
                k = self.cnt[e]
                toks.append((self.sems[e][(k - 1) // self.EPOCH], (k - 1) % self.EPOCH + 1))
        for pool in self.dq.values():
            toks.extend((sid, val) for sid, val in zip(pool["sids"], pool["vals"]) if val > 0)
        for e in self.eng:
            self.wait(e, toks)

    def dma(self, q, fn, rd=(), wr=(), nslots=16):
        if not self.enabled:
            return None
        self.wait(q, self._deps(rd, wr))
        pool = self.dq.setdefault(q, dict(sids=[], vals=[], n=0))
        i = pool["n"] % nslots
        pool["n"] += 1
        if len(pool["sids"]) <= i:
            pool["sids"].append(self.newsem("dma"))
            pool["vals"].append(0)
        sid = pool["sids"][i]
        if pool["vals"][i] > 0:
            self.wait(q, [(sid, pool["vals"][i])])
        ins = fn(self.eng[q])
        val = pool["vals"][i] + 16
        pool["vals"][i] = val
        ins.then_inc(self.semh[sid], 16)
        tok = (sid, val)
        self._mark(tok, rd, wr)
        return tok


def build_program(run="ABbCDEFGH", debug=False):
    nc = bass.Bass("TRN2", target_bir_lowering=False)
    dt_ = nc.dram_tensor
    x_d = dt_("x", [4096, D], F32, kind="ExternalInput").ap()
    meta_d = dt_("meta", [16, D], F32, kind="ExternalInput").ap()
    w_in = dt_("w_in", [D, 9280], F32, kind="ExternalInput").ap()
    w_co = dt_("w_conf_out", [D, D], F32, kind="ExternalInput").ap()
    w_so = dt_("w_ssm_out", [2048, D], F32, kind="ExternalInput").ap()
    w_o = dt_("w_out", [D, D], F32, kind="ExternalInput").ap()
    w_r = dt_("w_router", [D, NE], F32, kind="ExternalInput").ap()
    ned = NE if ("G" in run or os.environ.get("FORCE_NE") == "1") else 1
    w_eg = dt_("w_eg", [ned, D, 2048], F32, kind="ExternalInput").ap()
    w_eu = dt_("w_eu", [ned, D, 2048], F32, kind="ExternalInput").ap()
    w_ed = dt_("w_ed", [ned, 2048, D], F32, kind="ExternalInput").ap()
    small_d = dt_("smallT", [128, S_N], F32, kind="ExternalInput").ap()
    bc_d = dt_("bcast", [128, B_N], F32, kind="ExternalInput").ap()
    cf_d = dt_("cf32", [128, C_N], F32, kind="ExternalInput").ap()
    cb_d = dt_("cbf", [128, CB_N], BF16, kind="ExternalInput").ap()
    out_d = dt_("out", [4096, D], F32, kind="ExternalOutput").ap()
    skw = dict(kind="ExternalOutput") if debug else {}
    conv_d = dt_("conv_s", [128, 8, T], BF16, **skw).ap()
    m1_d = dt_("m1_s", [128, 8, T], BF16, **skw).ap()
    g2_d = dt_("g2_s", [128, 8, T], BF16, **skw).ap()
    xbc_d = dt_("xbc_s", [128, 24, T], BF16, **skw).ap()
    sz_d = dt_("sz_s", [NT, 128, 2048], BF16, **skw).ap()
    yb_d = dt_("yb_s", [NT, 128, 2048], BF16, **skw).ap()
    yn_d = dt_("yn_s", [NT, 128, 2048], BF16, **skw).ap()
    hn_d = dt_("hn_s", [T, D], BF16, **skw).ap()
    hacc_d = dt_("hacc_s", [T, D], F32, **skw).ap()
    uT_dbg = dt_("uT_s", [128, 8, T], BF16, **skw).ap() if debug else None
    dt_dbg = dt_("dt_s", [128, NT, 64], F32, **skw).ap() if debug else None
    aff_dbg = dt_("aff_s", [128, NT, NE], F32, **skw).ap() if debug else None
    posm_dbg = dt_("posm_s", [128, NT, NE], F32, **skw).ap() if debug else None

    win_v = w_in.rearrange("(kc p) n -> p kc n", p=128)

    with ExitStack() as st:
        kb = KB(nc, st)

        def sb(name, shape, dtype, stack=None):
            return (stack or st).enter_context(nc.sbuf_tensor("sb_" + name, shape, dtype))

        ps = [st.enter_context(nc.psum_tensor("ps%d" % i, [128, 512], F32)) for i in range(8)]
        psb = [Buf() for _ in range(8)]
        psn = [0]

        def bank():
            i = psn[0] % 8
            psn[0] += 1
            return ps[i], psb[i]

        cf = sb("cf", [128, C_N], F32)
        cbf = sb("cbf", [128, CB_N], BF16)
        sm = sb("sm", [128, S_N], F32)
        b_cf, b_cbf, b_sm = Buf(), Buf(), Buf()
        kb.dma("sp", lambda q: q.dma_start(out=cf[:], in_=cf_d), wr=[b_cf])
        kb.dma("sp", lambda q: q.dma_start(out=cbf[:], in_=cb_d), wr=[b_cbf])
        kb.dma("sp", lambda q: q.dma_start(out=sm[:], in_=small_d), wr=[b_sm])
        identb = cbf[:, CB_ID:CB_ID + 128]
        onesb = cbf[:, CB_ONES:CB_ONES + 128]
        identf = cf[:, C_ID:C_ID + 128]

        dtall = sb("dtall", [128, NT, 64], F32)
        b_dt = [Buf() for _ in range(NT)]

        def load_x_tile(i, xin, b_xin):
            if i == 0:
                kb.op("dve", lambda e: e.memset(xin[:], 0.0), wr=[b_xin])
                kb.dma("sp", lambda q: q.dma_start(out=xin[112:128, :], in_=meta_d), wr=[b_xin])
            else:
                kb.dma("sp", lambda q: q.dma_start(out=xin[:], in_=x_d[(i - 1) * 128:i * 128, :]), wr=[b_xin])

        def rms_rstd(src, b_src, junk, b_junk, ss, b_ss, rstd, b_rstd, n):
            kb.op("act", lambda e: e.activation(out=junk, in_=src, func=AF.Square, accum_out=ss[:]),
                  rd=(b_src if isinstance(b_src, list) else [b_src]), wr=[b_junk, b_ss])
            kb.op("act", lambda e: e.activation(out=ss[:], in_=ss[:], func=AF.Sqrt, bias=EPS, scale=1.0 / n),
                  rd=[b_ss], wr=[b_ss])
            kb.op("dve", lambda e: e.reciprocal(out=rstd[:], in_=ss[:]), rd=[b_ss], wr=[b_rstd])

        with ExitStack() as s1:
            uT = sb("uT", [128, 8, T], BF16, s1)
            b_uT = [Buf() for _ in range(NT)]

            def uT_bufs(t0, n):
                return b_uT[t0 // 128:(t0 + n) // 128]

            with ExitStack() as sa:
                kb.enabled = "A" in run
                wmix = sb("wmix", [128, D], F32, sa)
                b_wmix = Buf()
                kb.dma("sp", lambda q: q.dma_start(out=wmix[:], in_=bc_d[:, B_WMIX:B_WMIX + D]), wr=[b_wmix])
                xin = [sb("xinA%d" % i, [128, D], F32, sa) for i in range(4)]
                b_xin = [Buf() for _ in range(4)]
                junk = [sb("junkA%d" % i, [128, D], BF16, sa) for i in range(2)]
                b_junk = [Buf(), Buf()]
                ub = [sb("ubA%d" % i, [128, D], BF16, sa) for i in range(4)]
                b_ub = [Buf() for _ in range(4)]
                ss = [sb("ssA%d" % i, [128, 1], F32, sa) for i in range(4)]
                rs = [sb("rsA%d" % i, [128, 1], F32, sa) for i in range(4)]
                b_ss = [Buf() for _ in range(4)]
                b_rs = [Buf() for _ in range(4)]
                for i0 in range(0, NT, 4):
                    tiles = list(range(i0, min(i0 + 4, NT)))
                    for p, i in enumerate(tiles):
                        load_x_tile(i, xin[p], b_xin[p])
                    for p, i in enumerate(tiles):
                        kb.op("act", lambda e: e.activation(out=junk[p % 2][:], in_=xin[p][:], func=AF.Square, accum_out=ss[p][:]),
                              rd=[b_xin[p]], wr=[b_junk[p % 2], b_ss[p]])
                    for p, i in enumerate(tiles):
                        kb.op("act", lambda e: e.activation(out=ss[p][:], in_=ss[p][:], func=AF.Sqrt, bias=EPS, scale=1.0 / D),
                              rd=[b_ss[p]], wr=[b_ss[p]])
                    for p, i in enumerate(tiles):
                        kb.op("dve", lambda e: e.reciprocal(out=rs[p][:], in_=ss[p][:]), rd=[b_ss[p]], wr=[b_rs[p]])
                    for p, i in enumerate(tiles):
                        kb.op("dve", lambda e: e.scalar_tensor_tensor(out=ub[p][:], in0=xin[p][:], scalar=rs[p][:, 0:1],
                                                                       in1=wmix[:], op0=ALU.mult, op1=ALU.mult),
                              rd=[b_xin[p], b_rs[p], b_wmix], wr=[b_ub[p]])
                    for p, i in enumerate(tiles):
                        pt, pb = bank()
                        ptb = pt[:].bitcast(BF16)
                        kb.op("pe", lambda e: [e.transpose(ptb[:, kc * 128:(kc + 1) * 128],
                                                           ub[p][:, kc * 128:(kc + 1) * 128], identb)
                                               for kc in range(8)],
                              rd=[b_ub[p], b_cbf], wr=[pb])
                        kb.op("act", lambda e: e.activation(out=uT[:, :, i * 128:(i + 1) * 128],
                                                            in_=ptb.rearrange("p (k t) -> p k t", k=8), func=AF.Copy),
                              rd=[pb], wr=[b_uT[i]])

            kb.barrier()
            if debug:
                kb.enabled = "A" in run
                kb.dma("sp", lambda q: q.dma_start(out=uT_dbg, in_=uT[:]), rd=b_uT, wr=[Buf()])
            with ExitStack() as sbk:
                kb.enabled = "B" in run
                wA = [sb("wA%d" % i, [128, 8, 256], BF16, sbk) for i in range(2)]
                b_wA = [Buf(), Buf()]
                cT = [sb("cT%d" % i, [128, T + 30], BF16, sbk) for i in range(2)]
                b_cT = [Buf(), Buf()]
                dg = [sb("dg%d" % i, [128, 31, 128], BF16, sbk) for i in range(2)]
                b_dg = [Buf(), Buf()]
                sgt = [sb("sgt%d" % i, [128, 512], BF16, sbk) for i in range(2)]
                b_sgt = [Buf(), Buf()]
                cv = [sb("cv%d" % i, [128, 512], BF16, sbk) for i in range(2)]
                b_cv = [Buf(), Buf()]
                b_conv = [[Buf() for _ in TBS] for _ in range(8)]
                for i in range(2):
                    kb.op("pool", lambda e: e.memset(cT[i][:], 0.0), wr=[b_cT[i]])
                it = 0
                for j in range(8):
                    p = j % 2
                    kb.dma("pool", lambda q: q.dma_start(out=wA[p][:, :, 0:128], in_=win_v[:, :, j * 128:(j + 1) * 128]),
                           wr=[b_wA[p]])
                    kb.dma("pool", lambda q: q.dma_start(out=wA[p][:, :, 128:256],
                                                         in_=win_v[:, :, 1024 + j * 128:1024 + (j + 1) * 128]),
                           wr=[b_wA[p]])
                    for k in range(31):
                        kb.op("dve", lambda e: e.tensor_scalar(out=dg[p][:, k, :], in0=identb,
                                                               scalar1=sm[:, S_WCONF + j * 31 + k:S_WCONF + j * 31 + k + 1],
                                                               scalar2=None, op0=ALU.mult),
                              rd=[b_cbf, b_sm], wr=[b_dg[p]])
                    for (t0, n) in TBS:
                        pa, pab = bank()
                        pg, pgb = bank()
                        kb.op("pe", lambda e: [e.matmul(pa[:, 0:n], wA[p][:, kc, 0:128], uT[:, kc, t0:t0 + n],
                                                        start=(kc == 0), stop=(kc == 7)) for kc in range(8)],
                              rd=[b_wA[p]] + uT_bufs(t0, n), wr=[pab])
                        kb.op("pe", lambda e: [e.matmul(pg[:, 0:n], wA[p][:, kc, 128:256], uT[:, kc, t0:t0 + n],
                                                        start=(kc == 0), stop=(kc == 7)) for kc in range(8)],
                              rd=[b_wA[p]] + uT_bufs(t0, n), wr=[pgb])
                        q = it % 2
                        it += 1
                        kb.op("act", lambda e: e.activation(out=sgt[q][:, 0:n], in_=pg[:, 0:n], func=AF.Sigmoid),
                              rd=[pgb], wr=[b_sgt[q]])
                        kb.op("dve", lambda e: e.tensor_tensor(out=cT[p][:, 15 + t0:15 + t0 + n], in0=pa[:, 0:n],
                                                               in1=sgt[q][:, 0:n], op=ALU.mult),
                              rd=[pab, b_sgt[q]], wr=[b_cT[p]])
                    for ti, (t0, n) in enumerate(TBS):
                        pc, pcb = bank()
                        kb.op("pe", lambda e: [e.matmul(pc[:, 0:n], dg[p][:, k, :], cT[p][:, t0 + k:t0 + k + n],
                                                        start=(k == 0), stop=(k == 30)) for k in range(31)],
                              rd=[b_dg[p], b_cT[p]], wr=[pcb])
                        q = it % 2
                        it += 1
                        kb.op("act", lambda e: e.activation(out=cv[q][:, 0:n], in_=pc[:, 0:n], func=AF.Identity,
                                                            bias=sm[:, S_BCONF + j:S_BCONF + j + 1]),
                              rd=[pcb, b_sm], wr=[b_cv[q]])
                        kb.dma("sp", lambda qq: qq.dma_start(out=conv_d[:, j, t0:t0 + n], in_=cv[q][:, 0:n]),
                               rd=[b_cv[q]], wr=[b_conv[j][ti]])

            kb.barrier()
            with ExitStack() as sb2:
                kb.enabled = "b" in run
                wco = sb("wco", [128, 8, D], BF16, sb2)
                wg1 = sb("wg1", [128, 8, D], BF16, sb2)
                wg2 = sb("wg2", [128, 8, D], BF16, sb2)
                b_wco, b_wg1, b_wg2 = Buf(), Buf(), Buf()
                kb.dma("pool", lambda q: q.dma_start(out=wco[:], in_=w_co.rearrange("(kc p) n -> p kc n", p=128)),
                       wr=[b_wco])
                kb.dma("pool", lambda q: q.dma_start(out=wg1[:], in_=win_v[:, :, 7232:7232 + D]), wr=[b_wg1])
                kb.dma("pool", lambda q: q.dma_start(out=wg2[:], in_=win_v[:, :, 8256:8256 + D]), wr=[b_wg2])
                cvb = [sb("cvb%d" % i, [128, 8, 512], BF16, sb2) for i in range(2)]
                b_cvb = [Buf(), Buf()]
                sq = sb("sq", [128, 8, 512], BF16, sb2)
                b_sq = Buf()
                mean = sb("mean", [128, 512], F32, sb2)
                msq = sb("msq", [128, 512], F32, sb2)
                rstd = sb("rstdb", [128, 512], F32, sb2)
                nmr = sb("nmr", [128, 512], F32, sb2)
                b_mean, b_msq, b_rstd, b_nmr = Buf(), Buf(), Buf(), Buf()
                t2 = [sb("t2_%d" % i, [128, 512], F32, sb2) for i in range(2)]
                b_t2 = [Buf(), Buf()]
                cs2 = [sb("cs%d" % i, [128, 8, 512], BF16, sb2) for i in range(2)]
                b_cs2 = [Buf(), Buf()]
                sg = [sb("sg%d" % i, [128, 512], BF16, sb2) for i in range(2)]
                b_sg = [Buf(), Buf()]
                m1 = [sb("m1_0", [128, 8, 512], BF16, sb2)] * 2
                b_m1 = [Buf()] * 2
                g2 = [sb("g2_0", [128, 8, 512], BF16, sb2)] * 2
                b_g2 = [Buf()] * 2
                b_m1d = [Buf() for _ in TBS]
                b_g2d = [Buf() for _ in TBS]
                itc = [0]

                def b_LN(ti):
                    t0, n = TBS[ti]
                    it = itc[0]
                    p = ti % 2
                    cs, b_cs = cs2[p], b_cs2[p]
                    kb.dma("sp", lambda q: q.dma_start(out=cvb[p][:, :, 0:n], in_=conv_d[:, :, t0:t0 + n]),
                           rd=[b_conv[j][ti] for j in range(8)], wr=[b_cvb[p]])
                    kb.op("pool", lambda e: e.tensor_tensor(out=sq[:, :, 0:n], in0=cvb[p][:, :, 0:n],
                                                            in1=cvb[p][:, :, 0:n], op=ALU.mult),
                          rd=[b_cvb[p]], wr=[b_sq])
                    p1, p1b = bank()
                    p2, p2b = bank()
                    kb.op("pe", lambda e: [e.matmul(p1[:, 0:n], onesb, cvb[p][:, j, 0:n], start=(j == 0), stop=(j == 7))
                                           for j in range(8)], rd=[b_cbf, b_cvb[p]], wr=[p1b])
                    kb.op("pe", lambda e: [e.matmul(p2[:, 0:n], onesb, sq[:, j, 0:n], start=(j == 0), stop=(j == 7))
                                           for j in range(8)], rd=[b_cbf, b_sq], wr=[p2b])
                    kb.op("dve", lambda e: e.tensor_scalar(out=mean[:, 0:n], in0=p1[:, 0:n], scalar1=1.0 / D,
                                                           scalar2=None, op0=ALU.mult), rd=[p1b], wr=[b_mean])
                    kb.op("dve", lambda e: e.tensor_tensor(out=msq[:, 0:n], in0=mean[:, 0:n], in1=mean[:, 0:n],
                                                           op=ALU.mult), rd=[b_mean], wr=[b_msq])
                    kb.op("dve", lambda e: e.scalar_tensor_tensor(out=msq[:, 0:n], in0=p2[:, 0:n], scalar=1.0 / D,
                                                                  in1=msq[:, 0:n], op0=ALU.mult, op1=ALU.subtract),
                          rd=[p2b, b_msq], wr=[b_msq])
                    kb.op("act", lambda e: e.activation(out=msq[:, 0:n], in_=msq[:, 0:n], func=AF.Sqrt, bias=EPS),
                          rd=[b_msq], wr=[b_msq])
                    kb.op("dve", lambda e: e.reciprocal(out=rstd[:, 0:n], in_=msq[:, 0:n]), rd=[b_msq], wr=[b_rstd])
                    kb.op("dve", lambda e: e.scalar_tensor_tensor(out=nmr[:, 0:n], in0=mean[:, 0:n], scalar=-1.0,
                                                                  in1=rstd[:, 0:n], op0=ALU.mult, op1=ALU.mult),
                          rd=[b_mean, b_rstd], wr=[b_nmr])
                    for j in range(8):
                        q = it % 2
                        it += 1
                        kb.op("dve", lambda e: e.tensor_tensor(out=t2[q][:, 0:n], in0=cvb[p][:, j, 0:n],
                                                               in1=rstd[:, 0:n], op=ALU.mult),
                              rd=[b_cvb[p], b_rstd], wr=[b_t2[q]])
                        kb.op("dve", lambda e: e.tensor_tensor(out=t2[q][:, 0:n], in0=t2[q][:, 0:n],
                                                               in1=nmr[:, 0:n], op=ALU.add),
                              rd=[b_nmr], wr=[b_t2[q]])
                        kb.op("act", lambda e: e.activation(out=cs[:, j, 0:n], in_=t2[q][:, 0:n], func=AF.Silu,
                                                            scale=sm[:, S_LNG + j:S_LNG + j + 1],
                                                            bias=sm[:, S_LNB + j:S_LNB + j + 1]),
                              rd=[b_t2[q], b_sm], wr=[b_cs])
                    itc[0] = it

                def b_MM(ti):
                    t0, n = TBS[ti]
                    it = itc[0]
                    p = ti % 2
                    cs, b_cs = cs2[p], b_cs2[p]
                    for dc in range(8):
                        pbk, pbb = bank()
                        pgk, pgb = bank()
                        kb.op("pe", lambda e: [e.matmul(pbk[:, 0:n], wco[:, kc, dc * 128:(dc + 1) * 128], cs[:, kc, 0:n],
                                                        start=(kc == 0), stop=(kc == 7)) for kc in range(8)],
                              rd=[b_wco, b_cs], wr=[pbb])
                        kb.op("pe", lambda e: [e.matmul(pgk[:, 0:n], wg1[:, kc, dc * 128:(dc + 1) * 128],
                                                        uT[:, kc, t0:t0 + n], start=(kc == 0), stop=(kc == 7))
                                               for kc in range(8)],
                              rd=[b_wg1] + uT_bufs(t0, n), wr=[pgb])
                        q = it % 2
                        it += 1
                        kb.op("act", lambda e: e.activation(out=sg[q][:, 0:n], in_=pgk[:, 0:n], func=AF.Sigmoid),
                              rd=[pgb], wr=[b_sg[q]])
                        kb.op("dve", lambda e: e.tensor_tensor(out=m1[p][:, dc, 0:n], in0=pbk[:, 0:n],
                                                               in1=sg[q][:, 0:n], op=ALU.mult),
                              rd=[pbb, b_sg[q]], wr=[b_m1[p]])
                        pg2, pg2b = bank()
                        kb.op("pe", lambda e: [e.matmul(pg2[:, 0:n], wg2[:, kc, dc * 128:(dc + 1) * 128],
                                                        uT[:, kc, t0:t0 + n], start=(kc == 0), stop=(kc == 7))
                                               for kc in range(8)],
                              rd=[b_wg2] + uT_bufs(t0, n), wr=[pg2b])
                        kb.op("act", lambda e: e.activation(out=g2[p][:, dc, 0:n], in_=pg2[:, 0:n], func=AF.Sigmoid),
                              rd=[pg2b], wr=[b_g2[p]])
                    kb.dma("sp", lambda qq: qq.dma_start(out=m1_d[:, :, t0:t0 + n], in_=m1[p][:, :, 0:n]),
                           rd=[b_m1[p]], wr=[b_m1d[ti]])
                    kb.dma("sp", lambda qq: qq.dma_start(out=g2_d[:, :, t0:t0 + n], in_=g2[p][:, :, 0:n]),
                           rd=[b_g2[p]], wr=[b_g2d[ti]])
                    itc[0] = it

                b_LN(0)
                for ti in range(len(TBS)):
                    if ti + 1 < len(TBS):
                        b_LN(ti + 1)
                    b_MM(ti)

            kb.barrier()
            with ExitStack() as sc:
                kb.enabled = "C" in run
                wz = sb("wz", [128, 8, 2048], BF16, sc)
                wdt = sb("wdt", [128, 8, 64], BF16, sc)
                b_wz, b_wdt = Buf(), Buf()
                for qq_ in range(4):
                    kb.dma("pool", lambda q: q.dma_start(out=wz[:, :, qq_ * 512:(qq_ + 1) * 512],
                                                         in_=win_v[:, :, 2048 + qq_ * 512:2048 + (qq_ + 1) * 512]),
                           wr=[b_wz])
                kb.dma("pool", lambda q: q.dma_start(out=wdt[:], in_=win_v[:, :, 7168:7232]), wr=[b_wdt])
                dtb = sb("dtb", [128, 64], F32, sc)
                b_dtb = Buf()
                kb.dma("sp", lambda q: q.dma_start(out=dtb[:], in_=bc_d[:, B_DTB:B_DTB + 64]), wr=[b_dtb])
                szt = [sb("szt%d" % i, [128, 2048], BF16, sc) for i in range(2)]
                b_szt = [Buf(), Buf()]
                b_szd = [Buf() for _ in range(NT)]
                dte = sb("dte", [128, 64], F32, sc)
                b_dte = Buf()
                for i in range(NT):
                    p = i % 2
                    for qz in range(4):
                        pz, pzb = bank()
                        kb.op("pe", lambda e: [e.matmul(pz[:, :], uT[:, kc, i * 128:(i + 1) * 128],
                                                        wz[:, kc, qz * 512:(qz + 1) * 512],
                                                        start=(kc == 0), stop=(kc == 7)) for kc in range(8)],
                              rd=[b_wz, b_uT[i]], wr=[pzb])
                        kb.op("act", lambda e: e.activation(out=szt[p][:, qz * 512:(qz + 1) * 512], in_=pz[:, :],
                                                            func=AF.Silu), rd=[pzb], wr=[b_szt[p]])
                    kb.dma("sp", lambda q: q.dma_start(out=sz_d[i], in_=szt[p][:]), rd=[b_szt[p]], wr=[b_szd[i]])
                    pd, pdb = bank()
                    kb.op("pe", lambda e: [e.matmul(pd[:, 0:64], uT[:, kc, i * 128:(i + 1) * 128], wdt[:, kc, :],
                                                    start=(kc == 0), stop=(kc == 7)) for kc in range(8)],
                          rd=[b_wdt, b_uT[i]], wr=[pdb])
                    kb.op("dve", lambda e: e.tensor_tensor(out=dte[:], in0=pd[:, 0:64], in1=dtb[:], op=ALU.add),
                          rd=[pdb, b_dtb], wr=[b_dte])
                    kb.op("act", lambda e: e.activation(out=dte[:], in_=dte[:], func=AF.Exp), rd=[b_dte], wr=[b_dte])
                    kb.op("act", lambda e: e.activation(out=dtall[:, i, :], in_=dte[:], func=AF.Ln, bias=1.0),
                          rd=[b_dte], wr=[b_dt[i]])
                    if i == 0:
                        kb.op("dve", lambda e: e.memset(dtall[0:96, 0, :], 0.0), wr=[b_dt[0]])
                        kb.op("dve", lambda e: e.memset(dtall[96:112, 0, :], 0.0), wr=[b_dt[0]])
                if debug:
                    kb.dma("sp", lambda q: q.dma_start(out=dt_dbg, in_=dtall[:]), rd=b_dt, wr=[Buf()])
                wX = [sb("wX%d" % i, [128, 8, 128], BF16, sc) for i in range(2)]
                b_wX = [Buf(), Buf()]
                xT = [sb("xT%d" % i, [128, T + 6], BF16, sc) for i in range(2)]
                b_xT = [Buf(), Buf()]
                dg7 = [sb("dg7_%d" % i, [128, 7, 128], BF16, sc) for i in range(2)]
                b_dg7 = [Buf(), Buf()]
                xc = [sb("xc%d" % i, [128, 512], BF16, sc) for i in range(2)]
                b_xc = [Buf(), Buf()]
                b_xbcd = [[Buf() for _ in TBS] for _ in range(24)]
                for i in range(2):
                    kb.op("pool", lambda e: e.memset(xT[i][:], 0.0), wr=[b_xT[i]])
                it = 0
                for j in range(24):
                    p = j % 2
                    kb.dma("pool", lambda q: q.dma_start(out=wX[p][:], in_=win_v[:, :, 4096 + j * 128:4096 + (j + 1) * 128]),
                           wr=[b_wX[p]])
                    for k in range(7):
                        kb.op("dve", lambda e: e.tensor_scalar(out=dg7[p][:, k, :], in0=identb,
                                                               scalar1=sm[:, S_WSSM + j * 7 + k:S_WSSM + j * 7 + k + 1],
                                                               scalar2=None, op0=ALU.mult),
                              rd=[b_cbf, b_sm], wr=[b_dg7[p]])
                    for (t0, n) in TBS:
                        px, pxb = bank()
                        kb.op("pe", lambda e: [e.matmul(px[:, 0:n], wX[p][:, kc, :], uT[:, kc, t0:t0 + n],
                                                        start=(kc == 0), stop=(kc == 7)) for kc in range(8)],
                              rd=[b_wX[p]] + uT_bufs(t0, n), wr=[pxb])
                        kb.op("act", lambda e: e.activation(out=xT[p][:, 3 + t0:3 + t0 + n], in_=px[:, 0:n], func=AF.Copy),
                              rd=[pxb], wr=[b_xT[p]])
                    for ti, (t0, n) in enumerate(TBS):
                        pc, pcb = bank()
                        kb.op("pe", lambda e: [e.matmul(pc[:, 0:n], dg7[p][:, k, :], xT[p][:, t0 + k:t0 + k + n],
                                                        start=(k == 0), stop=(k == 6)) for k in range(7)],
                              rd=[b_dg7[p], b_xT[p]], wr=[pcb])
                        q = it % 2
                        it += 1
                        kb.op("act", lambda e: e.activation(out=xc[q][:, 0:n], in_=pc[:, 0:n], func=AF.Silu,
                                                            bias=sm[:, S_BSSM + j:S_BSSM + j + 1]),
                              rd=[pcb, b_sm], wr=[b_xc[q]])
                        if ti == 0:
                            kb.op("dve", lambda e: e.memset(xc[q][:, 0:112], 0.0), wr=[b_xc[q]])
                        kb.dma("sp", lambda qq: qq.dma_start(out=xbc_d[:, j, t0:t0 + n], in_=xc[q][:, 0:n]),
                               rd=[b_xc[q]], wr=[b_xbcd[j][ti]])

        kb.barrier()
        hnrow = Buf()
        hacc = Buf()
        b_ynd = [Buf() for _ in range(NT)]
        with ExitStack() as sd:
            kb.enabled = ("D" in run) or ("E" in run)
            abc = sb("abc", [128, 160], F32, sd)
            b_abc = Buf()
            kb.dma("sp", lambda q: q.dma_start(out=abc[:], in_=bc_d[:, B_DTB:B_DTB + 160]), wr=[b_abc])
            Aneg = sb("Aneg", [128, 64], F32, sd)
            b_A = Buf()
            kb.op("act", lambda e: e.activation(out=Aneg[:], in_=abc[:, 64:128], func=AF.Exp), rd=[b_abc], wr=[b_A])
            kb.op("dve", lambda e: e.tensor_scalar(out=Aneg[:], in0=Aneg[:], scalar1=-1.0, scalar2=None, op0=ALU.mult),
                  rd=[b_A], wr=[b_A])

            def dbl(name, shape, dtype):
                return [sb("%s_%d" % (name, i), shape, dtype, sd) for i in range(2)], [Buf(), Buf()]

            XT, b_XT = dbl("XT", [128, 24, 128], BF16)
            xs_tm2, b_xs2 = dbl("xs_tm", [128, 2048], BF16)
            b_tm2, b_btm2 = dbl("b_tm", [128, 512], BF16)
            cbm2, b_cbm2 = dbl("cbm", [128, 4, 128], BF16)
            a322, b_a322 = dbl("a32", [128, 32], F32)
            rhsA2, b_rhsA2 = dbl("rhsA", [128, 32, 128], F32)
            E2_, b_E2 = dbl("E", [128, 32, 128], BF16)
            dec2, b_dec2 = dbl("dec", [128, 96], F32)
            w22, b_w22 = dbl("w2", [128, 32], F32)
            xd2, b_xd2 = dbl("xd", [128, 32, 64], BF16)
            xdd2, b_xdd2 = dbl("xdd", [128, 32, 64], BF16)
            yacc2, b_yacc2 = dbl("yacc", [128, 2048], F32)
            S32 = sb("S32", [128, 2048], F32, sd)
            Sbf = sb("Sbf", [128, 2048], BF16, sd)
            b_S32g = [Buf() for _ in range(4)]
            b_Sbfg = [Buf() for _ in range(4)]
            b_Eg2 = [[Buf() for _ in range(4)] for _ in range(2)]
            b_yaccg2 = [[Buf() for _ in range(4)] for _ in range(2)]
            tmp = [sb("tmpo%d" % i, [128, 512], F32, sd) for i in range(2)]
            b_tmp = [Buf(), Buf()]
            b_ybd = [Buf() for _ in range(NT)]
            nchunk = [0]

            class Ctx:
                pass

            SEGR = os.environ.get("SEGR", "1") == "1"
            LR = sb("LR", [128, 2, 128], F32R, sd)
            b_LR = Buf()
            if SEGR:
                kb.op("dve", lambda e: e.tensor_copy(out=LR[:, 0, :], in_=cf[:, C_LF:C_LF + 128]), rd=[b_cf], wr=[b_LR])
                kb.op("dve", lambda e: e.tensor_copy(out=LR[:, 1, :], in_=cf[:, C_LB:C_LB + 128]), rd=[b_cf], wr=[b_LR])
            S_ENG = os.environ.get("S_ENG", "pool")
            GT_POOL = int(os.environ.get("GT_POOL", 0))

            def front1(c, d, preload=None):
                k = Ctx()
                p = nchunk[0] % 2
                nchunk[0] += 1
                k.p, k.c, k.d = p, c, d
                k.xs_tm, k.b_xs = xs_tm2[p], b_xs2[p]
                k.b_tm, k.b_btm = b_tm2[p], b_btm2[p]
                k.cbm, k.b_cbm = cbm2[p], b_cbm2[p]
                k.a32, k.b_a32 = a322[p], b_a322[p]
                k.rhsA, k.b_rhsA = rhsA2[p], b_rhsA2[p]
                k.E, k.b_E = E2_[p], b_E2[p]
                k.dec, k.b_dec = dec2[p], b_dec2[p]
                k.w2, k.b_w2 = w22[p], b_w22[p]
                k.xd, k.b_xd = xd2[p], b_xd2[p]
                k.xdd, k.b_xdd = xdd2[p], b_xdd2[p]
                k.yacc, k.b_yacc = yacc2[p], b_yaccg2[p]
                k.b_Eg = b_Eg2[p]
                k.xs3 = k.xs_tm[:].rearrange("p (h d) -> p h d", d=64)
                k.add_prev = preload is not None
                xs_tm, b_xs, b_tm, b_btm, cbm, b_cbm = k.xs_tm, k.b_xs, k.b_tm, k.b_btm, k.cbm, k.b_cbm
                a32, b_a32, rhsA, b_rhsA, E, b_E, dec, b_dec = k.a32, k.b_a32, k.rhsA, k.b_rhsA, k.E, k.b_E, k.dec, k.b_dec
                w2, b_w2, xd, b_xd, xdd, b_xdd, xs3 = k.w2, k.b_w2, k.xd, k.b_xd, k.xdd, k.b_xdd, k.xs3
                k.ybl = None
                if k.add_prev:
                    preload(k)
                rd_x = [b_xbcd[j][min(c // 4, 8)] for j in range(24)]
                kb.dma("sp", lambda q: q.dma_start(out=XT[p][:], in_=xbc_d[:, :, c * 128:(c + 1) * 128]),
                       rd=rd_x, wr=[b_XT[p]])
                X = XT[p]
                bX = b_XT[p]
                k.X, k.bX = X, bX
                U = cf[:, (C_UF if d == 0 else C_UB):(C_UF if d == 0 else C_UB) + 128]
                L = cf[:, (C_LF if d == 0 else C_LB):(C_LF if d == 0 else C_LB) + 128]
                onesf = cf[:, C_ONES:C_ONES + 128]
                Lr = LR[:, d, :] if SEGR else L
                dtc = dtall[:, c, d * 32:(d + 1) * 32]
                kb.op("dve", lambda e: e.tensor_tensor(out=a32[:], in0=dtc, in1=Aneg[:, d * 32:(d + 1) * 32],
                                                       op=ALU.mult), rd=[b_dt[c], b_A], wr=[b_a32])
                kb.op("pool", lambda e: e.tensor_tensor(out=(rhsA[:].bitcast(F32R) if SEGR else rhsA[:]), in0=a32[:].unsqueeze(2).broadcast_to([128, 32, 128]),
                                                        in1=U.unsqueeze(1).broadcast_to([128, 32, 128]), op=ALU.mult),
                      rd=[b_a32, b_cf], wr=[b_rhsA])
                psm, psmb = bank()
                kb.op("pe", lambda e: [e.matmul(psm[:, 0:32], U, a32[:], start=True, stop=True),
                                       e.matmul(psm[:, 32:64], L, a32[:], start=True, stop=True),
                                       e.matmul(psm[:, 64:96], onesf, a32[:], start=True, stop=True)],
                      rd=[b_cf, b_a32], wr=[psmb])
                kb.op("act", lambda e: e.activation(out=dec[:], in_=psm[:, 0:96], func=AF.Exp), rd=[psmb], wr=[b_dec])
                kb.op("dve", lambda e: e.tensor_tensor(out=w2[:], in0=dtc, in1=dec[:, 32:64], op=ALU.mult),
                      rd=[b_dt[c], b_dec], wr=[b_w2])
                for half in range(2):
                    pt, pb = bank()
                    ptb = pt[:].bitcast(BF16)
                    kb.op("pe", lambda e: [e.transpose(ptb[:, kk * 128:(kk + 1) * 128], X[:, half * 8 + kk, :], identb)
                                           for kk in range(8)], rd=[bX, b_cbf], wr=[pb])
                    kb.op("act", lambda e: e.activation(out=xs_tm[:, half * 1024:(half + 1) * 1024], in_=ptb,
                                                        func=AF.Copy), rd=[pb], wr=[b_xs])
                pt, pb = bank()
                ptb = pt[:].bitcast(BF16)
                kb.op("pe", lambda e: [e.transpose(ptb[:, kk * 128:(kk + 1) * 128], X[:, 16 + kk, :], identb)
                                       for kk in range(4)], rd=[bX, b_cbf], wr=[pb])
                kb.op("act", lambda e: e.activation(out=b_tm[:], in_=ptb[:, 0:512], func=AF.Copy), rd=[pb], wr=[b_btm])
                pcb_, pcbb = bank()
                kb.op("pe", lambda e: [e.matmul(pcb_[:, g * 128:(g + 1) * 128], X[:, 16 + g, :], X[:, 20 + g, :],
                                                start=True, stop=True) for g in range(4)], rd=[bX], wr=[pcbb])
                kb.op("dve", lambda e: e.tensor_tensor(out=cbm[:], in0=pcb_[:].rearrange("p (g l) -> p g l", g=4),
                                                       in1=U.unsqueeze(1).broadcast_to([128, 4, 128]), op=ALU.mult),
                      rd=[pcbb, b_cf], wr=[b_cbm])
                kb.op("pool", lambda e: e.tensor_tensor(out=xd[:], in0=xs3, in1=dtc.unsqueeze(2).broadcast_to([128, 32, 64]),
                                                        op=ALU.mult), rd=[b_xs, b_dt[c]], wr=[b_xd])
                kb.op("pool", lambda e: e.tensor_tensor(out=xdd[:], in0=xs3,
                                                        in1=w2[:].unsqueeze(2).broadcast_to([128, 32, 64]), op=ALU.mult),
                      rd=[b_xs, b_w2], wr=[b_xdd])
                for q8 in range(8):
                    pse, pseb = bank()
                    kb.op("pe", lambda e: e.matmul(pse[:, :], Lr, (rhsA[:, q8 * 4:(q8 + 1) * 4, :].bitcast(F32R) if SEGR else rhsA[:, q8 * 4:(q8 + 1) * 4, :]),
                                                   start=True, stop=True),
                          rd=[b_cf, b_rhsA, b_LR], wr=[pseb])
                    kb.op("act", lambda e: e.activation(out=E[:, q8 * 4:(q8 + 1) * 4, :],
                                                        in_=pse[:].rearrange("p (h l) -> p h l", h=4), func=AF.Exp),
                          rd=[pseb], wr=[k.b_Eg[q8 // 2]])
                return k

            def front2(k):
                for g in range(4):
                    kb.op("pool" if g < GT_POOL else "dve", lambda e: e.tensor_tensor(out=k.E[:, g * 8:(g + 1) * 8, :], in0=k.E[:, g * 8:(g + 1) * 8, :],
                                                           in1=k.cbm[:, g:g + 1, :].broadcast_to([128, 8, 128]),
                                                           op=ALU.mult), rd=[k.b_cbm], wr=[k.b_Eg[g]])

            def back(k):
                GT, b_GT, xd, b_xd, xdd, b_xdd = k.E, k.b_E, k.xd, k.b_xd, k.xdd, k.b_xdd
                X, bX, b_tm, b_btm, dec, b_dec = k.X, k.bX, k.b_tm, k.b_btm, k.dec, k.b_dec
                yacc, b_yacc = k.yacc, k.b_yacc
                for g in range(4):
                    py, pyb = bank()
                    po, pob = bank()
                    pst, pstb = bank()
                    if k.add_prev:
                        kb.op("pe", lambda e: [e.matmul(py[:, :], identb, k.ybl[:, g * 512:(g + 1) * 512], start=True, stop=False)]
                              + [e.matmul(py[:, hh * 64:(hh + 1) * 64], DI[:, g * 8 + hh, :],
                                          k.xs_tm[:, (g * 8 + hh) * 64:(g * 8 + hh + 1) * 64], start=False, stop=False)
                                 for hh in range(8)]
                              + [e.matmul(py[:, hh * 64:(hh + 1) * 64], GT[:, g * 8 + hh, :], xd[:, g * 8 + hh, :],
                                          start=False, stop=(hh == 7)) for hh in range(8)],
                              rd=[k.b_Eg[g], b_xd, k.b_ybl, k.b_xs, b_DI, b_cbf], wr=[pyb])
                    else:
                        kb.op("pe", lambda e: [e.matmul(py[:, hh * 64:(hh + 1) * 64], GT[:, g * 8 + hh, :], xd[:, g * 8 + hh, :],
                                                        start=True, stop=True) for hh in range(8)],
                              rd=[k.b_Eg[g], b_xd], wr=[pyb])
                    kb.op("pe", lambda e: e.matmul(po[:, :], X[:, 20 + g, :], Sbf[:, g * 512:(g + 1) * 512],
                                                   start=True, stop=True), rd=[bX, b_Sbfg[g]], wr=[pob])
                    kb.op("pe", lambda e: e.matmul(pst[:, :], b_tm[:, g * 128:(g + 1) * 128],
                                                   xdd[:, g * 8:(g + 1) * 8, :], start=True, stop=True),
                          rd=[b_btm, b_xdd], wr=[pstb])
                    q = g % 2
                    kb.op("dve", lambda e: e.tensor_tensor(
                        out=tmp[q][:].rearrange("p (h d) -> p h d", d=64),
                        in0=po[:].rearrange("p (h d) -> p h d", d=64),
                        in1=dec[:, g * 8:(g + 1) * 8].unsqueeze(2).broadcast_to([128, 8, 64]), op=ALU.mult),
                        rd=[pob, b_dec], wr=[b_tmp[q]])
                    kb.op("dve", lambda e: e.tensor_tensor(out=yacc[:, g * 512:(g + 1) * 512], in0=py[:, :], in1=tmp[q][:],
                                                           op=ALU.add), rd=[pyb, b_tmp[q]], wr=[b_yacc[g]])
                    kb.op(S_ENG, lambda e: e.tensor_tensor(
                        out=S32[:, g * 512:(g + 1) * 512].rearrange("p (h d) -> p h d", d=64),
                        in0=S32[:, g * 512:(g + 1) * 512].rearrange("p (h d) -> p h d", d=64),
                        in1=dec[:, 64 + g * 8:64 + (g + 1) * 8].unsqueeze(2).broadcast_to([128, 8, 64]), op=ALU.mult),
                        rd=[b_dec], wr=[b_S32g[g]])
                    kb.op("dve", lambda e: e.tensor_tensor(out=S32[:, g * 512:(g + 1) * 512], in0=pst[:, :],
                                                           in1=S32[:, g * 512:(g + 1) * 512], op=ALU.add),
                          rd=[pstb], wr=[b_S32g[g]])
                    kb.op("act", lambda e: e.activation(out=Sbf[:, g * 512:(g + 1) * 512], in_=S32[:, g * 512:(g + 1) * 512],
                                                        func=AF.Copy), rd=[b_S32g[g]], wr=[b_Sbfg[g]])

            def run_pass(order, d, preload_fn, post_fn):
                k = front1(order[0], d, preload_fn(order[0]) if preload_fn else None)
                front2(k)
                for i, c in enumerate(order):
                    kn = None
                    if i + 1 < len(order):
                        cn = order[i + 1]
                        kn = front1(cn, d, preload_fn(cn) if preload_fn else None)
                    back(k)
                    post_fn(k)
                    if kn is not None:
                        front2(kn)
                    k = kn

            kb.enabled = "D" in run
            kb.op("dve", lambda e: e.memset(S32[:], 0.0), wr=b_S32g)
            kb.op("dve", lambda e: e.memset(Sbf[:], 0.0), wr=b_Sbfg)
            run_pass(list(range(NT - 1, -1, -1)), 1, None,
                     lambda k: kb.dma("pool", lambda q: q.dma_start(out=yb_d[k.c], in_=k.yacc[:]), rd=k.b_yacc, wr=[b_ybd[k.c]]))

            kb.enabled = "E" in run
            kb.op("dve", lambda e: e.memset(S32[:], 0.0), wr=b_S32g)
            kb.op("dve", lambda e: e.memset(Sbf[:], 0.0), wr=b_Sbfg)
            wns = sb("wns", [128, 2048], F32, sd)
            b_wns = Buf()
            kb.dma("sp", lambda q: q.dma_start(out=wns[:], in_=bc_d[:, B_WSSM:B_WSSM + 2048]), wr=[b_wns])
            szl2, b_szl2 = dbl("szl", [128, 2048], BF16)
            yn2, b_yn2 = dbl("yn", [128, 2048], BF16)
            ybl2, b_ybl2 = dbl("ybl", [128, 2048], BF16)
            DI = sb("DI", [128, 32, 128], BF16, sd)
            b_DI = Buf()
            for hh_ in range(32):
                kb.op("dve", lambda e: e.tensor_scalar(out=DI[:, hh_, :], in0=identb, scalar1=abc[:, 128 + hh_:129 + hh_],
                                                       scalar2=None, op0=ALU.mult), rd=[b_cbf, b_abc], wr=[b_DI])
            ssf2, b_ssf2 = dbl("ssf", [128, 1], F32)
            rsf2, b_rsf2 = dbl("rsf", [128, 1], F32)

            def fwd_preload(c):
                def f(k):
                    k.ybl, k.b_ybl = ybl2[k.p], b_ybl2[k.p]
                    kb.dma("sp", lambda q: q.dma_start(out=k.ybl[:], in_=yb_d[c]), rd=[b_ybd[c]], wr=[k.b_ybl])
                return f

            def fwd_post(k):
                p, c = k.p, k.c
                yacc, b_yacc = k.yacc, k.b_yacc
                kb.dma("sp", lambda q: q.dma_start(out=szl2[p][:], in_=sz_d[c]), rd=[b_szd[c]], wr=[b_szl2[p]])
                kb.op("pool", lambda e: e.tensor_tensor(out=yacc[:], in0=yacc[:], in1=szl2[p][:], op=ALU.mult),
                      rd=[b_szl2[p]], wr=b_yacc)
                rms_rstd(yacc[:], b_yacc, yn2[p][:], b_yn2[p], ssf2[p], b_ssf2[p], rsf2[p], b_rsf2[p], 2048)
                kb.op("dve", lambda e: e.scalar_tensor_tensor(out=yn2[p][:], in0=yacc[:], scalar=rsf2[p][:, 0:1], in1=wns[:],
                                                              op0=ALU.mult, op1=ALU.mult),
                      rd=b_yacc + [b_rsf2[p], b_wns], wr=[b_yn2[p]])
                kb.dma("sp", lambda q: q.dma_start(out=yn_d[c], in_=yn2[p][:]), rd=[b_yn2[p]], wr=[b_ynd[c]])

            run_pass(list(range(NT)), 0, fwd_preload, fwd_post)

        kb.barrier()
        affTM = sb("affTM", [128, NT, NE], F32)
        b_affTM = Buf()
        affT = sb("affT", [NE, T], F32)
        b_affT = Buf()
        with ExitStack() as se:
            kb.enabled = "E" in run
            wso = sb("wso", [128, 16, D], BF16, se)
            wout = sb("wout", [128, 8, D], BF16, se)
            wr_ = sb("wr_", [128, 8, NE], BF16, se)
            b_wso, b_wout, b_wr = Buf(), Buf(), Buf()
            kb.dma("pool", lambda q: q.dma_start(out=wso[:], in_=w_so.rearrange("(kc p) n -> p kc n", p=128)), wr=[b_wso])
            kb.dma("pool", lambda q: q.dma_start(out=wout[:], in_=w_o.rearrange("(kc p) n -> p kc n", p=128)), wr=[b_wout])
            kb.dma("pool", lambda q: q.dma_start(out=wr_[:], in_=w_r.rearrange("(kc p) n -> p kc n", p=128)), wr=[b_wr])
            wnf = sb("wnf", [128, D], F32, se)
            b_wnf = Buf()
            kb.dma("sp", lambda q: q.dma_start(out=wnf[:], in_=bc_d[:, B_WFFN:B_WFFN + D]), wr=[b_wnf])

            def nbuf(name, shape, dtype, n):
                return [sb("%s_%d" % (name, i), shape, dtype, se) for i in range(n)], [Buf() for _ in range(n)]

            ynl1, b_ynl1 = nbuf("ynl", [128, 4, 2048], BF16, 1)
            ynT2, b_ynT2 = nbuf("ynT", [128, 16, 512], BF16, 1)
            ynT2, b_ynT2 = ynT2 * 2, b_ynT2 * 2
            m1l1, b_m1l1 = nbuf("m1l", [128, 8, 512], BF16, 1)
            g2l1, b_g2l1 = nbuf("g2l", [128, 8, 512], BF16, 1)
            mT2, _ = nbuf("mT", [128, 8, 512], BF16, 2)
            b_mT2 = [[Buf() for _ in range(8)] for _ in range(2)]
            xinF4, b_xinF4 = nbuf("xinF", [128, D], F32, 4)
            hT4, b_hT4 = nbuf("h_t", [128, D], F32, 4)
            hnb4, b_hnb4 = nbuf("hnb", [128, D], BF16, 4)
            hnT4, b_hnT4 = nbuf("hnT", [128, 8, 128], BF16, 4)
            junkE2, b_junkE2 = nbuf("junkE", [128, D], BF16, 2)
            ssE4, b_ssE4 = nbuf("ssE", [128, 1], F32, 4)
            rsE4, b_rsE4 = nbuf("rsE", [128, 1], F32, 4)
            lg4, b_lg4 = nbuf("lg", [128, NE], F32, 4)
            mx4, b_mx4 = nbuf("mx", [128, 1], F32, 4)
            sme4, b_sme4 = nbuf("sme", [128, 1], F32, 4)

            def stage_TS(ti):
                t0, n = TBS[ti]
                pb_ = ti % 2
                nt_ = n // 128
                c0 = t0 // 128
                ynl, b_ynl = ynl1[0], b_ynl1[0]
                ynT, b_ynT = ynT2[pb_], b_ynT2[pb_]
                m1l, b_m1l = m1l1[0], b_m1l1[0]
                g2l, b_g2l = g2l1[0], b_g2l1[0]
                mT, b_mT = mT2[pb_], b_mT2[pb_]
                kb.dma("sp", lambda q: q.dma_start(out=ynl[:, 0:nt_, :], in_=yn_d[c0:c0 + nt_].rearrange("t p f -> p t f")),
                       rd=b_ynd[c0:c0 + nt_], wr=[b_ynl])
                kb.dma("sp", lambda q: q.dma_start(out=m1l[:, :, 0:n], in_=m1_d[:, :, t0:t0 + n]), rd=[b_m1d[ti]], wr=[b_m1l])
                kb.dma("sp", lambda q: q.dma_start(out=g2l[:, :, 0:n], in_=g2_d[:, :, t0:t0 + n]), rd=[b_g2d[ti]], wr=[b_g2l])
                for tt in range(nt_):
                    for half in range(2):
                        pt, pb = bank()
                        ptb = pt[:].bitcast(BF16)
                        kb.op("pe", lambda e: [e.transpose(ptb[:, k * 128:(k + 1) * 128],
                                                           ynl[:, tt, (half * 8 + k) * 128:(half * 8 + k + 1) * 128], identb)
                                               for k in range(8)], rd=[b_ynl, b_cbf], wr=[pb])
                        kb.op("act", lambda e: e.activation(out=ynT[:, half * 8:(half + 1) * 8, tt * 128:(tt + 1) * 128],
                                                            in_=ptb.rearrange("p (k t) -> p k t", k=8), func=AF.Copy),
                              rd=[pb], wr=[b_ynT])
                for dc in range(8):
                    pbs, pbsb = bank()
                    kb.op("pe", lambda e: [e.matmul(pbs[:, 0:n], wso[:, kc, dc * 128:(dc + 1) * 128], ynT[:, kc, 0:n],
                                                    start=(kc == 0), stop=(kc == 15)) for kc in range(16)],
                          rd=[b_wso, b_ynT], wr=[pbsb])
                    kb.op("dve", lambda e: e.tensor_tensor(out=mT[:, dc, 0:n], in0=pbs[:, 0:n], in1=g2l[:, dc, 0:n],
                                                           op=ALU.mult), rd=[pbsb, b_g2l], wr=[b_mT[dc]])
                    kb.op("pool", lambda e: e.tensor_tensor(out=mT[:, dc, 0:n], in0=mT[:, dc, 0:n], in1=m1l[:, dc, 0:n],
                                                            op=ALU.add), rd=[b_m1l], wr=[b_mT[dc]])

            def stage_W(ti):
                t0, n = TBS[ti]
                pb_ = ti % 2
                nt_ = n // 128
                c0 = t0 // 128
                mT, b_mT = mT2[pb_], b_mT2[pb_]
                for tt in range(nt_):
                    c = c0 + tt
                    load_x_tile(c, xinF4[tt], b_xinF4[tt])
                    for dh in range(2):
                        ph, phb = bank()
                        kb.op("pe", lambda e: [e.matmul(ph[:, :], mT[:, kc, tt * 128:(tt + 1) * 128],
                                                        wout[:, kc, dh * 512:(dh + 1) * 512],
                                                        start=(kc == 0), stop=(kc == 7)) for kc in range(8)],
                              rd=[b_wout] + b_mT, wr=[phb])
                        kb.op("dve", lambda e: e.tensor_tensor(out=hT4[tt][:, dh * 512:(dh + 1) * 512], in0=ph[:, :],
                                                               in1=xinF4[tt][:, dh * 512:(dh + 1) * 512], op=ALU.add),
                              rd=[phb, b_xinF4[tt]], wr=[b_hT4[tt]])
                    kb.dma("sp", lambda q: q.dma_start(out=hacc_d[c * 128:(c + 1) * 128, :], in_=hT4[tt][:]),
                           rd=[b_hT4[tt]], wr=[hacc])
                for tt in range(nt_):
                    kb.op("act", lambda e: e.activation(out=junkE2[tt % 2][:], in_=hT4[tt][:], func=AF.Square,
                                                        accum_out=ssE4[tt][:]),
                          rd=[b_hT4[tt]], wr=[b_junkE2[tt % 2], b_ssE4[tt]])
                for tt in range(nt_):
                    kb.op("act", lambda e: e.activation(out=ssE4[tt][:], in_=ssE4[tt][:], func=AF.Sqrt, bias=EPS, scale=1.0 / D),
                          rd=[b_ssE4[tt]], wr=[b_ssE4[tt]])
                for tt in range(nt_):
                    kb.op("dve", lambda e: e.reciprocal(out=rsE4[tt][:], in_=ssE4[tt][:]), rd=[b_ssE4[tt]], wr=[b_rsE4[tt]])
                for tt in range(nt_):
                    c = c0 + tt
                    kb.op("dve", lambda e: e.scalar_tensor_tensor(out=hnb4[tt][:], in0=hT4[tt][:], scalar=rsE4[tt][:, 0:1],
                                                                  in1=wnf[:], op0=ALU.mult, op1=ALU.mult),
                          rd=[b_hT4[tt], b_rsE4[tt], b_wnf], wr=[b_hnb4[tt]])
                    kb.dma("sp", lambda q: q.dma_start(out=hn_d[c * 128:(c + 1) * 128, :], in_=hnb4[tt][:]),
                           rd=[b_hnb4[tt]], wr=[hnrow])

            def stage_R(ti):
                t0, n = TBS[ti]
                nt_ = n // 128
                c0 = t0 // 128
                prs = []
                for tt in range(nt_):
                    pt, pb = bank()
                    ptb = pt[:].bitcast(BF16)
                    kb.op("pe", lambda e: [e.transpose(ptb[:, k * 128:(k + 1) * 128], hnb4[tt][:, k * 128:(k + 1) * 128], identb)
                                           for k in range(8)], rd=[b_hnb4[tt], b_cbf], wr=[pb])
                    kb.op("act", lambda e: e.activation(out=hnT4[tt][:], in_=ptb.rearrange("p (k t) -> p k t", k=8), func=AF.Copy),
                          rd=[pb], wr=[b_hnT4[tt]])
                for tt in range(nt_):
                    pr, prb = bank()
                    prs.append((pr, prb))
                    kb.op("pe", lambda e: [e.matmul(pr[:, 0:NE], hnT4[tt][:, kc, :], wr_[:, kc, :], start=(kc == 0), stop=(kc == 7))
                                           for kc in range(8)], rd=[b_hnT4[tt], b_wr], wr=[prb])
                for tt in range(nt_):
                    pr, prb = prs[tt]
                    kb.op("dve", lambda e: e.reduce_max(out=mx4[tt][:], in_=pr[:, 0:NE], axis=AX.X), rd=[prb], wr=[b_mx4[tt]])
                for tt in range(nt_):
                    kb.op("dve", lambda e: e.tensor_scalar(out=mx4[tt][:], in0=mx4[tt][:], scalar1=-1.0, scalar2=None, op0=ALU.mult),
                          rd=[b_mx4[tt]], wr=[b_mx4[tt]])
                for tt in range(nt_):
                    pr, prb = prs[tt]
                    kb.op("act", lambda e: e.activation(out=lg4[tt][:], in_=pr[:, 0:NE], func=AF.Exp, bias=mx4[tt][:, 0:1],
                                                        accum_out=sme4[tt][:]), rd=[prb, b_mx4[tt]], wr=[b_lg4[tt], b_sme4[tt]])
                for tt in range(nt_):
                    kb.op("dve", lambda e: e.reciprocal(out=sme4[tt][:], in_=sme4[tt][:]), rd=[b_sme4[tt]], wr=[b_sme4[tt]])
                for tt in range(nt_):
                    c = c0 + tt
                    kb.op("dve", lambda e: e.tensor_scalar(out=affTM[:, c, :], in0=lg4[tt][:], scalar1=sme4[tt][:, 0:1], scalar2=None,
                                                           op0=ALU.mult), rd=[b_lg4[tt], b_sme4[tt]], wr=[b_affTM])
                for tt in range(nt_):
                    c = c0 + tt
                    pa_, pab_ = bank()
                    kb.op("pe", lambda e: e.transpose(pa_[0:NE, 0:128], affTM[:, c, :], identf), rd=[b_affTM, b_cf], wr=[pab_])
                    kb.op("act", lambda e: e.activation(out=affT[:, c * 128:(c + 1) * 128], in_=pa_[0:NE, 0:128], func=AF.Copy),
                          rd=[pab_], wr=[b_affT])

            stage_TS(0)
            for ti in range(len(TBS)):
                stage_W(ti)
                if ti + 1 < len(TBS):
                    stage_TS(ti + 1)
                stage_R(ti)

        kb.barrier()
        if debug:
            kb.enabled = "E" in run
            kb.dma("sp", lambda q: q.dma_start(out=aff_dbg, in_=affTM[:]), rd=[b_affTM], wr=[Buf()])
        with ExitStack() as sf:
            kb.enabled = "F" in run
            kb.op("dve", lambda e: e.memset(affT[:, 0:112], 0.0), wr=[b_affT])
            posmTM = sf.enter_context(nc.sbuf_tensor("posmTM", [128, NT, NE], F32))
            RT = sf.enter_context(nc.sbuf_tensor("RT", [128, NT, NE, 4], BF16))
            sr = ExitStack()
            lo = sr.enter_context(nc.sbuf_tensor("lo", [NE, 1], F32))
            hi = sr.enter_context(nc.sbuf_tensor("hi", [NE, 1], F32))
            mid = sr.enter_context(nc.sbuf_tensor("mid", [NE, 1], F32))
            cnt = sr.enter_context(nc.sbuf_tensor("cnt", [NE, 1], F32))
            ge = sr.enter_context(nc.sbuf_tensor("ge", [NE, 1], F32))
            dl = sr.enter_context(nc.sbuf_tensor("dl", [NE, 1], F32))
            jk = sr.enter_context(nc.sbuf_tensor("jk", [NE, T], BF16))
            maskT = sr.enter_context(nc.sbuf_tensor("maskT", [NE, T], F32))
            csum = sr.enter_context(nc.sbuf_tensor("csum", [NE, T], F32))
            onesT = sr.enter_context(nc.sbuf_tensor("onesT", [NE, T], F32))
            ahi = sr.enter_context(nc.sbuf_tensor("ahi", [128, NT, NE], BF16))
            alo = sr.enter_context(nc.sbuf_tensor("alo", [128, NT, NE], F32))
            b_r = Buf()
            b_posm = Buf()
            b_RT = Buf()
            kb.op("dve", lambda e: e.memset(lo[:], 0.0), wr=[b_r])
            kb.op("dve", lambda e: e.memset(hi[:], 1.0), wr=[b_r])
            kb.op("dve", lambda e: e.memset(onesT[:], 1.0), wr=[b_r])
            for itn in range(28):
                kb.op("dve", lambda e: e.tensor_tensor(out=mid[:], in0=lo[:], in1=hi[:], op=ALU.add), rd=[b_r], wr=[b_r])
                kb.op("dve", lambda e: e.tensor_scalar(out=mid[:], in0=mid[:], scalar1=0.5, scalar2=None, op0=ALU.mult),
                      rd=[b_r], wr=[b_r])
                kb.op("dve", lambda e: e.tensor_scalar(out=jk[:], in0=affT[:], scalar1=mid[:, 0:1], scalar2=None,
                                                       op0=ALU.is_gt, op1=ALU.add, accum_out=cnt[:]),
                      rd=[b_r, b_affT], wr=[b_r])
                kb.op("dve", lambda e: e.tensor_scalar(out=ge[:], in0=cnt[:], scalar1=float(CAP) - 0.5, scalar2=None,
                                                       op0=ALU.is_gt), rd=[b_r], wr=[b_r])
                kb.op("dve", lambda e: e.tensor_tensor(out=dl[:], in0=mid[:], in1=lo[:], op=ALU.subtract), rd=[b_r], wr=[b_r])
                kb.op("dve", lambda e: e.tensor_tensor(out=dl[:], in0=dl[:], in1=ge[:], op=ALU.mult), rd=[b_r], wr=[b_r])
                kb.op("dve", lambda e: e.tensor_tensor(out=lo[:], in0=lo[:], in1=dl[:], op=ALU.add), rd=[b_r], wr=[b_r])
                kb.op("dve", lambda e: e.tensor_tensor(out=dl[:], in0=hi[:], in1=mid[:], op=ALU.subtract), rd=[b_r], wr=[b_r])
                kb.op("dve", lambda e: e.tensor_tensor(out=dl[:], in0=dl[:], in1=ge[:], op=ALU.mult), rd=[b_r], wr=[b_r])
                kb.op("dve", lambda e: e.tensor_tensor(out=hi[:], in0=mid[:], in1=dl[:], op=ALU.add), rd=[b_r], wr=[b_r])
            kb.op("dve", lambda e: e.tensor_scalar(out=maskT[:], in0=affT[:], scalar1=lo[:, 0:1], scalar2=None,
                                                   op0=ALU.is_gt), rd=[b_r, b_affT], wr=[b_r])
            kb.op("dve", lambda e: e.tensor_tensor_scan(out=csum[:], data0=onesT[:], data1=maskT[:], initial=0.0,
                                                        op0=ALU.mult, op1=ALU.add), rd=[b_r], wr=[b_r])
            kb.op("dve", lambda e: e.tensor_tensor(out=csum[:], in0=csum[:], in1=maskT[:], op=ALU.mult), rd=[b_r], wr=[b_r])
            kb.op("dve", lambda e: e.tensor_scalar(out=csum[:], in0=csum[:], scalar1=-1.0, scalar2=None, op0=ALU.add),
                  rd=[b_r], wr=[b_r])
            for c in range(NT):
                pp, ppb = bank()
                kb.op("pe", lambda e: e.transpose(pp[:, 0:NE], csum[:, c * 128:(c + 1) * 128], identf[0:NE, 0:NE]),
                      rd=[b_r, b_cf], wr=[ppb])
                kb.op("act", lambda e: e.activation(out=posmTM[:, c, :], in_=pp[:, 0:NE], func=AF.Copy),
                      rd=[ppb], wr=[b_posm])
            kb.op("dve", lambda e: e.tensor_copy(out=ahi[:], in_=affTM[:]), rd=[b_affTM], wr=[b_RT])
            kb.op("dve", lambda e: e.tensor_tensor(out=alo[:], in0=affTM[:], in1=ahi[:], op=ALU.subtract),
                  rd=[b_affTM], wr=[b_RT])
            kb.op("dve", lambda e: e.tensor_copy(out=RT[:, :, :, 2], in_=ahi[:]), wr=[b_RT])
            kb.op("dve", lambda e: e.tensor_copy(out=RT[:, :, :, 3], in_=alo[:]), wr=[b_RT])
            tv = cbf[:, CB_TV:CB_TV + 66].rearrange("p (t two) -> p t two", two=2)
            kb.op("dve", lambda e: e.tensor_copy(out=RT[:, :, :, 0], in_=tv[:, :, 0:1].broadcast_to([128, NT, NE])),
                  rd=[b_cbf], wr=[b_RT])
            kb.op("dve", lambda e: e.tensor_copy(out=RT[:, :, :, 1], in_=tv[:, :, 1:2].broadcast_to([128, NT, NE])),
                  rd=[b_cbf], wr=[b_RT])

            kb.barrier()
            sr.close()

            if debug:
                kb.enabled = "F" in run
                kb.dma("sp", lambda q: q.dma_start(out=posm_dbg, in_=posmTM[:]), rd=[b_posm], wr=[Buf()])
            kb.enabled = "G" in run
            NSLOT = 8
            wring = [sf.enter_context(nc.sbuf_tensor("wring%d" % i, [128, 4096], BF16)) for i in range(NSLOT)]
            b_wring = [Buf() for _ in range(NSLOT)]
            units = []
            for e_ in range(NE):
                for q4 in range(4):
                    units.append(("g", e_, q4))
                    units.append(("u", e_, q4))
                for q4 in range(4):
                    units.append(("d", e_, q4))
            slot_of = {}

            def issue_unit(ui):
                kind, e_, q4 = units[ui]
                s_ = ui % NSLOT
                slot_of[(kind, e_, q4)] = s_
                if not kb.enabled or os.environ.get("GNOW") == "1":
                    return
                if kind in ("g", "u"):
                    src = (w_eg if kind == "g" else w_eu)[e_].rearrange("(kc p) n -> p kc n", p=128)[:, :, q4 * 512:(q4 + 1) * 512]
                    dst = wring[s_][:].rearrange("p (kc n) -> p kc n", kc=8)
                else:
                    src = w_ed[e_].rearrange("(fc p) n -> p fc n", p=128)[:, q4 * 4:(q4 + 1) * 4, :]
                    dst = wring[s_][:].rearrange("p (fc n) -> p fc n", fc=4)
                MAXOUT = int(os.environ.get("MAXOUT", 2))
                if len(unit_toks) >= MAXOUT:
                    kb.wait("pool", [unit_toks[-MAXOUT]])
                unit_toks.append(kb.dma("pool", lambda q: q.dma_start(out=dst, in_=src), wr=[b_wring[s_]]))

            unit_toks = []
            PREF = 6
            nissued = [0]

            def ensure(ui):
                while nissued[0] <= min(ui + PREF, len(units) - 1):
                    issue_unit(nissued[0])
                    nissued[0] += 1

            sel = [sf.enter_context(nc.sbuf_tensor("sel%d" % i, [128, 516], BF16)) for i in range(3)]
            b_sel = [Buf() for _ in range(3)]
            iq = sf.enter_context(nc.sbuf_tensor("iq", [4, 516], F32))
            b_iq = Buf()
            iqT = sf.enter_context(nc.sbuf_tensor("iqT", [128, 5, 4], F32))
            b_iqT = Buf()
            idxf = sf.enter_context(nc.sbuf_tensor("idxf", [128, 5], F32))
            idxi = [sf.enter_context(nc.sbuf_tensor("idxi%d" % i, [128, 5], I32)) for i in range(2)]
            afs = [sf.enter_context(nc.sbuf_tensor("afs%d" % i, [128, 5], F32)) for i in range(2)]
            b_idx = [Buf(), Buf()]
            xg2 = [sf.enter_context(nc.sbuf_tensor("xg%d" % i, [128, 5, D], BF16)) for i in range(2)]
            b_xg2 = [Buf(), Buf()]
            xgT2 = [sf.enter_context(nc.sbuf_tensor("xgT%d" % i, [128, 8, 640], BF16)) for i in range(2)]
            b_xgT2 = [Buf(), Buf()]
            hTe = sf.enter_context(nc.sbuf_tensor("hTe", [128, 16, 640], BF16))
            b_hTe = [Buf() for _ in range(16)]
            sgl = [sf.enter_context(nc.sbuf_tensor("sgl%d" % i, [128, 257], BF16)) for i in range(2)]
            b_sgl = [Buf(), Buf()]
            yw = [sf.enter_context(nc.sbuf_tensor("yw%d" % i, [128, D], F32)) for i in range(2)]
            b_yw = [Buf(), Buf()]
            hg = [sf.enter_context(nc.sbuf_tensor("hg%d" % i, [128, D], F32)) for i in range(2)]
            b_hg = [Buf(), Buf()]
            for i in range(2):
                kb.op("pool", lambda e: e.memset(xg2[i][:], 0.0), wr=[b_xg2[i]])
            iota = cf[:, C_IOTA:C_IOTA + 516]
            uic = [0]
            ityc = [0]

            def prepA(e_):
                ip = e_ % 2
                pi1, pi1b = bank()
                pi2, pi2b = bank()
                for c in range(NT):
                    s3 = c % 3
                    kb.op("dve", lambda e: e.tensor_scalar(out=sel[s3][:], in0=iota, scalar1=posmTM[:, c, e_:e_ + 1],
                                                           scalar2=None, op0=ALU.is_equal),
                          rd=[b_cf, b_posm], wr=[b_sel[s3]])
                    kb.op("pe", lambda e: [e.matmul(pi1[0:4, :], RT[:, c, e_, :], sel[s3][:, 0:512],
                                                    start=(c == 0), stop=(c == NT - 1)),
                                           e.matmul(pi2[0:4, 0:4], RT[:, c, e_, :], sel[s3][:, 512:516],
                                                    start=(c == 0), stop=(c == NT - 1))],
                          rd=[b_RT, b_sel[s3]], wr=[pi1b, pi2b])
                kb.op("act", lambda e: e.activation(out=iq[:, 0:512], in_=pi1[0:4, :], func=AF.Copy), rd=[pi1b], wr=[b_iq])
                kb.op("act", lambda e: e.activation(out=iq[:, 512:516], in_=pi2[0:4, 0:4], func=AF.Copy), rd=[pi2b], wr=[b_iq])
                pq, pqb = bank()
                kb.op("pe", lambda e: ([e.transpose(pq[:, jb * 4:(jb + 1) * 4], iq[:, jb * 128:(jb + 1) * 128],
                                                    identf[0:4, 0:4]) for jb in range(4)]
                                       + [e.transpose(pq[0:4, 16:20], iq[:, 512:516], identf[0:4, 0:4])]),
                      rd=[b_iq, b_cf], wr=[pqb])
                kb.op("dve", lambda e: e.memset(iqT[:], 0.0), wr=[b_iqT])
                kb.op("act", lambda e: e.activation(out=iqT[:, 0:4, :], in_=pq[:, 0:16].rearrange("p (j f) -> p j f", f=4),
                                                    func=AF.Copy), rd=[pqb], wr=[b_iqT])
                kb.op("act", lambda e: e.activation(out=iqT[0:4, 4, :], in_=pq[0:4, 16:20], func=AF.Copy),
                      rd=[pqb], wr=[b_iqT])
                kb.op("dve", lambda e: e.scalar_tensor_tensor(out=idxf[:], in0=iqT[:, :, 0], scalar=128.0, in1=iqT[:, :, 1],
                                                              op0=ALU.mult, op1=ALU.add), rd=[b_iqT], wr=[b_idx[ip]])
                kb.op("dve", lambda e: e.tensor_copy(out=idxi[ip][:], in_=idxf[:]), wr=[b_idx[ip]])
                kb.op("dve", lambda e: e.tensor_tensor(out=afs[ip][:], in0=iqT[:, :, 2], in1=iqT[:, :, 3], op=ALU.add),
                      rd=[b_iqT], wr=[b_idx[ip]])
                for jb in range(5):
                    M = 128 if jb < 4 else 2
                    kb.dma("pool", lambda q: q.indirect_dma_start(
                        out=xg2[ip][0:M, jb, :], out_offset=None, in_=hn_d[:, :],
                        in_offset=bass.IndirectOffsetOnAxis(ap=idxi[ip][0:M, jb:jb + 1], axis=0)),
                        rd=[b_idx[ip], hnrow], wr=[b_xg2[ip]])

            def prepB(e_):
                ip = e_ % 2
                for jb in range(5):
                    pt, pb = bank()
                    ptb = pt[:].bitcast(BF16)
                    kb.op("pe", lambda e: [e.transpose(ptb[:, kk * 128:(kk + 1) * 128], xg2[ip][:, jb, kk * 128:(kk + 1) * 128], identb)
                                           for kk in range(8)], rd=[b_xg2[ip], b_cbf], wr=[pb])
                    kb.op("act", lambda e: e.activation(out=xgT2[ip][:, :, jb * 128:(jb + 1) * 128],
                                                        in_=ptb.rearrange("p (k t) -> p k t", k=8), func=AF.Copy),
                          rd=[pb], wr=[b_xgT2[ip]])

            def gateup(e_, q4s):
                ip = e_ % 2
                xgT, b_xgT = xgT2[ip], b_xgT2[ip]
                for q4 in q4s:
                    ensure(uic[0])
                    sg_ = slot_of[("g", e_, q4)]
                    su_ = slot_of[("u", e_, q4)]
                    wgv = wring[sg_][:].rearrange("p (kc n) -> p kc n", kc=8)
                    wuv = wring[su_][:].rearrange("p (kc n) -> p kc n", kc=8)
                    for f4 in range(4):
                        fc = q4 * 4 + f4
                        for hf in range(2):
                            c0 = hf * 257
                            pgk, pgb = bank()
                            puk, pub = bank()
                            kb.op("pe", lambda e: [e.matmul(pgk[:, 0:257], wgv[:, kc, f4 * 128:(f4 + 1) * 128],
                                                            xgT[:, kc, c0:c0 + 257], start=(kc == 0), stop=(kc == 7))
                                                   for kc in range(8)], rd=[b_wring[sg_], b_xgT], wr=[pgb])
                            kb.op("pe", lambda e: [e.matmul(puk[:, 0:257], wuv[:, kc, f4 * 128:(f4 + 1) * 128],
                                                            xgT[:, kc, c0:c0 + 257], start=(kc == 0), stop=(kc == 7))
                                                   for kc in range(8)], rd=[b_wring[su_], b_xgT], wr=[pub])
                            qy = ityc[0] % 2
                            ityc[0] += 1
                            kb.op("act", lambda e: e.activation(out=sgl[qy][:], in_=pgk[:, 0:257], func=AF.Silu),
                                  rd=[pgb], wr=[b_sgl[qy]])
                            kb.op("dve", lambda e: e.tensor_tensor(out=hTe[:, fc, c0:c0 + 257], in0=puk[:, 0:257],
                                                                   in1=sgl[qy][:], op=ALU.mult),
                                  rd=[pub, b_sgl[qy]], wr=[b_hTe[fc]])
                    uic[0] += 2
                    ensure(uic[0])

            def down(e_):
                ip = e_ % 2
                sd_ = [slot_of[("d", e_, q4)] for q4 in range(4)]
                for jb in range(5):
                    M = 128 if jb < 4 else 2
                    qy = ityc[0] % 2
                    ityc[0] += 1
                    kb.dma("pool", lambda q: q.indirect_dma_start(
                        out=hg[qy][0:M, :], out_offset=None, in_=hacc_d[:, :],
                        in_offset=bass.IndirectOffsetOnAxis(ap=idxi[ip][0:M, jb:jb + 1], axis=0)),
                        rd=[b_idx[ip], hacc], wr=[b_hg[qy]])
                    for dh in range(2):
                        pdn, pdnb = bank()
                        kb.op("pe", lambda e: [e.matmul(pdn[0:M, :], hTe[:, fc, jb * 128:jb * 128 + M],
                                                        wring[sd_[fc // 4]][:].rearrange("p (f n) -> p f n", f=4)[:, fc % 4, dh * 512:(dh + 1) * 512],
                                                        start=(fc == 0), stop=(fc == 15)) for fc in range(16)],
                              rd=[b_wring[s_] for s_ in sd_] + b_hTe, wr=[pdnb])
                        kb.op("dve", lambda e: e.scalar_tensor_tensor(
                            out=yw[qy][0:M, dh * 512:(dh + 1) * 512], in0=pdn[0:M, :], scalar=afs[ip][0:M, jb:jb + 1],
                            in1=hg[qy][0:M, dh * 512:(dh + 1) * 512], op0=ALU.mult, op1=ALU.add),
                            rd=[pdnb, b_idx[ip], b_hg[qy]], wr=[b_yw[qy]])
                    kb.dma("pool", lambda q: q.indirect_dma_start(
                        out=hacc_d[:, :], out_offset=bass.IndirectOffsetOnAxis(ap=idxi[ip][0:M, jb:jb + 1], axis=0),
                        in_=yw[qy][0:M, :], in_offset=None),
                        rd=[b_yw[qy], b_idx[ip]], wr=[hacc])
                uic[0] += 4

            ensure(0)
            prepA(0)
            prepB(0)
            for e_ in range(NE):
                gateup(e_, [0, 1])
                if e_ + 1 < NE:
                    prepA(e_ + 1)
                gateup(e_, [2, 3])
                if e_ + 1 < NE:
                    prepB(e_ + 1)
                down(e_)

        kb.barrier()
        with ExitStack() as sh:
            kb.enabled = "H" in run
            wfin = sb("wfin", [128, D], F32, sh)
            b_wfin = Buf()
            kb.dma("sp", lambda q: q.dma_start(out=wfin[:], in_=bc_d[:, B_WFIN:B_WFIN + D]), wr=[b_wfin])
            hl = [sb("hl%d" % i, [128, D], F32, sh) for i in range(4)]
            b_hl = [Buf() for _ in range(4)]
            ol = [sb("ol%d" % i, [128, D], F32, sh) for i in range(4)]
            b_ol = [Buf() for _ in range(4)]
            jf = [sb("jf%d" % i, [128, D], BF16, sh) for i in range(2)]
            b_jf = [Buf(), Buf()]
            s1_ = [sb("s1_%d" % i, [128, 1], F32, sh) for i in range(4)]
            r1_ = [sb("r1_%d" % i, [128, 1], F32, sh) for i in range(4)]
            b_s1 = [Buf() for _ in range(4)]
            b_r1 = [Buf() for _ in range(4)]
            outb = Buf()
            for c0 in range(1, NT, 4):
                cs_ = list(range(c0, min(c0 + 4, NT)))
                for i, c in enumerate(cs_):
                    kb.dma("sp", lambda q: q.dma_start(out=hl[i][:], in_=hacc_d[c * 128:(c + 1) * 128, :]), rd=[hacc], wr=[b_hl[i]])
                for i, c in enumerate(cs_):
                    kb.op("act", lambda e: e.activation(out=jf[i % 2][:], in_=hl[i][:], func=AF.Square, accum_out=s1_[i][:]),
                          rd=[b_hl[i]], wr=[b_jf[i % 2], b_s1[i]])
                for i, c in enumerate(cs_):
                    kb.op("act", lambda e: e.activation(out=s1_[i][:], in_=s1_[i][:], func=AF.Sqrt, bias=EPS, scale=1.0 / D),
                          rd=[b_s1[i]], wr=[b_s1[i]])
                for i, c in enumerate(cs_):
                    kb.op("dve", lambda e: e.reciprocal(out=r1_[i][:], in_=s1_[i][:]), rd=[b_s1[i]], wr=[b_r1[i]])
                for i, c in enumerate(cs_):
                    kb.op("dve" if i % 2 == 0 else "pool", lambda e: e.scalar_tensor_tensor(out=ol[i][:], in0=hl[i][:], scalar=r1_[i][:, 0:1], in1=wfin[:],
                                                                  op0=ALU.mult, op1=ALU.mult),
                          rd=[b_hl[i], b_r1[i], b_wfin], wr=[b_ol[i]]) if False else \
                        kb.op("dve", lambda e: e.scalar_tensor_tensor(out=ol[i][:], in0=hl[i][:], scalar=r1_[i][:, 0:1], in1=wfin[:],
                                                                      op0=ALU.mult, op1=ALU.mult),
                              rd=[b_hl[i], b_r1[i], b_wfin], wr=[b_ol[i]])
                    kb.dma("sp", lambda q: q.dma_start(out=out_d[(c - 1) * 128:c * 128, :], in_=ol[i][:]), rd=[b_ol[i]], wr=[outb])
            kb.enabled = True
            kb.barrier()
    return nc


def _host_consts():
    l = np.arange(128)
    Uf = (l[:, None] <= l[None, :]).astype(np.float32)
    Lf = (l[:, None] > l[None, :]).astype(np.float32)
    Ub = (l[:, None] >= l[None, :]).astype(np.float32)
    Lb = (l[:, None] < l[None, :]).astype(np.float32)
    cf = np.zeros((128, C_N), np.float32)
    cf[:, C_UF:C_UF + 128] = Uf
    cf[:, C_LF:C_LF + 128] = Lf
    cf[:, C_UB:C_UB + 128] = Ub
    cf[:, C_LB:C_LB + 128] = Lb
    cf[:, C_ONES:C_ONES + 128] = 1.0
    cf[:, C_ID:C_ID + 128] = np.eye(128, dtype=np.float32)
    cf[:, C_IOTA:C_IOTA + 516] = np.arange(516, dtype=np.float32)[None, :]
    cb = np.zeros((128, CB_N), np.float32)
    cb[:, CB_ID:CB_ID + 128] = np.eye(128)
    cb[:, CB_ONES:CB_ONES + 128] = 1.0
    tv = np.zeros((128, NT, 2), np.float32)
    tv[:, :, 0] = np.arange(NT)[None, :]
    tv[:, :, 1] = np.arange(128)[:, None]
    cb[:, CB_TV:CB_TV + 66] = tv.reshape(128, 66)
    return cf, cb.astype(ml_dtypes.bfloat16)


_NC_CACHE = {}


def kernel(x, meta_tokens, w_norm_mix, w_in, w_conf_dw, b_conf_dw, conf_ln_g, conf_ln_b, w_conf_out,
           w_ssm_conv, b_ssm_conv, ssm_dt_bias, ssm_a_log, ssm_d, w_ssm_norm, w_ssm_out, w_out,
           w_norm_ffn, w_router, w_exp_gate, w_exp_up, w_exp_down, w_norm_final):
    f = lambda a: np.ascontiguousarray(np.asarray(a, dtype=np.float32))
    x = f(x)
    small = np.zeros((128, S_N), np.float32)
    small[:, S_BCONF:S_BCONF + 8] = f(b_conf_dw)[0].reshape(8, 128).T
    small[:, S_LNG:S_LNG + 8] = f(conf_ln_g)[0].reshape(8, 128).T
    small[:, S_LNB:S_LNB + 8] = f(conf_ln_b)[0].reshape(8, 128).T
    small[:, S_WCONF:S_WCONF + 248] = f(w_conf_dw)[0].T.reshape(8, 128, 31).transpose(1, 0, 2).reshape(128, 248)
    small[:, S_WSSM:S_WSSM + 168] = f(w_ssm_conv)[0].T.reshape(24, 128, 7).transpose(1, 0, 2).reshape(128, 168)
    small[:, S_BSSM:S_BSSM + 24] = f(b_ssm_conv)[0].reshape(24, 128).T
    row = np.concatenate([f(w_norm_mix)[0], f(w_ssm_norm)[0], f(w_norm_ffn)[0], f(w_norm_final),
                          f(ssm_dt_bias)[0].reshape(64), f(ssm_a_log)[0].reshape(64), f(ssm_d)[0]])
    bcast = np.ascontiguousarray(np.broadcast_to(row[None, :], (128, B_N)))
    cf, cb = _host_consts()
    if "nc" not in _NC_CACHE:
        _NC_CACHE["nc"] = build_program()
    nc = _NC_CACHE["nc"]
    shared = {
        "meta": f(meta_tokens), "w_in": f(w_in)[0], "w_conf_out": f(w_conf_out)[0], "w_ssm_out": f(w_ssm_out)[0],
        "w_out": f(w_out)[0], "w_router": f(w_router)[0], "w_eg": f(w_exp_gate)[0], "w_eu": f(w_exp_up)[0],
        "w_ed": f(w_exp_down)[0], "smallT": small, "bcast": bcast, "cf32": cf, "cbf": cb,
    }
    in_maps = [dict(shared, x=x[b]) for b in range(8)]
    res = run_bass_kernel_spmd(nc, in_maps, core_ids=list(range(8)))
    return np.stack([np.asarray(r["out"], dtype=np.float32) for r in res.results], axis=0)
```

```python
import os
import numpy as np
import ml_dtypes
from contextlib import ExitStack
import concourse.bass as bass
import concourse.mybir as mybir
from concourse.bass_utils import run_bass_kernel_spmd

F32 = mybir.dt.float32
F32R = mybir.dt.float32r
BF16 = mybir.dt.bfloat16
I32 = mybir.dt.int32
AF = mybir.ActivationFunctionType
ALU = mybir.AluOpType
AX = mybir.AxisListType

T = 4224
NT = 33
D = 1024
CAP = 514
NE = 16
TBS = [(i * 512, 512) for i in range(8)] + [(4096, 128)]
EPS = 1e-6
S_BCONF, S_LNG, S_LNB, S_WCONF, S_WSSM, S_BSSM, S_N = 0, 8, 16, 24, 272, 440, 464
B_WMIX, B_WSSM, B_WFFN, B_WFIN, B_DTB, B_ALOG, B_DSK, B_N = 0, 1024, 3072, 4096, 5120, 5184, 5248, 5280
C_UF, C_LF, C_UB, C_LB, C_ONES, C_ID, C_IOTA, C_N = 0, 128, 256, 384, 512, 640, 768, 768 + 516
CB_ID, CB_ONES, CB_TV, CB_N = 0, 128, 256, 256 + 66


class Buf:
    __slots__ = ("w", "r")

    def __init__(self):
        self.w = None
        self.r = {}


class KB:
    EPOCH = 2048

    def __init__(self, nc, st):
        self.nc = nc
        self.st = st
        self.eng = dict(pe=nc.tensor, act=nc.scalar, dve=nc.vector, pool=nc.gpsimd, sp=nc.sync)
        self.cnt = {e: 0 for e in self.eng}
        self.sems = {e: [] for e in self.eng}
        self.known = {e: {} for e in self.eng}
        self.semh = []
        self.origin = []
        self.dq = {}
        self.enabled = True

    def newsem(self, origin):
        h = self.st.enter_context(self.nc.semaphore("s%d" % len(self.semh)))
        self.semh.append(h)
        self.origin.append(origin)
        return len(self.semh) - 1

    def wait(self, e, toks, keep_one=False):
        need = {}
        kn = self.known[e]
        for sid, val in toks:
            if e == "pe" and self.origin[sid] == "pe":
                continue
            if kn.get(sid, 0) < val and need.get(sid, 0) < val:
                need[sid] = val
        items = list(need.items())
        attach = items.pop() if (keep_one and items) else None
        for sid, val in items:
            self.eng[e].wait_ge(self.semh[sid], val)
            kn[sid] = val
        if attach is not None:
            kn[attach[0]] = attach[1]
        return attach

    def _deps(self, rd, wr):
        toks = []
        for b in rd:
            if b.w is not None:
                toks.append(b.w)
        for b in wr:
            if b.w is not None:
                toks.append(b.w)
            toks.extend(b.r.items())
        return toks

    def _mark(self, tok, rd, wr):
        sid, val = tok
        for b in rd:
            if b.r.get(sid, 0) < val:
                b.r[sid] = val
        for b in wr:
            b.w = tok
            b.r = {}

    def op(self, e, fn, rd=(), wr=()):
        if not self.enabled:
            return None
        attach = self.wait(e, self._deps(rd, wr), keep_one=True)
        ins = fn(self.eng[e])
        if isinstance(ins, (list, tuple)):
            first, ins = ins[0], ins[-1]
        else:
            first = ins
        if attach is not None:
            first.wait_op(self.semh[attach[0]], attach[1], "sem-ge")
        self.cnt[e] += 1
        k = self.cnt[e]
        ep = (k - 1) // self.EPOCH
        while len(self.sems[e]) <= ep:
            self.sems[e].append(self.newsem(e))
        sid = self.sems[e][ep]
        val = (k - 1) % self.EPOCH + 1
        ins.then_inc(self.semh[sid], 1)
        tok = (sid, val)
        self._mark(tok, rd, wr)
        return tok

    def barrier(self):
        toks = []
        for e in self.eng:
            if self.cnt[e] > 0:
                k = self.cnt[e]
                toks.append((self.sems[e][(k - 1) // self.EPOCH], (k - 1) % self.EPOCH + 1))
        for pool in self.dq.values():
            toks.extend((sid, val) for sid, val in zip(pool["sids"], pool["vals"]) if val > 0)
        for e in self.eng:
            self.wait(e, toks)

    def dma(self, q, fn, rd=(), wr=(), nslots=16):
        if not self.enabled:
            return None
        self.wait(q, self._deps(rd, wr))
        pool = self.dq.setdefault(q, dict(sids=[], vals=[], n=0))
        i = pool["n"] % nslots
        pool["n"] += 1
        if len(pool["sids"]) <= i:
            pool["sids"].append(self.newsem("dma"))
            pool["vals"].append(0)
        sid = pool["sids"][i]
        if pool["vals"][i] > 0:
            self.wait(q, [(sid, pool["vals"][i])])
        ins = fn(self.eng[q])
        val = pool["vals"][i] + 16
        pool["vals"][i] = val
        ins.then_inc(self.semh[sid], 16)
        tok = (sid, val)
        self._mark(tok, rd, wr)
        return tok


def build_program(run="ABbCDEFGH", debug=False):
    nc = bass.Bass("TRN2", target_bir_lowering=False)
    dt_ = nc.dram_tensor
    x_d = dt_("x", [4096, D], F32, kind="ExternalInput").ap()
    meta_d = dt_("meta", [16, D], F32, kind="ExternalInput").ap()
    w_in = dt_("w_in", [D, 9280], F32, kind="ExternalInput").ap()
    w_co = dt_("w_conf_out", [D, D], F32, kind="ExternalInput").ap()
    w_so = dt_("w_ssm_out", [2048, D], F32, kind="ExternalInput").ap()
    w_o = dt_("w_out", [D, D], F32, kind="ExternalInput").ap()
    w_r = dt_("w_router", [D, NE], F32, kind="ExternalInput").ap()
    ned = NE if ("G" in run or os.environ.get("FORCE_NE") == "1") else 1
    w_eg = dt_("w_eg", [ned, D, 2048], F32, kind="ExternalInput").ap()
    w_eu = dt_("w_eu", [ned, D, 2048], F32, kind="ExternalInput").ap()
    w_ed = dt_("w_ed", [ned, 2048, D], F32, kind="ExternalInput").ap()
    small_d = dt_("smallT", [128, S_N], F32, kind="ExternalInput").ap()
    bc_d = dt_("bcast", [128, B_N], F32, kind="ExternalInput").ap()
    cf_d = dt_("cf32", [128, C_N], F32, kind="ExternalInput").ap()
    cb_d = dt_("cbf", [128, CB_N], BF16, kind="ExternalInput").ap()
    out_d = dt_("out", [4096, D], F32, kind="ExternalOutput").ap()
    skw = dict(kind="ExternalOutput") if debug else {}
    conv_d = dt_("conv_s", [128, 8, T], BF16, **skw).ap()
    m1_d = dt_("m1_s", [128, 8, T], BF16, **skw).ap()
    g2_d = dt_("g2_s", [128, 8, T], BF16, **skw).ap()
    xbc_d = dt_("xbc_s", [128, 24, T], BF16, **skw).ap()
    sz_d = dt_("sz_s", [NT, 128, 2048], BF16, **skw).ap()
    yb_d = dt_("yb_s", [NT, 128, 2048], BF16, **skw).ap()
    yn_d = dt_("yn_s", [NT, 128, 2048], BF16, **skw).ap()
    hn_d = dt_("hn_s", [T, D], BF16, **skw).ap()
    hacc_d = dt_("hacc_s", [T, D], F32, **skw).ap()
    uT_dbg = dt_("uT_s", [128, 8, T], BF16, **skw).ap() if debug else None
    dt_dbg = dt_("dt_s", [128, NT, 64], F32, **skw).ap() if debug else None
    aff_dbg = dt_("aff_s", [128, NT, NE], F32, **skw).ap() if debug else None
    posm_dbg = dt_("posm_s", [128, NT, NE], F32, **skw).ap() if debug else None

    win_v = w_in.rearrange("(kc p) n -> p kc n", p=128)

    with ExitStack() as st:
        kb = KB(nc, st)

        def sb(name, shape, dtype, stack=None):
            return (stack or st).enter_context(nc.sbuf_tensor("sb_" + name, shape, dtype))

        ps = [st.enter_context(nc.psum_tensor("ps%d" % i, [128, 512], F32)) for i in range(8)]
        psb = [Buf() for _ in range(8)]
        psn = [0]

        def bank():
            i = psn[0] % 8
            psn[0] += 1
            return ps[i], psb[i]

        cf = sb("cf", [128, C_N], F32)
        cbf = sb("cbf", [128, CB_N], BF16)
        sm = sb("sm", [128, S_N], F32)
        b_cf, b_cbf, b_sm = Buf(), Buf(), Buf()
        kb.dma("sp", lambda q: q.dma_start(out=cf[:], in_=cf_d), wr=[b_cf])
        kb.dma("sp", lambda q: q.dma_start(out=cbf[:], in_=cb_d), wr=[b_cbf])
        kb.dma("sp", lambda q: q.dma_start(out=sm[:], in_=small_d), wr=[b_sm])
        identb = cbf[:, CB_ID:CB_ID + 128]
        onesb = cbf[:, CB_ONES:CB_ONES + 128]
        identf = cf[:, C_ID:C_ID + 128]

        dtall = sb("dtall", [128, NT, 64], F32)
        b_dt = [Buf() for _ in range(NT)]

        def load_x_tile(i, xin, b_xin):
            if i == 0:
                kb.op("dve", lambda e: e.memset(xin[:], 0.0), wr=[b_xin])
                kb.dma("sp", lambda q: q.dma_start(out=xin[112:128, :], in_=meta_d), wr=[b_xin])
            else:
                kb.dma("sp", lambda q: q.dma_start(out=xin[:], in_=x_d[(i - 1) * 128:i * 128, :]), wr=[b_xin])

        def rms_rstd(src, b_src, junk, b_junk, ss, b_ss, rstd, b_rstd, n):
            kb.op("act", lambda e: e.activation(out=junk, in_=src, func=AF.Square, accum_out=ss[:]),
                  rd=(b_src if isinstance(b_src, list) else [b_src]), wr=[b_junk, b_ss])
            kb.op("act", lambda e: e.activation(out=ss[:], in_=ss[:], func=AF.Sqrt, bias=EPS, scale=1.0 / n),
                  rd=[b_ss], wr=[b_ss])
            kb.op("dve", lambda e: e.reciprocal(out=rstd[:], in_=ss[:]), rd=[b_ss], wr=[b_rstd])

        with ExitStack() as s1:
            uT = sb("uT", [128, 8, T], BF16, s1)
            b_uT = [Buf() for _ in range(NT)]

            def uT_bufs(t0, n):
                return b_uT[t0 // 128:(t0 + n) // 128]

            with ExitStack() as sa:
                kb.enabled = "A" in run
                wmix = sb("wmix", [128, D], F32, sa)
                b_wmix = Buf()
                kb.dma("sp", lambda q: q.dma_start(out=wmix[:], in_=bc_d[:, B_WMIX:B_WMIX + D]), wr=[b_wmix])
                xin = [sb("xinA%d" % i, [128, D], F32, sa) for i in range(4)]
                b_xin = [Buf() for _ in range(4)]
                junk = [sb("junkA%d" % i, [128, D], BF16, sa) for i in range(2)]
                b_junk = [Buf(), Buf()]
                ub = [sb("ubA%d" % i, [128, D], BF16, sa) for i in range(4)]
                b_ub = [Buf() for _ in range(4)]
                ss = [sb("ssA%d" % i, [128, 1], F32, sa) for i in range(4)]
                rs = [sb("rsA%d" % i, [128, 1], F32, sa) for i in range(4)]
                b_ss = [Buf() for _ in range(4)]
                b_rs = [Buf() for _ in range(4)]
                for i0 in range(0, NT, 4):
                    tiles = list(range(i0, min(i0 + 4, NT)))
                    for p, i in enumerate(tiles):
                        load_x_tile(i, xin[p], b_xin[p])
                    for p, i in enumerate(tiles):
                        kb.op("act", lambda e: e.activation(out=junk[p % 2][:], in_=xin[p][:], func=AF.Square, accum_out=ss[p][:]),
                              rd=[b_xin[p]], wr=[b_junk[p % 2], b_ss[p]])
                    for p, i in enumerate(tiles):
                        kb.op("act", lambda e: e.activation(out=ss[p][:], in_=ss[p][:], func=AF.Sqrt, bias=EPS, scale=1.0 / D),
                              rd=[b_ss[p]], wr=[b_ss[p]])
                    for p, i in enumerate(tiles):
                        kb.op("dve", lambda e: e.reciprocal(out=rs[p][:], in_=ss[p][:]), rd=[b_ss[p]], wr=[b_rs[p]])
                    for p, i in enumerate(tiles):
                        kb.op("dve", lambda e: e.scalar_tensor_tensor(out=ub[p][:], in0=xin[p][:], scalar=rs[p][:, 0:1],
                                                                       in1=wmix[:], op0=ALU.mult, op1=ALU.mult),
                              rd=[b_xin[p], b_rs[p], b_wmix], wr=[b_ub[p]])
                    for p, i in enumerate(tiles):
                        pt, pb = bank()
                        ptb = pt[:].bitcast(BF16)
                        kb.op("pe", lambda e: [e.transpose(ptb[:, kc * 128:(kc + 1) * 128],
                                                           ub[p][:, kc * 128:(kc + 1) * 128], identb)
                                               for kc in range(8)],
                              rd=[b_ub[p], b_cbf], wr=[pb])
                        kb.op("act", lambda e: e.activation(out=uT[:, :, i * 128:(i + 1) * 128],
                                                            in_=ptb.rearrange("p (k t) -> p k t", k=8), func=AF.Copy),
                              rd=[pb], wr=[b_uT[i]])

            kb.barrier()
            if debug:
                kb.enabled = "A" in run
                kb.dma("sp", lambda q: q.dma_start(out=uT_dbg, in_=uT[:]), rd=b_uT, wr=[Buf()])
            with ExitStack() as sbk:
                kb.enabled = "B" in run
                wA = [sb("wA%d" % i, [128, 8, 256], BF16, sbk) for i in range(2)]
                b_wA = [Buf(), Buf()]
                cT = [sb("cT%d" % i, [128, T + 30], BF16, sbk) for i in range(2)]
                b_cT = [Buf(), Buf()]
                dg = [sb("dg%d" % i, [128, 31, 128], BF16, sbk) for i in range(2)]
                b_dg = [Buf(), Buf()]
                sgt = [sb("sgt%d" % i, [128, 512], BF16, sbk) for i in range(2)]
                b_sgt = [Buf(), Buf()]
                cv = [sb("cv%d" % i, [128, 512], BF16, sbk) for i in range(2)]
                b_cv = [Buf(), Buf()]
                b_conv = [[Buf() for _ in TBS] for _ in range(8)]
                for i in range(2):
                    kb.op("pool", lambda e: e.memset(cT[i][:], 0.0), wr=[b_cT[i]])
                it = 0
                for j in range(8):
                    p = j % 2
                    kb.dma("pool", lambda q: q.dma_start(out=wA[p][:, :, 0:128], in_=win_v[:, :, j * 128:(j + 1) * 128]),
                           wr=[b_wA[p]])
                    kb.dma("pool", lambda q: q.dma_start(out=wA[p][:, :, 128:256],
                                                         in_=win_v[:, :, 1024 + j * 128:1024 + (j + 1) * 128]),
                           wr=[b_wA[p]])
                    for k in range(31):
                        kb.op("dve", lambda e: e.tensor_scalar(out=dg[p][:, k, :], in0=identb,
                                                               scalar1=sm[:, S_WCONF + j * 31 + k:S_WCONF + j * 31 + k + 1],
                                                               scalar2=None, op0=ALU.mult),
                              rd=[b_cbf, b_sm], wr=[b_dg[p]])
                    for (t0, n) in TBS:
                        pa, pab = bank()
                        pg, pgb = bank()
                        kb.op("pe", lambda e: [e.matmul(pa[:, 0:n], wA[p][:, kc, 0:128], uT[:, kc, t0:t0 + n],
                                                        start=(kc == 0), stop=(kc == 7)) for kc in range(8)],
                              rd=[b_wA[p]] + uT_bufs(t0, n), wr=[pab])
                        kb.op("pe", lambda e: [e.matmul(pg[:, 0:n], wA[p][:, kc, 128:256], uT[:, kc, t0:t0 + n],
                                                        start=(kc == 0), stop=(kc == 7)) for kc in range(8)],
                              rd=[b_wA[p]] + uT_bufs(t0, n), wr=[pgb])
                        q = it % 2
                        it += 1
                        kb.op("act", lambda e: e.activation(out=sgt[q][:, 0:n], in_=pg[:, 0:n], func=AF.Sigmoid),
                              rd=[pgb], wr=[b_sgt[q]])
                        kb.op("dve", lambda e: e.tensor_tensor(out=cT[p][:, 15 + t0:15 + t0 + n], in0=pa[:, 0:n],
                                                               in1=sgt[q][:, 0:n], op=ALU.mult),
                              rd=[pab, b_sgt[q]], wr=[b_cT[p]])
                    for ti, (t0, n) in enumerate(TBS):
                        pc, pcb = bank()
                        kb.op("pe", lambda e: [e.matmul(pc[:, 0:n], dg[p][:, k, :], cT[p][:, t0 + k:t0 + k + n],
                                                        start=(k == 0), stop=(k == 30)) for k in range(31)],
                              rd=[b_dg[p], b_cT[p]], wr=[pcb])
                        q = it % 2
                        it += 1
                        kb.op("act", lambda e: e.activation(out=cv[q][:, 0:n], in_=pc[:, 0:n], func=AF.Identity,
                                                            bias=sm[:, S_BCONF + j:S_BCONF + j + 1]),
                              rd=[pcb, b_sm], wr=[b_cv[q]])
                        kb.dma("sp", lambda qq: qq.dma_start(out=conv_d[:, j, t0:t0 + n], in_=cv[q][:, 0:n]),
                               rd=[b_cv[q]], wr=[b_conv[j][ti]])

            kb.barrier()
            with ExitStack() as sb2:
                kb.enabled = "b" in run
                wco = sb("wco", [128, 8, D], BF16, sb2)
                wg1 = sb("wg1", [128, 8, D], BF16, sb2)
                wg2 = sb("wg2", [128, 8, D], BF16, sb2)
                b_wco, b_wg1, b_wg2 = Buf(), Buf(), Buf()
                kb.dma("pool", lambda q: q.dma_start(out=wco[:], in_=w_co.rearrange("(kc p) n -> p kc n", p=128)),
                       wr=[b_wco])
                kb.dma("pool", lambda q: q.dma_start(out=wg1[:], in_=win_v[:, :, 7232:7232 + D]), wr=[b_wg1])
                kb.dma("pool", lambda q: q.dma_start(out=wg2[:], in_=win_v[:, :, 8256:8256 + D]), wr=[b_wg2])
                cvb = [sb("cvb%d" % i, [128, 8, 512], BF16, sb2) for i in range(2)]
                b_cvb = [Buf(), Buf()]
                sq = sb("sq", [128, 8, 512], BF16, sb2)
                b_sq = Buf()
                mean = sb("mean", [128, 512], F32, sb2)
                msq = sb("msq", [128, 512], F32, sb2)
                rstd = sb("rstdb", [128, 512], F32, sb2)
                nmr = sb("nmr", [128, 512], F32, sb2)
                b_mean, b_msq, b_rstd, b_nmr = Buf(), Buf(), Buf(), Buf()
                t2 = [sb("t2_%d" % i, [128, 512], F32, sb2) for i in range(2)]
                b_t2 = [Buf(), Buf()]
                cs2 = [sb("cs%d" % i, [128, 8, 512], BF16, sb2) for i in range(2)]
                b_cs2 = [Buf(), Buf()]
                sg = [sb("sg%d" % i, [128, 512], BF16, sb2) for i in range(2)]
                b_sg = [Buf(), Buf()]
                m1 = [sb("m1_0", [128, 8, 512], BF16, sb2)] * 2
                b_m1 = [Buf()] * 2
                g2 = [sb("g2_0", [128, 8, 512], BF16, sb2)] * 2
                b_g2 = [Buf()] * 2
                b_m1d = [Buf() for _ in TBS]
                b_g2d = [Buf() for _ in TBS]
                itc = [0]

                def b_LN(ti):
                    t0, n = TBS[ti]
                    it = itc[0]
                    p = ti % 2
                    cs, b_cs = cs2[p], b_cs2[p]
                    kb.dma("sp", lambda q: q.dma_start(out=cvb[p][:, :, 0:n], in_=conv_d[:, :, t0:t0 + n]),
                           rd=[b_conv[j][ti] for j in range(8)], wr=[b_cvb[p]])
                    kb.op("pool", lambda e: e.tensor_tensor(out=sq[:, :, 0:n], in0=cvb[p][:, :, 0:n],
                                                            in1=cvb[p][:, :, 0:n], op=ALU.mult),
                          rd=[b_cvb[p]], wr=[b_sq])
                    p1, p1b = bank()
                    p2, p2b = bank()
                    kb.op("pe", lambda e: [e.matmul(p1[:, 0:n], onesb, cvb[p][:, j, 0:n], start=(j == 0), stop=(j == 7))
                                           for j in range(8)], rd=[b_cbf, b_cvb[p]], wr=[p1b])
                    kb.op("pe", lambda e: [e.matmul(p2[:, 0:n], onesb, sq[:, j, 0:n], start=(j == 0), stop=(j == 7))
                                           for j in range(8)], rd=[b_cbf, b_sq], wr=[p2b])
                    kb.op("dve", lambda e: e.tensor_scalar(out=mean[:, 0:n], in0=p1[:, 0:n], scalar1=1.0 / D,
                                                           scalar2=None, op0=ALU.mult), rd=[p1b], wr=[b_mean])
                    kb.op("dve", lambda e: e.tensor_tensor(out=msq[:, 0:n], in0=mean[:, 0:n], in1=mean[:, 0:n],
                                                           op=ALU.mult), rd=[b_mean], wr=[b_msq])
                    kb.op("dve", lambda e: e.scalar_tensor_tensor(out=msq[:, 0:n], in0=p2[:, 0:n], scalar=1.0 / D,
                                                                  in1=msq[:, 0:n], op0=ALU.mult, op1=ALU.subtract),
                          rd=[p2b, b_msq], wr=[b_msq])
                    kb.op("act", lambda e: e.activation(out=msq[:, 0:n], in_=msq[:, 0:n], func=AF.Sqrt, bias=EPS),
                          rd=[b_msq], wr=[b_msq])
                    kb.op("dve", lambda e: e.reciprocal(out=rstd[:, 0:n], in_=msq[:, 0:n]), rd=[b_msq], wr=[b_rstd])
                    kb.op("dve", lambda e: e.scalar_tensor_tensor(out=nmr[:, 0:n], in0=mean[:, 0:n], scalar=-1.0,
                                                                  in1=rstd[:, 0:n], op0=ALU.mult, op1=ALU.mult),
                          rd=[b_mean, b_rstd], wr=[b_nmr])
                    for j in range(8):
                        q = it % 2
                        it += 1
                        kb.op("dve", lambda e: e.tensor_tensor(out=t2[q][:, 0:n], in0=cvb[p][:, j, 0:n],
                                                               in1=rstd[:, 0:n], op=ALU.mult),
                              rd=[b_cvb[p], b_rstd], wr=[b_t2[q]])
                        kb.op("dve", lambda e: e.tensor_tensor(out=t2[q][:, 0:n], in0=t2[q][:, 0:n],
                                                               in1=nmr[:, 0:n], op=ALU.add),
                              rd=[b_nmr], wr=[b_t2[q]])
                        kb.op("act", lambda e: e.activation(out=cs[:, j, 0:n], in_=t2[q][:, 0:n], func=AF.Silu,
                                                            scale=sm[:, S_LNG + j:S_LNG + j + 1],
                                                            bias=sm[:, S_LNB + j:S_LNB + j + 1]),
                              rd=[b_t2[q], b_sm], wr=[b_cs])
                    itc[0] = it

                def b_MM(ti):
                    t0, n = TBS[ti]
                    it = itc[0]
                    p = ti % 2
                    cs, b_cs = cs2[p], b_cs2[p]
                    for dc in range(8):
                        pbk, pbb = bank()
                        pgk, pgb = bank()
                        kb.op("pe", lambda e: [e.matmul(pbk[:, 0:n], wco[:, kc, dc * 128:(dc + 1) * 128], cs[:, kc, 0:n],
                                                        start=(kc == 0), stop=(kc == 7)) for kc in range(8)],
                              rd=[b_wco, b_cs], wr=[pbb])
                        kb.op("pe", lambda e: [e.matmul(pgk[:, 0:n], wg1[:, kc, dc * 128:(dc + 1) * 128],
                                                        uT[:, kc, t0:t0 + n], start=(kc == 0), stop=(kc == 7))
                                               for kc in range(8)],
                              rd=[b_wg1] + uT_bufs(t0, n), wr=[pgb])
                        q = it % 2
                        it += 1
                        kb.op("act", lambda e: e.activation(out=sg[q][:, 0:n], in_=pgk[:, 0:n], func=AF.Sigmoid),
                              rd=[pgb], wr=[b_sg[q]])
                        kb.op("dve", lambda e: e.tensor_tensor(out=m1[p][:, dc, 0:n], in0=pbk[:, 0:n],
                                                               in1=sg[q][:, 0:n], op=ALU.mult),
                              rd=[pbb, b_sg[q]], wr=[b_m1[p]])
                        pg2, pg2b = bank()
                        kb.op("pe", lambda e: [e.matmul(pg2[:, 0:n], wg2[:, kc, dc * 128:(dc + 1) * 128],
                                                        uT[:, kc, t0:t0 + n], start=(kc == 0), stop=(kc == 7))
                                               for kc in range(8)],
                              rd=[b_wg2] + uT_bufs(t0, n), wr=[pg2b])
                        kb.op("act", lambda e: e.activation(out=g2[p][:, dc, 0:n], in_=pg2[:, 0:n], func=AF.Sigmoid),
                              rd=[pg2b], wr=[b_g2[p]])
                    kb.dma("sp", lambda qq: qq.dma_start(out=m1_d[:, :, t0:t0 + n], in_=m1[p][:, :, 0:n]),
                           rd=[b_m1[p]], wr=[b_m1d[ti]])
                    kb.dma("sp", lambda qq: qq.dma_start(out=g2_d[:, :, t0:t0 + n], in_=g2[p][:, :, 0:n]),
                           rd=[b_g2[p]], wr=[b_g2d[ti]])
                    itc[0] = it

                b_LN(0)
                for ti in range(len(TBS)):
                    if ti + 1 < len(TBS):
                        b_LN(ti + 1)
                    b_MM(ti)

            kb.barrier()
            with ExitStack() as sc:
                kb.enabled = "C" in run
                wz = sb("wz", [128, 8, 2048], BF16, sc)
                wdt = sb("wdt", [128, 8, 64], BF16, sc)
                b_wz, b_wdt = Buf(), Buf()
                for qq_ in range(4):
                    kb.dma("pool", lambda q: q.dma_start(out=wz[:, :, qq_ * 512:(qq_ + 1) * 512],
                                                         in_=win_v[:, :, 2048 + qq_ * 512:2048 + (qq_ + 1) * 512]),
                           wr=[b_wz])
                kb.dma("pool", lambda q: q.dma_start(out=wdt[:], in_=win_v[:, :, 7168:7232]), wr=[b_wdt])
                dtb = sb("dtb", [128, 64], F32, sc)
                b_dtb = Buf()
                kb.dma("sp", lambda q: q.dma_start(out=dtb[:], in_=bc_d[:, B_DTB:B_DTB + 64]), wr=[b_dtb])
                szt = [sb("szt%d" % i, [128, 2048], BF16, sc) for i in range(2)]
                b_szt = [Buf(), Buf()]
                b_szd = [Buf() for _ in range(NT)]
                dte = sb("dte", [128, 64], F32, sc)
                b_dte = Buf()
                for i in range(NT):
                    p = i % 2
                    for qz in range(4):
                        pz, pzb = bank()
                        kb.op("pe", lambda e: [e.matmul(pz[:, :], uT[:, kc, i * 128:(i + 1) * 128],
                                                        wz[:, kc, qz * 512:(qz + 1) * 512],
                                                        start=(kc == 0), stop=(kc == 7)) for kc in range(8)],
                              rd=[b_wz, b_uT[i]], wr=[pzb])
                        kb.op("act", lambda e: e.activation(out=szt[p][:, qz * 512:(qz + 1) * 512], in_=pz[:, :],
                                                            func=AF.Silu), rd=[pzb], wr=[b_szt[p]])
                    kb.dma("sp", lambda q: q.dma_start(out=sz_d[i], in_=szt[p][:]), rd=[b_szt[p]], wr=[b_szd[i]])
                    pd, pdb = bank()
                    kb.op("pe", lambda e: [e.matmul(pd[:, 0:64], uT[:, kc, i * 128:(i + 1) * 128], wdt[:, kc, :],
                                                    start=(kc == 0), stop=(kc == 7)) for kc in range(8)],
                          rd=[b_wdt, b_uT[i]], wr=[pdb])
                    kb.op("dve", lambda e: e.tensor_tensor(out=dte[:], in0=pd[:, 0:64], in1=dtb[:], op=ALU.add),
                          rd=[pdb, b_dtb], wr=[b_dte])
                    kb.op("act", lambda e: e.activation(out=dte[:], in_=dte[:], func=AF.Exp), rd=[b_dte], wr=[b_dte])
                    kb.op("act", lambda e: e.activation(out=dtall[:, i, :], in_=dte[:], func=AF.Ln, bias=1.0),
                          rd=[b_dte], wr=[b_dt[i]])
                    if i == 0:
                        kb.op("dve", lambda e: e.memset(dtall[0:96, 0, :], 0.0), wr=[b_dt[0]])
                        kb.op("dve", lambda e: e.memset(dtall[96:112, 0, :], 0.0), wr=[b_dt[0]])
                if debug:
                    kb.dma("sp", lambda q: q.dma_start(out=dt_dbg, in_=dtall[:]), rd=b_dt, wr=[Buf()])
                wX = [sb("wX%d" % i, [128, 8, 128], BF16, sc) for i in range(2)]
                b_wX = [Buf(), Buf()]
                xT = [sb("xT%d" % i, [128, T + 6], BF16, sc) for i in range(2)]
                b_xT = [Buf(), Buf()]
                dg7 = [sb("dg7_%d" % i, [128, 7, 128], BF16, sc) for i in range(2)]
                b_dg7 = [Buf(), Buf()]
                xc = [sb("xc%d" % i, [128, 512], BF16, sc) for i in range(2)]
                b_xc = [Buf(), Buf()]
                b_xbcd = [[Buf() for _ in TBS] for _ in range(24)]
                for i in range(2):
                    kb.op("pool", lambda e: e.memset(xT[i][:], 0.0), wr=[b_xT[i]])
                it = 0
                for j in range(24):
                    p = j % 2
                    kb.dma("pool", lambda q: q.dma_start(out=wX[p][:], in_=win_v[:, :, 4096 + j * 128:4096 + (j + 1) * 128]),
                           wr=[b_wX[p]])
                    for k in range(7):
                        kb.op("dve", lambda e: e.tensor_scalar(out=dg7[p][:, k, :], in0=identb,
                                                               scalar1=sm[:, S_WSSM + j * 7 + k:S_WSSM + j * 7 + k + 1],
                                                               scalar2=None, op0=ALU.mult),
                              rd=[b_cbf, b_sm], wr=[b_dg7[p]])
                    for (t0, n) in TBS:
                        px, pxb = bank()
                        kb.op("pe", lambda e: [e.matmul(px[:, 0:n], wX[p][:, kc, :], uT[:, kc, t0:t0 + n],
                                                        start=(kc == 0), stop=(kc == 7)) for kc in range(8)],
                              rd=[b_wX[p]] + uT_bufs(t0, n), wr=[pxb])
                        kb.op("act", lambda e: e.activation(out=xT[p][:, 3 + t0:3 + t0 + n], in_=px[:, 0:n], func=AF.Copy),
                              rd=[pxb], wr=[b_xT[p]])
                    for ti, (t0, n) in enumerate(TBS):
                        pc, pcb = bank()
                        kb.op("pe", lambda e: [e.matmul(pc[:, 0:n], dg7[p][:, k, :], xT[p][:, t0 + k:t0 + k + n],
                                                        start=(k == 0), stop=(k == 6)) for k in range(7)],
                              rd=[b_dg7[p], b_xT[p]], wr=[pcb])
                        q = it % 2
                        it += 1
                        kb.op("act", lambda e: e.activation(out=xc[q][:, 0:n], in_=pc[:, 0:n], func=AF.Silu,
                                                            bias=sm[:, S_BSSM + j:S_BSSM + j + 1]),
                              rd=[pcb, b_sm], wr=[b_xc[q]])
                        if ti == 0:
                            kb.op("dve", lambda e: e.memset(xc[q][:, 0:112], 0.0), wr=[b_xc[q]])
                        kb.dma("sp", lambda qq: qq.dma_start(out=xbc_d[:, j, t0:t0 + n], in_=xc[q][:, 0:n]),
                               rd=[b_xc[q]], wr=[b_xbcd[j][ti]])

        kb.barrier()
        hnrow = Buf()
        hacc = Buf()
        b_ynd = [Buf() for _ in range(NT)]
        with ExitStack() as sd:
            kb.enabled = ("D" in run) or ("E" in run)
            abc = sb("abc", [128, 160], F32, sd)
            b_abc = Buf()
            kb.dma("sp", lambda q: q.dma_start(out=abc[:], in_=bc_d[:, B_DTB:B_DTB + 160]), wr=[b_abc])
            Aneg = sb("Aneg", [128, 64], F32, sd)
            b_A = Buf()
            kb.op("act", lambda e: e.activation(out=Aneg[:], in_=abc[:, 64:128], func=AF.Exp), rd=[b_abc], wr=[b_A])
            kb.op("dve", lambda e: e.tensor_scalar(out=Aneg[:], in0=Aneg[:], scalar1=-1.0, scalar2=None, op0=ALU.mult),
                  rd=[b_A], wr=[b_A])

            def dbl(name, shape, dtype):
                return [sb("%s_%d" % (name, i), shape, dtype, sd) for i in range(2)], [Buf(), Buf()]

            XT, b_XT = dbl("XT", [128, 24, 128], BF16)
            xs_tm2, b_xs2 = dbl("xs_tm", [128, 2048], BF16)
            b_tm2, b_btm2 = dbl("b_tm", [128, 512], BF16)
            cbm2, b_cbm2 = dbl("cbm", [128, 4, 128], BF16)
            a322, b_a322 = dbl("a32", [128, 32], F32)
            rhsA2, b_rhsA2 = dbl("rhsA", [128, 32, 128], F32)
            E2_, b_E2 = dbl("E", [128, 32, 128], BF16)
            dec2, b_dec2 = dbl("dec", [128, 96], F32)
            w22, b_w22 = dbl("w2", [128, 32], F32)
            xd2, b_xd2 = dbl("xd", [128, 32, 64], BF16)
            xdd2, b_xdd2 = dbl("xdd", [128, 32, 64], BF16)
            yacc2, b_yacc2 = dbl("yacc", [128, 2048], F32)
            S32 = sb("S32", [128, 2048], F32, sd)
            Sbf = sb("Sbf", [128, 2048], BF16, sd)
            b_S32g = [Buf() for _ in range(4)]
            b_Sbfg = [Buf() for _ in range(4)]
            b_Eg2 = [[Buf() for _ in range(4)] for _ in range(2)]
            b_yaccg2 = [[Buf() for _ in range(4)] for _ in range(2)]
            tmp = [sb("tmpo%d" % i, [128, 512], F32, sd) for i in range(2)]
            b_tmp = [Buf(), Buf()]
            b_ybd = [Buf() for _ in range(NT)]
            nchunk = [0]

            class Ctx:
                pass

            SEGR = os.environ.get("SEGR", "1") == "1"
            LR = sb("LR", [128, 2, 128], F32R, sd)
            b_LR = Buf()
            if SEGR:
                kb.op("dve", lambda e: e.tensor_copy(out=LR[:, 0, :], in_=cf[:, C_LF:C_LF + 128]), rd=[b_cf], wr=[b_LR])
                kb.op("dve", lambda e: e.tensor_copy(out=LR[:, 1, :], in_=cf[:, C_LB:C_LB + 128]), rd=[b_cf], wr=[b_LR])
            S_ENG = os.environ.get("S_ENG", "pool")
            GT_POOL = int(os.environ.get("GT_POOL", 0))

            def front1(c, d, preload=None):
                k = Ctx()
                p = nchunk[0] % 2
                nchunk[0] += 1
                k.p, k.c, k.d = p, c, d
                k.xs_tm, k.b_xs = xs_tm2[p], b_xs2[p]
                k.b_tm, k.b_btm = b_tm2[p], b_btm2[p]
                k.cbm, k.b_cbm = cbm2[p], b_cbm2[p]
                k.a32, k.b_a32 = a322[p], b_a322[p]
                k.rhsA, k.b_rhsA = rhsA2[p], b_rhsA2[p]
                k.E, k.b_E = E2_[p], b_E2[p]
                k.dec, k.b_dec = dec2[p], b_dec2[p]
                k.w2, k.b_w2 = w22[p], b_w22[p]
                k.xd, k.b_xd = xd2[p], b_xd2[p]
                k.xdd, k.b_xdd = xdd2[p], b_xdd2[p]
                k.yacc, k.b_yacc = yacc2[p], b_yaccg2[p]
                k.b_Eg = b_Eg2[p]
                k.xs3 = k.xs_tm[:].rearrange("p (h d) -> p h d", d=64)
                k.add_prev = preload is not None
                xs_tm, b_xs, b_tm, b_btm, cbm, b_cbm = k.xs_tm, k.b_xs, k.b_tm, k.b_btm, k.cbm, k.b_cbm
                a32, b_a32, rhsA, b_rhsA, E, b_E, dec, b_dec = k.a32, k.b_a32, k.rhsA, k.b_rhsA, k.E, k.b_E, k.dec, k.b_dec
                w2, b_w2, xd, b_xd, xdd, b_xdd, xs3 = k.w2, k.b_w2, k.xd, k.b_xd, k.xdd, k.b_xdd, k.xs3
                k.ybl = None
                if k.add_prev:
                    preload(k)
                rd_x = [b_xbcd[j][min(c // 4, 8)] for j in range(24)]
                kb.dma("sp", lambda q: q.dma_start(out=XT[p][:], in_=xbc_d[:, :, c * 128:(c + 1) * 128]),
                       rd=rd_x, wr=[b_XT[p]])
                X = XT[p]
                bX = b_XT[p]
                k.X, k.bX = X, bX
                U = cf[:, (C_UF if d == 0 else C_UB):(C_UF if d == 0 else C_UB) + 128]
                L = cf[:, (C_LF if d == 0 else C_LB):(C_LF if d == 0 else C_LB) + 128]
                onesf = cf[:, C_ONES:C_ONES + 128]
                Lr = LR[:, d, :] if SEGR else L
                dtc = dtall[:, c, d * 32:(d + 1) * 32]
                kb.op("dve", lambda e: e.tensor_tensor(out=a32[:], in0=dtc, in1=Aneg[:, d * 32:(d + 1) * 32],
                                                       op=ALU.mult), rd=[b_dt[c], b_A], wr=[b_a32])
                kb.op("pool", lambda e: e.tensor_tensor(out=(rhsA[:].bitcast(F32R) if SEGR else rhsA[:]), in0=a32[:].unsqueeze(2).broadcast_to([128, 32, 128]),
                                                        in1=U.unsqueeze(1).broadcast_to([128, 32, 128]), op=ALU.mult),
                      rd=[b_a32, b_cf], wr=[b_rhsA])
                psm, psmb = bank()
                kb.op("pe", lambda e: [e.matmul(psm[:, 0:32], U, a32[:], start=True, stop=True),
                                       e.matmul(psm[:, 32:64], L, a32[:], start=True, stop=True),
                                       e.matmul(psm[:, 64:96], onesf, a32[:], start=True, stop=True)],
                      rd=[b_cf, b_a32], wr=[psmb])
                kb.op("act", lambda e: e.activation(out=dec[:], in_=psm[:, 0:96], func=AF.Exp), rd=[psmb], wr=[b_dec])
                kb.op("dve", lambda e: e.tensor_tensor(out=w2[:], in0=dtc, in1=dec[:, 32:64], op=ALU.mult),
                      rd=[b_dt[c], b_dec], wr=[b_w2])
                for half in range(2):
                    pt, pb = bank()
                    ptb = pt[:].bitcast(BF16)
                    kb.op("pe", lambda e: [e.transpose(ptb[:, kk * 128:(kk + 1) * 128], X[:, half * 8 + kk, :], identb)
                                           for kk in range(8)], rd=[bX, b_cbf], wr=[pb])
                    kb.op("act", lambda e: e.activation(out=xs_tm[:, half * 1024:(half + 1) * 1024], in_=ptb,
                                                        func=AF.Copy), rd=[pb], wr=[b_xs])
                pt, pb = bank()
                ptb = pt[:].bitcast(BF16)
                kb.op("pe", lambda e: [e.transpose(ptb[:, kk * 128:(kk + 1) * 128], X[:, 16 + kk, :], identb)
                                       for kk in range(4)], rd=[bX, b_cbf], wr=[pb])
                kb.op("act", lambda e: e.activation(out=b_tm[:], in_=ptb[:, 0:512], func=AF.Copy), rd=[pb], wr=[b_btm])
                pcb_, pcbb = bank()
                kb.op("pe", lambda e: [e.matmul(pcb_[:, g * 128:(g + 1) * 128], X[:, 16 + g, :], X[:, 20 + g, :],
                                                start=True, stop=True) for g in range(4)], rd=[bX], wr=[pcbb])
                kb.op("dve", lambda e: e.tensor_tensor(out=cbm[:], in0=pcb_[:].rearrange("p (g l) -> p g l", g=4),
                                                       in1=U.unsqueeze(1).broadcast_to([128, 4, 128]), op=ALU.mult),
                      rd=[pcbb, b_cf], wr=[b_cbm])
                kb.op("pool", lambda e: e.tensor_tensor(out=xd[:], in0=xs3, in1=dtc.unsqueeze(2).broadcast_to([128, 32, 64]),
                                                        op=ALU.mult), rd=[b_xs, b_dt[c]], wr=[b_xd])
                kb.op("pool", lambda e: e.tensor_tensor(out=xdd[:], in0=xs3,
                                                        in1=w2[:].unsqueeze(2).broadcast_to([128, 32, 64]), op=ALU.mult),
                      rd=[b_xs, b_w2], wr=[b_xdd])
                for q8 in range(8):
                    pse, pseb = bank()
                    kb.op("pe", lambda e: e.matmul(pse[:, :], Lr, (rhsA[:, q8 * 4:(q8 + 1) * 4, :].bitcast(F32R) if SEGR else rhsA[:, q8 * 4:(q8 + 1) * 4, :]),
                                                   start=True, stop=True),
                          rd=[b_cf, b_rhsA, b_LR], wr=[pseb])
                    kb.op("act", lambda e: e.activation(out=E[:, q8 * 4:(q8 + 1) * 4, :],
                                                        in_=pse[:].rearrange("p (h l) -> p h l", h=4), func=AF.Exp),
                          rd=[pseb], wr=[k.b_Eg[q8 // 2]])
                return k

            def front2(k):
                for g in range(4):
                    kb.op("pool" if g < GT_POOL else "dve", lambda e: e.tensor_tensor(out=k.E[:, g * 8:(g + 1) * 8, :], in0=k.E[:, g * 8:(g + 1) * 8, :],
                                                           in1=k.cbm[:, g:g + 1, :].broadcast_to([128, 8, 128]),
                                                           op=ALU.mult), rd=[k.b_cbm], wr=[k.b_Eg[g]])

            def back(k):
                GT, b_GT, xd, b_xd, xdd, b_xdd = k.E, k.b_E, k.xd, k.b_xd, k.xdd, k.b_xdd
                X, bX, b_tm, b_btm, dec, b_dec = k.X, k.bX, k.b_tm, k.b_btm, k.dec, k.b_dec
                yacc, b_yacc = k.yacc, k.b_yacc
                for g in range(4):
                    py, pyb = bank()
                    po, pob = bank()
                    pst, pstb = bank()
                    if k.add_prev:
                        kb.op("pe", lambda e: [e.matmul(py[:, :], identb, k.ybl[:, g * 512:(g + 1) * 512], start=True, stop=False)]
                              + [e.matmul(py[:, hh * 64:(hh + 1) * 64], DI[:, g * 8 + hh, :],
                                          k.xs_tm[:, (g * 8 + hh) * 64:(g * 8 + hh + 1) * 64], start=False, stop=False)
                                 for hh in range(8)]
                              + [e.matmul(py[:, hh * 64:(hh + 1) * 64], GT[:, g * 8 + hh, :], xd[:, g * 8 + hh, :],
                                          start=False, stop=(hh == 7)) for hh in range(8)],
                              rd=[k.b_Eg[g], b_xd, k.b_ybl, k.b_xs, b_DI, b_cbf], wr=[pyb])
                    else:
                        kb.op("pe", lambda e: [e.matmul(py[:, hh * 64:(hh + 1) * 64], GT[:, g * 8 + hh, :], xd[:, g * 8 + hh, :],
                                                        start=True, stop=True) for hh in range(8)],
                              rd=[k.b_Eg[g], b_xd], wr=[pyb])
                    kb.op("pe", lambda e: e.matmul(po[:, :], X[:, 20 + g, :], Sbf[:, g * 512:(g + 1) * 512],
                                                   start=True, stop=True), rd=[bX, b_Sbfg[g]], wr=[pob])
                    kb.op("pe", lambda e: e.matmul(pst[:, :], b_tm[:, g * 128:(g + 1) * 128],
                                                   xdd[:, g * 8:(g + 1) * 8, :], start=True, stop=True),
                          rd=[b_btm, b_xdd], wr=[pstb])
                    q = g % 2
                    kb.op("dve", lambda e: e.tensor_tensor(
                        out=tmp[q][:].rearrange("p (h d) -> p h d", d=64),
                        in0=po[:].rearrange("p (h d) -> p h d", d=64),
                        in1=dec[:, g * 8:(g + 1) * 8].unsqueeze(2).broadcast_to([128, 8, 64]), op=ALU.mult),
                        rd=[pob, b_dec], wr=[b_tmp[q]])
                    kb.op("dve", lambda e: e.tensor_tensor(out=yacc[:, g * 512:(g + 1) * 512], in0=py[:, :], in1=tmp[q][:],
                                                           op=ALU.add), rd=[pyb, b_tmp[q]], wr=[b_yacc[g]])
                    kb.op(S_ENG, lambda e: e.tensor_tensor(
                        out=S32[:, g * 512:(g + 1) * 512].rearrange("p (h d) -> p h d", d=64),
                        in0=S32[:, g * 512:(g + 1) * 512].rearrange("p (h d) -> p h d", d=64),
                        in1=dec[:, 64 + g * 8:64 + (g + 1) * 8].unsqueeze(2).broadcast_to([128, 8, 64]), op=ALU.mult),
                        rd=[b_dec], wr=[b_S32g[g]])
                    kb.op("dve", lambda e: e.tensor_tensor(out=S32[:, g * 512:(g + 1) * 512], in0=pst[:, :],
                                                           in1=S32[:, g * 512:(g + 1) * 512], op=ALU.add),
                          rd=[pstb], wr=[b_S32g[g]])
                    kb.op("act", lambda e: e.activation(out=Sbf[:, g * 512:(g + 1) * 512], in_=S32[:, g * 512:(g + 1) * 512],
                                                        func=AF.Copy), rd=[b_S32g[g]], wr=[b_Sbfg[g]])

            def run_pass(order, d, preload_fn, post_fn):
                k = front1(order[0], d, preload_fn(order[0]) if preload_fn else None)
                front2(k)
                for i, c in enumerate(order):
                    kn = None
                    if i + 1 < len(order):
                        cn = order[i + 1]
                        kn = front1(cn, d, preload_fn(cn) if preload_fn else None)
                    back(k)
                    post_fn(k)
                    if kn is not None:
                        front2(kn)
                    k = kn

            kb.enabled = "D" in run
            kb.op("dve", lambda e: e.memset(S32[:], 0.0), wr=b_S32g)
            kb.op("dve", lambda e: e.memset(Sbf[:], 0.0), wr=b_Sbfg)
            run_pass(list(range(NT - 1, -1, -1)), 1, None,
                     lambda k: kb.dma("pool", lambda q: q.dma_start(out=yb_d[k.c], in_=k.yacc[:]), rd=k.b_yacc, wr=[b_ybd[k.c]]))

            kb.enabled = "E" in run
            kb.op("dve", lambda e: e.memset(S32[:], 0.0), wr=b_S32g)
            kb.op("dve", lambda e: e.memset(Sbf[:], 0.0), wr=b_Sbfg)
            wns = sb("wns", [128, 2048], F32, sd)
            b_wns = Buf()
            kb.dma("sp", lambda q: q.dma_start(out=wns[:], in_=bc_d[:, B_WSSM:B_WSSM + 2048]), wr=[b_wns])
            szl2, b_szl2 = dbl("szl", [128, 2048], BF16)
            yn2, b_yn2 = dbl("yn", [128, 2048], BF16)
            ybl2, b_ybl2 = dbl("ybl", [128, 2048], BF16)
            DI = sb("DI", [128, 32, 128], BF16, sd)
            b_DI = Buf()
            for hh_ in range(32):
                kb.op("dve", lambda e: e.tensor_scalar(out=DI[:, hh_, :], in0=identb, scalar1=abc[:, 128 + hh_:129 + hh_],
                                                       scalar2=None, op0=ALU.mult), rd=[b_cbf, b_abc], wr=[b_DI])
            ssf2, b_ssf2 = dbl("ssf", [128, 1], F32)
            rsf2, b_rsf2 = dbl("rsf", [128, 1], F32)

            def fwd_preload(c):
                def f(k):
                    k.ybl, k.b_ybl = ybl2[k.p], b_ybl2[k.p]
                    kb.dma("sp", lambda q: q.dma_start(out=k.ybl[:], in_=yb_d[c]), rd=[b_ybd[c]], wr=[k.b_ybl])
                return f

            def fwd_post(k):
                p, c = k.p, k.c
                yacc, b_yacc = k.yacc, k.b_yacc
                kb.dma("sp", lambda q: q.dma_start(out=szl2[p][:], in_=sz_d[c]), rd=[b_szd[c]], wr=[b_szl2[p]])
                kb.op("pool", lambda e: e.tensor_tensor(out=yacc[:], in0=yacc[:], in1=szl2[p][:], op=ALU.mult),
                      rd=[b_szl2[p]], wr=b_yacc)
                rms_rstd(yacc[:], b_yacc, yn2[p][:], b_yn2[p], ssf2[p], b_ssf2[p], rsf2[p], b_rsf2[p], 2048)
                kb.op("dve", lambda e: e.scalar_tensor_tensor(out=yn2[p][:], in0=yacc[:], scalar=rsf2[p][:, 0:1], in1=wns[:],
                                                              op0=ALU.mult, op1=ALU.mult),
                      rd=b_yacc + [b_rsf2[p], b_wns], wr=[b_yn2[p]])
                kb.dma("sp", lambda q: q.dma_start(out=yn_d[c], in_=yn2[p][:]), rd=[b_yn2[p]], wr=[b_ynd[c]])

            run_pass(list(range(NT)), 0, fwd_preload, fwd_post)

        kb.barrier()
        affTM = sb("affTM", [128, NT, NE], F32)
        b_affTM = Buf()
        affT = sb("affT", [NE, T], F32)
        b_affT = Buf()
        with ExitStack() as se:
            kb.enabled = "E" in run
            wso = sb("wso", [128, 16, D], BF16, se)
            wout = sb("wout", [128, 8, D], BF16, se)
            wr_ = sb("wr_", [128, 8, NE], BF16, se)
            b_wso, b_wout, b_wr = Buf(), Buf(), Buf()
            kb.dma("pool", lambda q: q.dma_start(out=wso[:], in_=w_so.rearrange("(kc p) n -> p kc n", p=128)), wr=[b_wso])
            kb.dma("pool", lambda q: q.dma_start(out=wout[:], in_=w_o.rearrange("(kc p) n -> p kc n", p=128)), wr=[b_wout])
            kb.dma("pool", lambda q: q.dma_start(out=wr_[:], in_=w_r.rearrange("(kc p) n -> p kc n", p=128)), wr=[b_wr])
            wnf = sb("wnf", [128, D], F32, se)
            b_wnf = Buf()
            kb.dma("sp", lambda q: q.dma_start(out=wnf[:], in_=bc_d[:, B_WFFN:B_WFFN + D]), wr=[b_wnf])

            def nbuf(name, shape, dtype, n):
                return [sb("%s_%d" % (name, i), shape, dtype, se) for i in range(n)], [Buf() for _ in range(n)]

            ynl1, b_ynl1 = nbuf("ynl", [128, 4, 2048], BF16, 1)
            ynT2, b_ynT2 = nbuf("ynT", [128, 16, 512], BF16, 1)
            ynT2, b_ynT2 = ynT2 * 2, b_ynT2 * 2
            m1l1, b_m1l1 = nbuf("m1l", [128, 8, 512], BF16, 1)
            g2l1, b_g2l1 = nbuf("g2l", [128, 8, 512], BF16, 1)
            mT2, b_mT2 = nbuf("mT", [128, 8, 512], BF16, 2)
            xinF4, b_xinF4 = nbuf("xinF", [128, D], F32, 4)
            hT4, b_hT4 = nbuf("h_t", [128, D], F32, 4)
            hnb4, b_hnb4 = nbuf("hnb", [128, D], BF16, 4)
            hnT4, b_hnT4 = nbuf("hnT", [128, 8, 128], BF16, 4)
            junkE2, b_junkE2 = nbuf("junkE", [128, D], BF16, 2)
            ssE4, b_ssE4 = nbuf("ssE", [128, 1], F32, 4)
            rsE4, b_rsE4 = nbuf("rsE", [128, 1], F32, 4)
            lg4, b_lg4 = nbuf("lg", [128, NE], F32, 4)
            mx4, b_mx4 = nbuf("mx", [128, 1], F32, 4)
            sme4, b_sme4 = nbuf("sme", [128, 1], F32, 4)

            def stage_TS(ti):
                t0, n = TBS[ti]
                pb_ = ti % 2
                nt_ = n // 128
                c0 = t0 // 128
                ynl, b_ynl = ynl1[0], b_ynl1[0]
                ynT, b_ynT = ynT2[pb_], b_ynT2[pb_]
                m1l, b_m1l = m1l1[0], b_m1l1[0]
                g2l, b_g2l = g2l1[0], b_g2l1[0]
                mT, b_mT = mT2[pb_], b_mT2[pb_]
                kb.dma("sp", lambda q: q.dma_start(out=ynl[:, 0:nt_, :], in_=yn_d[c0:c0 + nt_].rearrange("t p f -> p t f")),
                       rd=b_ynd[c0:c0 + nt_], wr=[b_ynl])
                kb.dma("sp", lambda q: q.dma_start(out=m1l[:, :, 0:n], in_=m1_d[:, :, t0:t0 + n]), rd=[b_m1d[ti]], wr=[b_m1l])
                kb.dma("sp", lambda q: q.dma_start(out=g2l[:, :, 0:n], in_=g2_d[:, :, t0:t0 + n]), rd=[b_g2d[ti]], wr=[b_g2l])
                for tt in range(nt_):
                    for half in range(2):
                        pt, pb = bank()
                        ptb = pt[:].bitcast(BF16)
                        kb.op("pe", lambda e: [e.transpose(ptb[:, k * 128:(k + 1) * 128],
                                                           ynl[:, tt, (half * 8 + k) * 128:(half * 8 + k + 1) * 128], identb)
                                               for k in range(8)], rd=[b_ynl, b_cbf], wr=[pb])
                        kb.op("act", lambda e: e.activation(out=ynT[:, half * 8:(half + 1) * 8, tt * 128:(tt + 1) * 128],
                                                            in_=ptb.rearrange("p (k t) -> p k t", k=8), func=AF.Copy),
                              rd=[pb], wr=[b_ynT])
                for dc in range(8):
                    pbs, pbsb = bank()
                    kb.op("pe", lambda e: [e.matmul(pbs[:, 0:n], wso[:, kc, dc * 128:(dc + 1) * 128], ynT[:, kc, 0:n],
                                                    start=(kc == 0), stop=(kc == 15)) for kc in range(16)],
                          rd=[b_wso, b_ynT], wr=[pbsb])
                    kb.op("dve", lambda e: e.tensor_tensor(out=mT[:, dc, 0:n], in0=pbs[:, 0:n], in1=g2l[:, dc, 0:n],
                                                           op=ALU.mult), rd=[pbsb, b_g2l], wr=[b_mT])
                    kb.op("pool", lambda e: e.tensor_tensor(out=mT[:, dc, 0:n], in0=mT[:, dc, 0:n], in1=m1l[:, dc, 0:n],
                                                            op=ALU.add), rd=[b_m1l], wr=[b_mT])

            def stage_W(ti):
                t0, n = TBS[ti]
                pb_ = ti % 2
                nt_ = n // 128
                c0 = t0 // 128
                mT, b_mT = mT2[pb_], b_mT2[pb_]
                for tt in range(nt_):
                    c = c0 + tt
                    load_x_tile(c, xinF4[tt], b_xinF4[tt])
                    for dh in range(2):
                        ph, phb = bank()
                        kb.op("pe", lambda e: [e.matmul(ph[:, :], mT[:, kc, tt * 128:(tt + 1) * 128],
                                                        wout[:, kc, dh * 512:(dh + 1) * 512],
                                                        start=(kc == 0), stop=(kc == 7)) for kc in range(8)],
                              rd=[b_wout, b_mT], wr=[phb])
                        kb.op("dve", lambda e: e.tensor_tensor(out=hT4[tt][:, dh * 512:(dh + 1) * 512], in0=ph[:, :],
                                                               in1=xinF4[tt][:, dh * 512:(dh + 1) * 512], op=ALU.add),
                              rd=[phb, b_xinF4[tt]], wr=[b_hT4[tt]])
                    kb.dma("sp", lambda q: q.dma_start(out=hacc_d[c * 128:(c + 1) * 128, :], in_=hT4[tt][:]),
                           rd=[b_hT4[tt]], wr=[hacc])
                for tt in range(nt_):
                    kb.op("act", lambda e: e.activation(out=junkE2[tt % 2][:], in_=hT4[tt][:], func=AF.Square,
                                                        accum_out=ssE4[tt][:]),
                          rd=[b_hT4[tt]], wr=[b_junkE2[tt % 2], b_ssE4[tt]])
                for tt in range(nt_):
                    kb.op("act", lambda e: e.activation(out=ssE4[tt][:], in_=ssE4[tt][:], func=AF.Sqrt, bias=EPS, scale=1.0 / D),
                          rd=[b_ssE4[tt]], wr=[b_ssE4[tt]])
                for tt in range(nt_):
                    kb.op("dve", lambda e: e.reciprocal(out=rsE4[tt][:], in_=ssE4[tt][:]), rd=[b_ssE4[tt]], wr=[b_rsE4[tt]])
                for tt in range(nt_):
                    c = c0 + tt
                    kb.op("dve", lambda e: e.scalar_tensor_tensor(out=hnb4[tt][:], in0=hT4[tt][:], scalar=rsE4[tt][:, 0:1],
                                                                  in1=wnf[:], op0=ALU.mult, op1=ALU.mult),
                          rd=[b_hT4[tt], b_rsE4[tt], b_wnf], wr=[b_hnb4[tt]])
                    kb.dma("sp", lambda q: q.dma_start(out=hn_d[c * 128:(c + 1) * 128, :], in_=hnb4[tt][:]),
                           rd=[b_hnb4[tt]], wr=[hnrow])

            def stage_R(ti):
                t0, n = TBS[ti]
                nt_ = n // 128
                c0 = t0 // 128
                prs = []
                for tt in range(nt_):
                    pt, pb = bank()
                    ptb = pt[:].bitcast(BF16)
                    kb.op("pe", lambda e: [e.transpose(ptb[:, k * 128:(k + 1) * 128], hnb4[tt][:, k * 128:(k + 1) * 128], identb)
                                           for k in range(8)], rd=[b_hnb4[tt], b_cbf], wr=[pb])
                    kb.op("act", lambda e: e.activation(out=hnT4[tt][:], in_=ptb.rearrange("p (k t) -> p k t", k=8), func=AF.Copy),
                          rd=[pb], wr=[b_hnT4[tt]])
                for tt in range(nt_):
                    pr, prb = bank()
                    prs.append((pr, prb))
                    kb.op("pe", lambda e: [e.matmul(pr[:, 0:NE], hnT4[tt][:, kc, :], wr_[:, kc, :], start=(kc == 0), stop=(kc == 7))
                                           for kc in range(8)], rd=[b_hnT4[tt], b_wr], wr=[prb])
                for tt in range(nt_):
                    pr, prb = prs[tt]
                    kb.op("dve", lambda e: e.reduce_max(out=mx4[tt][:], in_=pr[:, 0:NE], axis=AX.X), rd=[prb], wr=[b_mx4[tt]])
                for tt in range(nt_):
                    kb.op("dve", lambda e: e.tensor_scalar(out=mx4[tt][:], in0=mx4[tt][:], scalar1=-1.0, scalar2=None, op0=ALU.mult),
                          rd=[b_mx4[tt]], wr=[b_mx4[tt]])
                for tt in range(nt_):
                    pr, prb = prs[tt]
                    kb.op("act", lambda e: e.activation(out=lg4[tt][:], in_=pr[:, 0:NE], func=AF.Exp, bias=mx4[tt][:, 0:1],
                                                        accum_out=sme4[tt][:]), rd=[prb, b_mx4[tt]], wr=[b_lg4[tt], b_sme4[tt]])
                for tt in range(nt_):
                    kb.op("dve", lambda e: e.reciprocal(out=sme4[tt][:], in_=sme4[tt][:]), rd=[b_sme4[tt]], wr=[b_sme4[tt]])
                for tt in range(nt_):
                    c = c0 + tt
                    kb.op("dve", lambda e: e.tensor_scalar(out=affTM[:, c, :], in0=lg4[tt][:], scalar1=sme4[tt][:, 0:1], scalar2=None,
                                                           op0=ALU.mult), rd=[b_lg4[tt], b_sme4[tt]], wr=[b_affTM])
                for tt in range(nt_):
                    c = c0 + tt
                    pa_, pab_ = bank()
                    kb.op("pe", lambda e: e.transpose(pa_[0:NE, 0:128], affTM[:, c, :], identf), rd=[b_affTM, b_cf], wr=[pab_])
                    kb.op("act", lambda e: e.activation(out=affT[:, c * 128:(c + 1) * 128], in_=pa_[0:NE, 0:128], func=AF.Copy),
                          rd=[pab_], wr=[b_affT])

            stage_TS(0)
            for ti in range(len(TBS)):
                stage_W(ti)
                if ti + 1 < len(TBS):
                    stage_TS(ti + 1)
                stage_R(ti)

        kb.barrier()
        if debug:
            kb.enabled = "E" in run
            kb.dma("sp", lambda q: q.dma_start(out=aff_dbg, in_=affTM[:]), rd=[b_affTM], wr=[Buf()])
        with ExitStack() as sf:
            kb.enabled = "F" in run
            kb.op("dve", lambda e: e.memset(affT[:, 0:112], 0.0), wr=[b_affT])
            posmTM = sf.enter_context(nc.sbuf_tensor("posmTM", [128, NT, NE], F32))
            RT = sf.enter_context(nc.sbuf_tensor("RT", [128, NT, NE, 4], BF16))
            sr = ExitStack()
            lo = sr.enter_context(nc.sbuf_tensor("lo", [NE, 1], F32))
            hi = sr.enter_context(nc.sbuf_tensor("hi", [NE, 1], F32))
            mid = sr.enter_context(nc.sbuf_tensor("mid", [NE, 1], F32))
            cnt = sr.enter_context(nc.sbuf_tensor("cnt", [NE, 1], F32))
            ge = sr.enter_context(nc.sbuf_tensor("ge", [NE, 1], F32))
            dl = sr.enter_context(nc.sbuf_tensor("dl", [NE, 1], F32))
            jk = sr.enter_context(nc.sbuf_tensor("jk", [NE, T], BF16))
            maskT = sr.enter_context(nc.sbuf_tensor("maskT", [NE, T], F32))
            csum = sr.enter_context(nc.sbuf_tensor("csum", [NE, T], F32))
            onesT = sr.enter_context(nc.sbuf_tensor("onesT", [NE, T], F32))
            ahi = sr.enter_context(nc.sbuf_tensor("ahi", [128, NT, NE], BF16))
            alo = sr.enter_context(nc.sbuf_tensor("alo", [128, NT, NE], F32))
            b_r = Buf()
            b_posm = Buf()
            b_RT = Buf()
            kb.op("dve", lambda e: e.memset(lo[:], 0.0), wr=[b_r])
            kb.op("dve", lambda e: e.memset(hi[:], 1.0), wr=[b_r])
            kb.op("dve", lambda e: e.memset(onesT[:], 1.0), wr=[b_r])
            for itn in range(28):
                kb.op("dve", lambda e: e.tensor_tensor(out=mid[:], in0=lo[:], in1=hi[:], op=ALU.add), rd=[b_r], wr=[b_r])
                kb.op("dve", lambda e: e.tensor_scalar(out=mid[:], in0=mid[:], scalar1=0.5, scalar2=None, op0=ALU.mult),
                      rd=[b_r], wr=[b_r])
                kb.op("dve", lambda e: e.tensor_scalar(out=jk[:], in0=affT[:], scalar1=mid[:, 0:1], scalar2=None,
                                                       op0=ALU.is_gt, op1=ALU.add, accum_out=cnt[:]),
                      rd=[b_r, b_affT], wr=[b_r])
                kb.op("dve", lambda e: e.tensor_scalar(out=ge[:], in0=cnt[:], scalar1=float(CAP) - 0.5, scalar2=None,
                                                       op0=ALU.is_gt), rd=[b_r], wr=[b_r])
                kb.op("dve", lambda e: e.tensor_tensor(out=dl[:], in0=mid[:], in1=lo[:], op=ALU.subtract), rd=[b_r], wr=[b_r])
                kb.op("dve", lambda e: e.tensor_tensor(out=dl[:], in0=dl[:], in1=ge[:], op=ALU.mult), rd=[b_r], wr=[b_r])
                kb.op("dve", lambda e: e.tensor_tensor(out=lo[:], in0=lo[:], in1=dl[:], op=ALU.add), rd=[b_r], wr=[b_r])
                kb.op("dve", lambda e: e.tensor_tensor(out=dl[:], in0=hi[:], in1=mid[:], op=ALU.subtract), rd=[b_r], wr=[b_r])
                kb.op("dve", lambda e: e.tensor_tensor(out=dl[:], in0=dl[:], in1=ge[:], op=ALU.mult), rd=[b_r], wr=[b_r])
                kb.op("dve", lambda e: e.tensor_tensor(out=hi[:], in0=mid[:], in1=dl[:], op=ALU.add), rd=[b_r], wr=[b_r])
            kb.op("dve", lambda e: e.tensor_scalar(out=maskT[:], in0=affT[:], scalar1=lo[:, 0:1], scalar2=None,
                                                   op0=ALU.is_gt), rd=[b_r, b_affT], wr=[b_r])
            kb.op("dve", lambda e: e.tensor_tensor_scan(out=csum[:], data0=onesT[:], data1=maskT[:], initial=0.0,
                                                        op0=ALU.mult, op1=ALU.add), rd=[b_r], wr=[b_r])
            kb.op("dve", lambda e: e.tensor_tensor(out=csum[:], in0=csum[:], in1=maskT[:], op=ALU.mult), rd=[b_r], wr=[b_r])
            kb.op("dve", lambda e: e.tensor_scalar(out=csum[:], in0=csum[:], scalar1=-1.0, scalar2=None, op0=ALU.add),
                  rd=[b_r], wr=[b_r])
            for c in range(NT):
                pp, ppb = bank()
                kb.op("pe", lambda e: e.transpose(pp[:, 0:NE], csum[:, c * 128:(c + 1) * 128], identf[0:NE, 0:NE]),
                      rd=[b_r, b_cf], wr=[ppb])
                kb.op("act", lambda e: e.activation(out=posmTM[:, c, :], in_=pp[:, 0:NE], func=AF.Copy),
                      rd=[ppb], wr=[b_posm])
            kb.op("dve", lambda e: e.tensor_copy(out=ahi[:], in_=affTM[:]), rd=[b_affTM], wr=[b_RT])
            kb.op("dve", lambda e: e.tensor_tensor(out=alo[:], in0=affTM[:], in1=ahi[:], op=ALU.subtract),
                  rd=[b_affTM], wr=[b_RT])
            kb.op("dve", lambda e: e.tensor_copy(out=RT[:, :, :, 2], in_=ahi[:]), wr=[b_RT])
            kb.op("dve", lambda e: e.tensor_copy(out=RT[:, :, :, 3], in_=alo[:]), wr=[b_RT])
            tv = cbf[:, CB_TV:CB_TV + 66].rearrange("p (t two) -> p t two", two=2)
            kb.op("dve", lambda e: e.tensor_copy(out=RT[:, :, :, 0], in_=tv[:, :, 0:1].broadcast_to([128, NT, NE])),
                  rd=[b_cbf], wr=[b_RT])
            kb.op("dve", lambda e: e.tensor_copy(out=RT[:, :, :, 1], in_=tv[:, :, 1:2].broadcast_to([128, NT, NE])),
                  rd=[b_cbf], wr=[b_RT])

            kb.barrier()
            sr.close()

            if debug:
                kb.enabled = "F" in run
                kb.dma("sp", lambda q: q.dma_start(out=posm_dbg, in_=posmTM[:]), rd=[b_posm], wr=[Buf()])
            kb.enabled = "G" in run
            NSLOT = 10
            wring = [sf.enter_context(nc.sbuf_tensor("wring%d" % i, [128, 4096], BF16)) for i in range(NSLOT)]
            b_wring = [Buf() for _ in range(NSLOT)]
            units = []
            for e_ in range(NE):
                for q4 in range(4):
                    units.append(("g", e_, q4))
                    units.append(("u", e_, q4))
                for q4 in range(4):
                    units.append(("d", e_, q4))
            slot_of = {}

            def issue_unit(ui):
                kind, e_, q4 = units[ui]
                s_ = ui % NSLOT
                slot_of[(kind, e_, q4)] = s_
                if not kb.enabled or os.environ.get("GNOW") == "1":
                    return
                if kind in ("g", "u"):
                    src = (w_eg if kind == "g" else w_eu)[e_].rearrange("(kc p) n -> p kc n", p=128)[:, :, q4 * 512:(q4 + 1) * 512]
                    dst = wring[s_][:].rearrange("p (kc n) -> p kc n", kc=8)
                else:
                    src = w_ed[e_].rearrange("(fc p) n -> p fc n", p=128)[:, q4 * 4:(q4 + 1) * 4, :]
                    dst = wring[s_][:].rearrange("p (fc n) -> p fc n", fc=4)
                MAXOUT = int(os.environ.get("MAXOUT", 2))
                if len(unit_toks) >= MAXOUT:
                    kb.wait("pool", [unit_toks[-MAXOUT]])
                unit_toks.append(kb.dma("pool", lambda q: q.dma_start(out=dst, in_=src), wr=[b_wring[s_]]))

            unit_toks = []
            PREF = 8
            nissued = [0]

            def ensure(ui):
                while nissued[0] <= min(ui + PREF, len(units) - 1):
                    issue_unit(nissued[0])
                    nissued[0] += 1

            sel = [sf.enter_context(nc.sbuf_tensor("sel%d" % i, [128, 516], BF16)) for i in range(3)]
            b_sel = [Buf() for _ in range(3)]
            iq = sf.enter_context(nc.sbuf_tensor("iq", [4, 516], F32))
            b_iq = Buf()
            iqT = sf.enter_context(nc.sbuf_tensor("iqT", [128, 5, 4], F32))
            b_iqT = Buf()
            idxf = sf.enter_context(nc.sbuf_tensor("idxf", [128, 5], F32))
            idxi = [sf.enter_context(nc.sbuf_tensor("idxi%d" % i, [128, 5], I32)) for i in range(2)]
            afs = [sf.enter_context(nc.sbuf_tensor("afs%d" % i, [128, 5], F32)) for i in range(2)]
            b_idx = [Buf(), Buf()]
            xg2 = [sf.enter_context(nc.sbuf_tensor("xg%d" % i, [128, 5, D], BF16)) for i in range(2)]
            b_xg2 = [Buf(), Buf()]
            xgT2 = [sf.enter_context(nc.sbuf_tensor("xgT%d" % i, [128, 8, 640], BF16)) for i in range(2)]
            b_xgT2 = [Buf(), Buf()]
            hTe = sf.enter_context(nc.sbuf_tensor("hTe", [128, 16, 640], BF16))
            b_hTe = Buf()
            sgl = [sf.enter_context(nc.sbuf_tensor("sgl%d" % i, [128, 257], BF16)) for i in range(2)]
            b_sgl = [Buf(), Buf()]
            yw = [sf.enter_context(nc.sbuf_tensor("yw%d" % i, [128, D], F32)) for i in range(2)]
            b_yw = [Buf(), Buf()]
            hg = [sf.enter_context(nc.sbuf_tensor("hg%d" % i, [128, D], F32)) for i in range(2)]
            b_hg = [Buf(), Buf()]
            for i in range(2):
                kb.op("pool", lambda e: e.memset(xg2[i][:], 0.0), wr=[b_xg2[i]])
            iota = cf[:, C_IOTA:C_IOTA + 516]
            uic = [0]
            ityc = [0]

            def prepA(e_):
                ip = e_ % 2
                pi1, pi1b = bank()
                pi2, pi2b = bank()
                for c in range(NT):
                    s3 = c % 3
                    kb.op("dve", lambda e: e.tensor_scalar(out=sel[s3][:], in0=iota, scalar1=posmTM[:, c, e_:e_ + 1],
                                                           scalar2=None, op0=ALU.is_equal),
                          rd=[b_cf, b_posm], wr=[b_sel[s3]])
                    kb.op("pe", lambda e: [e.matmul(pi1[0:4, :], RT[:, c, e_, :], sel[s3][:, 0:512],
                                                    start=(c == 0), stop=(c == NT - 1)),
                                           e.matmul(pi2[0:4, 0:4], RT[:, c, e_, :], sel[s3][:, 512:516],
                                                    start=(c == 0), stop=(c == NT - 1))],
                          rd=[b_RT, b_sel[s3]], wr=[pi1b, pi2b])
                kb.op("act", lambda e: e.activation(out=iq[:, 0:512], in_=pi1[0:4, :], func=AF.Copy), rd=[pi1b], wr=[b_iq])
                kb.op("act", lambda e: e.activation(out=iq[:, 512:516], in_=pi2[0:4, 0:4], func=AF.Copy), rd=[pi2b], wr=[b_iq])
                pq, pqb = bank()
                kb.op("pe", lambda e: ([e.transpose(pq[:, jb * 4:(jb + 1) * 4], iq[:, jb * 128:(jb + 1) * 128],
                                                    identf[0:4, 0:4]) for jb in range(4)]
                                       + [e.transpose(pq[0:4, 16:20], iq[:, 512:516], identf[0:4, 0:4])]),
                      rd=[b_iq, b_cf], wr=[pqb])
                kb.op("dve", lambda e: e.memset(iqT[:], 0.0), wr=[b_iqT])
                kb.op("act", lambda e: e.activation(out=iqT[:, 0:4, :], in_=pq[:, 0:16].rearrange("p (j f) -> p j f", f=4),
                                                    func=AF.Copy), rd=[pqb], wr=[b_iqT])
                kb.op("act", lambda e: e.activation(out=iqT[0:4, 4, :], in_=pq[0:4, 16:20], func=AF.Copy),
                      rd=[pqb], wr=[b_iqT])
                kb.op("dve", lambda e: e.scalar_tensor_tensor(out=idxf[:], in0=iqT[:, :, 0], scalar=128.0, in1=iqT[:, :, 1],
                                                              op0=ALU.mult, op1=ALU.add), rd=[b_iqT], wr=[b_idx[ip]])
                kb.op("dve", lambda e: e.tensor_copy(out=idxi[ip][:], in_=idxf[:]), wr=[b_idx[ip]])
                kb.op("dve", lambda e: e.tensor_tensor(out=afs[ip][:], in0=iqT[:, :, 2], in1=iqT[:, :, 3], op=ALU.add),
                      rd=[b_iqT], wr=[b_idx[ip]])
                for jb in range(5):
                    M = 128 if jb < 4 else 2
                    kb.dma("pool", lambda q: q.indirect_dma_start(
                        out=xg2[ip][0:M, jb, :], out_offset=None, in_=hn_d[:, :],
                        in_offset=bass.IndirectOffsetOnAxis(ap=idxi[ip][0:M, jb:jb + 1], axis=0)),
                        rd=[b_idx[ip], hnrow], wr=[b_xg2[ip]])

            def prepB(e_):
                ip = e_ % 2
                for jb in range(5):
                    pt, pb = bank()
                    ptb = pt[:].bitcast(BF16)
                    kb.op("pe", lambda e: [e.transpose(ptb[:, kk * 128:(kk + 1) * 128], xg2[ip][:, jb, kk * 128:(kk + 1) * 128], identb)
                                           for kk in range(8)], rd=[b_xg2[ip], b_cbf], wr=[pb])
                    kb.op("act", lambda e: e.activation(out=xgT2[ip][:, :, jb * 128:(jb + 1) * 128],
                                                        in_=ptb.rearrange("p (k t) -> p k t", k=8), func=AF.Copy),
                          rd=[pb], wr=[b_xgT2[ip]])

            def gateup(e_, q4s):
                ip = e_ % 2
                xgT, b_xgT = xgT2[ip], b_xgT2[ip]
                for q4 in q4s:
                    ensure(uic[0])
                    sg_ = slot_of[("g", e_, q4)]
                    su_ = slot_of[("u", e_, q4)]
                    wgv = wring[sg_][:].rearrange("p (kc n) -> p kc n", kc=8)
                    wuv = wring[su_][:].rearrange("p (kc n) -> p kc n", kc=8)
                    for f4 in range(4):
                        fc = q4 * 4 + f4
                        for hf in range(2):
                            c0 = hf * 257
                            pgk, pgb = bank()
                            puk, pub = bank()
                            kb.op("pe", lambda e: [e.matmul(pgk[:, 0:257], wgv[:, kc, f4 * 128:(f4 + 1) * 128],
                                                            xgT[:, kc, c0:c0 + 257], start=(kc == 0), stop=(kc == 7))
                                                   for kc in range(8)], rd=[b_wring[sg_], b_xgT], wr=[pgb])
                            kb.op("pe", lambda e: [e.matmul(puk[:, 0:257], wuv[:, kc, f4 * 128:(f4 + 1) * 128],
                                                            xgT[:, kc, c0:c0 + 257], start=(kc == 0), stop=(kc == 7))
                                                   for kc in range(8)], rd=[b_wring[su_], b_xgT], wr=[pub])
                            qy = ityc[0] % 2
                            ityc[0] += 1
                            kb.op("act", lambda e: e.activation(out=sgl[qy][:], in_=pgk[:, 0:257], func=AF.Silu),
                                  rd=[pgb], wr=[b_sgl[qy]])
                            kb.op("dve", lambda e: e.tensor_tensor(out=hTe[:, fc, c0:c0 + 257], in0=puk[:, 0:257],
                                                                   in1=sgl[qy][:], op=ALU.mult),
                                  rd=[pub, b_sgl[qy]], wr=[b_hTe])
                    uic[0] += 2
                    ensure(uic[0])

            def down(e_):
                ip = e_ % 2
                sd_ = [slot_of[("d", e_, q4)] for q4 in range(4)]
                for jb in range(5):
                    M = 128 if jb < 4 else 2
                    qy = ityc[0] % 2
                    ityc[0] += 1
                    kb.dma("pool", lambda q: q.indirect_dma_start(
                        out=hg[qy][0:M, :], out_offset=None, in_=hacc_d[:, :],
                        in_offset=bass.IndirectOffsetOnAxis(ap=idxi[ip][0:M, jb:jb + 1], axis=0)),
                        rd=[b_idx[ip], hacc], wr=[b_hg[qy]])
                    for dh in range(2):
                        pdn, pdnb = bank()
                        kb.op("pe", lambda e: [e.matmul(pdn[0:M, :], hTe[:, fc, jb * 128:jb * 128 + M],
                                                        wring[sd_[fc // 4]][:].rearrange("p (f n) -> p f n", f=4)[:, fc % 4, dh * 512:(dh + 1) * 512],
                                                        start=(fc == 0), stop=(fc == 15)) for fc in range(16)],
                              rd=[b_wring[s_] for s_ in sd_] + [b_hTe], wr=[pdnb])
                        kb.op("dve", lambda e: e.scalar_tensor_tensor(
                            out=yw[qy][0:M, dh * 512:(dh + 1) * 512], in0=pdn[0:M, :], scalar=afs[ip][0:M, jb:jb + 1],
                            in1=hg[qy][0:M, dh * 512:(dh + 1) * 512], op0=ALU.mult, op1=ALU.add),
                            rd=[pdnb, b_idx[ip], b_hg[qy]], wr=[b_yw[qy]])
                    kb.dma("pool", lambda q: q.indirect_dma_start(
                        out=hacc_d[:, :], out_offset=bass.IndirectOffsetOnAxis(ap=idxi[ip][0:M, jb:jb + 1], axis=0),
                        in_=yw[qy][0:M, :], in_offset=None),
                        rd=[b_yw[qy], b_idx[ip]], wr=[hacc])
                uic[0] += 4

            ensure(0)
            prepA(0)
            prepB(0)
            for e_ in range(NE):
                gateup(e_, [0, 1])
                if e_ + 1 < NE:
                    prepA(e_ + 1)
                gateup(e_, [2, 3])
                if e_ + 1 < NE:
                    prepB(e_ + 1)
                down(e_)

        kb.barrier()
        with ExitStack() as sh:
            kb.enabled = "H" in run
            wfin = sb("wfin", [128, D], F32, sh)
            b_wfin = Buf()
            kb.dma("sp", lambda q: q.dma_start(out=wfin[:], in_=bc_d[:, B_WFIN:B_WFIN + D]), wr=[b_wfin])
            hl = [sb("hl%d" % i, [128, D], F32, sh) for i in range(4)]
            b_hl = [Buf() for _ in range(4)]
            ol = [sb("ol%d" % i, [128, D], F32, sh) for i in range(4)]
            b_ol = [Buf() for _ in range(4)]
            jf = [sb("jf%d" % i, [128, D], BF16, sh) for i in range(2)]
            b_jf = [Buf(), Buf()]
            s1_ = [sb("s1_%d" % i, [128, 1], F32, sh) for i in range(4)]
            r1_ = [sb("r1_%d" % i, [128, 1], F32, sh) for i in range(4)]
            b_s1 = [Buf() for _ in range(4)]
            b_r1 = [Buf() for _ in range(4)]
            outb = Buf()
            for c0 in range(1, NT, 4):
                cs_ = list(range(c0, min(c0 + 4, NT)))
                for i, c in enumerate(cs_):
                    kb.dma("sp", lambda q: q.dma_start(out=hl[i][:], in_=hacc_d[c * 128:(c + 1) * 128, :]), rd=[hacc], wr=[b_hl[i]])
                for i, c in enumerate(cs_):
                    kb.op("act", lambda e: e.activation(out=jf[i % 2][:], in_=hl[i][:], func=AF.Square, accum_out=s1_[i][:]),
                          rd=[b_hl[i]], wr=[b_jf[i % 2], b_s1[i]])
                for i, c in enumerate(cs_):
                    kb.op("act", lambda e: e.activation(out=s1_[i][:], in_=s1_[i][:], func=AF.Sqrt, bias=EPS, scale=1.0 / D),
                          rd=[b_s1[i]], wr=[b_s1[i]])
                for i, c in enumerate(cs_):
                    kb.op("dve", lambda e: e.reciprocal(out=r1_[i][:], in_=s1_[i][:]), rd=[b_s1[i]], wr=[b_r1[i]])
                for i, c in enumerate(cs_):
                    kb.op("dve" if i % 2 == 0 else "pool", lambda e: e.scalar_tensor_tensor(out=ol[i][:], in0=hl[i][:], scalar=r1_[i][:, 0:1], in1=wfin[:],
                                                                  op0=ALU.mult, op1=ALU.mult),
                          rd=[b_hl[i], b_r1[i], b_wfin], wr=[b_ol[i]]) if False else \
                        kb.op("dve", lambda e: e.scalar_tensor_tensor(out=ol[i][:], in0=hl[i][:], scalar=r1_[i][:, 0:1], in1=wfin[:],
                                                                      op0=ALU.mult, op1=ALU.mult),
                              rd=[b_hl[i], b_r1[i], b_wfin], wr=[b_ol[i]])
                    kb.dma("sp", lambda q: q.dma_start(out=out_d[(c - 1) * 128:c * 128, :], in_=ol[i][:]), rd=[b_ol[i]], wr=[outb])
            kb.enabled = True
            kb.barrier()
    return nc


def _host_consts():
    l = np.arange(128)
    Uf = (l[:, None] <= l[None, :]).astype(np.float32)
    Lf = (l[:, None] > l[None, :]).astype(np.float32)
    Ub = (l[:, None] >= l[None, :]).astype(np.float32)
    Lb = (l[:, None] < l[None, :]).astype(np.float32)
    cf = np.zeros((128, C_N), np.float32)
    cf[:, C_UF:C_UF + 128] = Uf
    cf[:, C_LF:C_LF + 128] = Lf
    cf[:, C_UB:C_UB + 128] = Ub
    cf[:, C_LB:C_LB + 128] = Lb
    cf[:, C_ONES:C_ONES + 128] = 1.0
    cf[:, C_ID:C_ID + 128] = np.eye(128, dtype=np.float32)
    cf[:, C_IOTA:C_IOTA + 516] = np.arange(516, dtype=np.float32)[None, :]
    cb = np.zeros((128, CB_N), np.float32)
    cb[:, CB_ID:CB_ID + 128] = np.eye(128)
    cb[:, CB_ONES:CB_ONES + 128] = 1.0
    tv = np.zeros((128, NT, 2), np.float32)
    tv[:, :, 0] = np.arange(NT)[None, :]
    tv[:, :, 1] = np.arange(128)[:, None]
    cb[:, CB_TV:CB_TV + 66] = tv.reshape(128, 66)
    return cf, cb.astype(ml_dtypes.bfloat16)


_NC_CACHE = {}


def kernel(x, meta_tokens, w_norm_mix, w_in, w_conf_dw, b_conf_dw, conf_ln_g, conf_ln_b, w_conf_out,
           w_ssm_conv, b_ssm_conv, ssm_dt_bias, ssm_a_log, ssm_d, w_ssm_norm, w_ssm_out, w_out,
           w_norm_ffn, w_router, w_exp_gate, w_exp_up, w_exp_down, w_norm_final):
    f = lambda a: np.ascontiguousarray(np.asarray(a, dtype=np.float32))
    x = f(x)
    small = np.zeros((128, S_N), np.float32)
    small[:, S_BCONF:S_BCONF + 8] = f(b_conf_dw)[0].reshape(8, 128).T
    small[:, S_LNG:S_LNG + 8] = f(conf_ln_g)[0].reshape(8, 128).T
    small[:, S_LNB:S_LNB + 8] = f(conf_ln_b)[0].reshape(8, 128).T
    small[:, S_WCONF:S_WCONF + 248] = f(w_conf_dw)[0].T.reshape(8, 128, 31).transpose(1, 0, 2).reshape(128, 248)
    small[:, S_WSSM:S_WSSM + 168] = f(w_ssm_conv)[0].T.reshape(24, 128, 7).transpose(1, 0, 2).reshape(128, 168)
    small[:, S_BSSM:S_BSSM + 24] = f(b_ssm_conv)[0].reshape(24, 128).T
    row = np.concatenate([f(w_norm_mix)[0], f(w_ssm_norm)[0], f(w_norm_ffn)[0], f(w_norm_final),
                          f(ssm_dt_bias)[0].reshape(64), f(ssm_a_log)[0].reshape(64), f(ssm_d)[0]])
    bcast = np.ascontiguousarray(np.broadcast_to(row[None, :], (128, B_N)))
    cf, cb = _host_consts()
    if "nc" not in _NC_CACHE:
        _NC_CACHE["nc"] = build_program()
    nc = _NC_CACHE["nc"]
    shared = {
        "meta": f(meta_tokens), "w_in": f(w_in)[0], "w_conf_out": f(w_conf_out)[0], "w_ssm_out": f(w_ssm_out)[0],
        "w_out": f(w_out)[0], "w_router": f(w_router)[0], "w_eg": f(w_exp_gate)[0], "w_eu": f(w_exp_up)[0],
        "w_ed": f(w_exp_down)[0], "smallT": small, "bcast": bcast, "cf32": cf, "cbf": cb,
    }
    in_maps = [dict(shared, x=x[b]) for b in range(8)]
    res = run_bass_kernel_spmd(nc, in_maps, core_ids=list(range(8)))
    return np.stack([np.asarray(r["out"], dtype=np.float32) for r in res.results], axis=0)
```
